# Optimizing a Trainium2 kernel written in Bass

```python
import math
import jax
import jax.numpy as jnp
from jax import lax
import numpy as np

D_MODEL = 1024
BATCH = 8
SEQ = 2048
DEPTH = 2

CTX_LEN = 256
GRID_W = 64

DA_HEADS = 4
DA_DH = 64
DA_DV = 128
ML_HEADS = 4
ML_DH = 64
ML_CONV = 3
ML_CHUNK = 64
GL_HEADS = 4
GL_DK = 32
GL_DV = 64
GL_RANK = 16
GL_NORMALIZER = 16.0
GL_CHUNK = 64
Q_BLOCK = 128
ROPE_BASE = 10000.0
MOE_GROUPS = 4
MOE_PER_GROUP = 8
MOE_EXPERTS = MOE_GROUPS * MOE_PER_GROUP
MOE_TOP_K = 2
MOE_HIDDEN = 512
MOE_BLOCK = 128
ADA_STD = 0.5
DN_ALPHA = (2 * DEPTH) ** 0.25
DN_BETA = (8 * DEPTH) ** -0.25
LN_EPS = 1e-6

DA_W = DA_HEADS * DA_DV
ML_W = ML_HEADS * ML_DH
GL_W = GL_HEADS * GL_DV
MIX_W = DA_W + ML_W + GL_W
IN_SIZES = (
    DA_HEADS * 2 * DA_DH,
    DA_HEADS * 2 * DA_DH,
    DA_W,
    2 * ML_W,
    ML_W,
    ML_W,
    2 * ML_HEADS,
    2 * ML_HEADS,
    GL_HEADS * GL_DK,
    GL_HEADS * GL_DK,
    GL_W,
    GL_W,
    2 * GL_RANK,
)
IN_W = sum(IN_SIZES)
IN_OFFSETS = tuple(int(o) for o in np.cumsum(IN_SIZES)[:-1])

F32 = jnp.float32

kernel_name = 'hybrid_diffattn_mlstm_gla_hmoe'


def _ln(x):
    xf = x.astype(F32)
    mu = jnp.mean(xf, -1, keepdims=True)
    var = jnp.mean(jnp.square(xf - mu), -1, keepdims=True)
    return (xf - mu) * lax.rsqrt(var + LN_EPS)


def modulate(x, shift, scale):
    return (_ln(x) * (1.0 + scale) + shift).astype(x.dtype)


def post_norm(x, g, b):
    return (_ln(x) * g + b).astype(x.dtype)


def head_rms(h, g, dtype):
    hf = h.astype(F32)
    y = hf * lax.rsqrt(jnp.mean(jnp.square(hf), -1, keepdims=True) + LN_EPS) * g
    return y.reshape(*h.shape[:-2], -1).astype(dtype)


def rope_2d(n_rows, dim):
    n_freq = dim // 4
    inv_freq = ROPE_BASE ** (-jnp.arange(n_freq, dtype=F32) / n_freq)
    row = jnp.repeat(jnp.arange(n_rows, dtype=F32), GRID_W)
    col = jnp.tile(jnp.arange(GRID_W, dtype=F32), n_rows)
    ang_r = row[:, None] * inv_freq
    ang_c = col[:, None] * inv_freq
    ang = jnp.concatenate([ang_r, ang_r, ang_c, ang_c], axis=-1)
    return jnp.cos(ang), jnp.sin(ang)


def apply_rope(x, cos, sin):
    x1, x2, x3, x4 = jnp.split(x, 4, axis=-1)
    rot = jnp.concatenate([-x2, x1, -x4, x3], axis=-1)
    c, s = cos[:, None, None, :], sin[:, None, None, :]
    return (x.astype(F32) * c + rot.astype(F32) * s).astype(x.dtype)


def short_conv(x, w, b):
    y = lax.conv_general_dilated(x, w[:, None, :].astype(x.dtype), window_strides=(1,), padding='SAME',
                                 dimension_numbers=('NWC', 'WIO', 'NWC'), feature_group_count=x.shape[-1])
    return y + b.astype(x.dtype)


def diff_attend(q, k, v, lam):
    s = jnp.einsum('bqhmd,bkhmd->bhmqk', q, k, preferred_element_type=F32) * DA_DH ** -0.5
    p = jax.nn.softmax(s, axis=-1)
    a = p[:, :, 0] - lam * p[:, :, 1]
    return jnp.einsum('bhqk,bkhd->bqhd', a.astype(v.dtype), v)


def diff_attention(lat, ctx, lam_vecs, norm_g, lam_init, cos, sin, ctx_out):
    (q_l, k_l, v_l), (q_c, k_c, v_c) = lat, ctx
    B, S, _ = q_l.shape
    qk_heads = lambda t: t.reshape(t.shape[0], t.shape[1], DA_HEADS, 2, DA_DH)
    v_heads = lambda t: t.reshape(t.shape[0], t.shape[1], DA_HEADS, DA_DV)
    q_l = apply_rope(qk_heads(q_l), cos, sin)
    k_l = apply_rope(qk_heads(k_l), cos, sin)
    q_c, k_c, v_c, v_l = qk_heads(q_c), qk_heads(k_c), v_heads(v_c), v_heads(v_l)
    lq1, lk1, lq2, lk2 = lam_vecs.astype(F32)
    lam = jnp.exp(jnp.dot(lq1, lk1)) - jnp.exp(jnp.dot(lq2, lk2)) + lam_init
    k_all = jnp.concatenate([k_c, k_l], axis=1)
    v_all = jnp.concatenate([v_c, v_l], axis=1)
    n_blocks = S // Q_BLOCK
    q_blocks = jnp.moveaxis(q_l.reshape(B, n_blocks, Q_BLOCK, DA_HEADS, 2, DA_DH), 1, 0)
    o_l = lax.map(lambda qb: diff_attend(qb, k_all, v_all, lam), q_blocks)
    o_l = jnp.moveaxis(o_l, 0, 1).reshape(B, S, DA_HEADS, DA_DV)
    out_l = head_rms(o_l, norm_g, q_l.dtype) * (1.0 - lam_init)
    out_c = head_rms(diff_attend(q_c, k_c, v_c, lam), norm_g, q_c.dtype) * (1.0 - lam_init) if ctx_out else None
    return out_l, out_c


def _chunks(t, L):
    B, H, N = t.shape[:3]
    return jnp.moveaxis(t.reshape(B, H, N // L, L, *t.shape[3:]), 2, 0)


def _unchunk(t):
    t = jnp.moveaxis(t, 0, 2)
    return t.reshape(t.shape[0], t.shape[1], -1, *t.shape[4:])


def mlstm_scan(q, k, v, ig, lf, state):
    L = ML_CHUNK
    causal = jnp.tril(jnp.ones((L, L), dtype=bool))

    def step(carry, xs):
        C, n, m = carry
        qc, kc, vc, ic, fc = xs
        b = jnp.cumsum(fc, axis=-1)
        log_d = jnp.where(causal, b[..., :, None] - b[..., None, :] + ic[..., None, :], -jnp.inf)
        log_a = b + m[..., None]
        m_t = jnp.maximum(log_a, log_d.max(-1))
        d = jnp.exp(log_d - m_t[..., None])
        a = jnp.exp(log_a - m_t)
        s = jnp.einsum('bhld,bhsd->bhls', qc, kc) * d
        num = a[..., None] * jnp.einsum('bhld,bhde->bhle', qc, C) + jnp.einsum('bhls,bhse->bhle', s, vc)
        qn = a * jnp.einsum('bhld,bhd->bhl', qc, n) + s.sum(-1)
        h = num / jnp.maximum(jnp.abs(qn), jnp.exp(-m_t))[..., None]
        m_new = m_t[..., -1]
        w = jnp.exp(b[..., -1:] - b + ic - m_new[..., None])
        carry_decay = jnp.exp(b[..., -1] + m - m_new)
        C = carry_decay[..., None, None] * C + jnp.einsum('bhs,bhsd,bhse->bhde', w, kc, vc)
        n = carry_decay[..., None] * n + jnp.einsum('bhs,bhsd->bhd', w, kc)
        return (C, n, m_new), h

    state, h = lax.scan(step, state, tuple(_chunks(t, L) for t in (q, k, v, ig, lf)))
    return _unchunk(h), state


def gla_scan(q, k, v, la, state):
    L = GL_CHUNK
    causal = jnp.tril(jnp.ones((L, L), dtype=bool))[..., None]

    def step(S, xs):
        qc, kc, vc, lc = xs
        b = jnp.cumsum(lc, axis=-2)
        inter = jnp.einsum('bhld,bhde->bhle', qc * jnp.exp(b), S)
        decay = jnp.exp(jnp.where(causal, b[..., :, None, :] - b[..., None, :, :], -jnp.inf))
        att = jnp.einsum('bhld,bhlsd,bhsd->bhls', qc, decay, kc)
        h = inter + jnp.einsum('bhls,bhse->bhle', att, vc)
        S = jnp.exp(b[..., -1, :])[..., None] * S + jnp.einsum('bhsd,bhse->bhde', kc * jnp.exp(b[..., -1:, :] - b), vc)
        return S, h

    state, h = lax.scan(step, state, tuple(_chunks(t, L) for t in (q, k, v, la)))
    return _unchunk(h), state


def bidirectional_scan(scan_fn, init, shared_c, gates_c, shared_l, gates_l):
    outs_c, outs_l = [], []
    for d in range(2):
        f = (lambda t: jnp.flip(t, axis=2)) if d else (lambda t: t)
        h_c, state = scan_fn(*[f(t) for t in shared_c], *[f(g[d]) for g in gates_c], init)
        h_l, _ = scan_fn(*[f(t) for t in shared_l], *[f(g[d]) for g in gates_l], state)
        outs_c.append(f(h_c))
        outs_l.append(f(h_l))
    return outs_c[0] + outs_c[1], outs_l[0] + outs_l[1]


def mlstm_mixer(lat, ctx, conv_w, conv_b, ib, fb, norm_g, ctx_out):
    def prep(qk, v, ig, fg):
        B, N, _ = v.shape
        q, k = jnp.split(jax.nn.silu(short_conv(qk, conv_w, conv_b)), 2, axis=-1)
        heads = lambda t: t.reshape(B, N, ML_HEADS, ML_DH).transpose(0, 2, 1, 3).astype(F32)
        gates = lambda g, bias: (g.reshape(B, N, 2, ML_HEADS).astype(F32) + bias).transpose(2, 0, 3, 1)
        return (heads(q) * ML_DH ** -0.5, heads(k), heads(v)), (gates(ig, ib), jax.nn.log_sigmoid(gates(fg, fb)))

    qk_l, v_l, o_l, i_l, f_l = lat
    qk_c, v_c, o_c, i_c, f_c = ctx
    shared_l, gates_l = prep(qk_l, v_l, i_l, f_l)
    shared_c, gates_c = prep(qk_c, v_c, i_c, f_c)
    B = v_l.shape[0]
    init = (jnp.zeros((B, ML_HEADS, ML_DH, ML_DH), F32), jnp.zeros((B, ML_HEADS, ML_DH), F32),
            jnp.zeros((B, ML_HEADS), F32))
    h_c, h_l = bidirectional_scan(mlstm_scan, init, shared_c, gates_c, shared_l, gates_l)
    finish = lambda h, o: head_rms(h.transpose(0, 2, 1, 3), norm_g, o.dtype) * jax.nn.sigmoid(o)
    return finish(h_l, o_l), (finish(h_c, o_c) if ctx_out else None)


def gla_mixer(lat, ctx, wa, ba, norm_g, ctx_out):
    def prep(q, k, v, a):
        B, N, _ = v.shape
        heads = lambda t, dh: t.reshape(B, N, GL_HEADS, dh).transpose(0, 2, 1, 3).astype(F32)
        z = jnp.einsum('bnrj,rjc->rbnc', a.reshape(B, N, 2, GL_RANK), wa) + ba[:, None, None, :]
        log_a = jax.nn.log_sigmoid(z.astype(F32)) / GL_NORMALIZER
        log_a = log_a.reshape(2, B, N, GL_HEADS, GL_DK).transpose(0, 1, 3, 2, 4)
        return (heads(q, GL_DK) * GL_DK ** -0.5, heads(k, GL_DK), heads(v, GL_DV)), (log_a,)

    q_l, k_l, v_l, r_l, a_l = lat
    q_c, k_c, v_c, r_c, a_c = ctx
    shared_l, gates_l = prep(q_l, k_l, v_l, a_l)
    shared_c, gates_c = prep(q_c, k_c, v_c, a_c)
    init = jnp.zeros((v_l.shape[0], GL_HEADS, GL_DK, GL_DV), F32)
    h_c, h_l = bidirectional_scan(gla_scan, init, shared_c, gates_c, shared_l, gates_l)
    finish = lambda h, r: head_rms(h.transpose(0, 2, 1, 3), norm_g, r.dtype) * jax.nn.silu(r)
    return finish(h_l, r_l), (finish(h_c, r_c) if ctx_out else None)


def token_mixers(h_l, h_c, p, lam_init, cos, sin, ctx_out):
    pl = jnp.split(h_l @ p['w_in'], IN_OFFSETS, axis=-1)
    pc = jnp.split(h_c @ p['w_in'], IN_OFFSETS, axis=-1)
    da_l, da_c = diff_attention(pl[0:3], pc[0:3], p['da_lambda'], p['da_norm'], lam_init, cos, sin, ctx_out)
    ml_l, ml_c = mlstm_mixer(pl[3:8], pc[3:8], p['ml_conv_w'], p['ml_conv_b'], p['ml_ib'], p['ml_fb'],
                             p['ml_norm'], ctx_out)
    gl_l, gl_c = gla_mixer(pl[8:13], pc[8:13], p['gl_wa'], p['gl_ba'], p['gl_norm'], ctx_out)
    y_l = jnp.concatenate([da_l, ml_l, gl_l], axis=-1) @ p['w_out']
    y_c = jnp.concatenate([da_c, ml_c, gl_c], axis=-1) @ p['w_out'] if ctx_out else None
    return y_l, y_c


def hier_moe(t, p):
    T, D = t.shape
    g_prob = jax.nn.softmax((t @ p['moe_wg']).astype(F32), axis=-1)
    g_gate, g_idx = lax.top_k(g_prob, 1)
    e_logits = (t @ p['moe_we']).astype(F32).reshape(T, MOE_GROUPS, MOE_PER_GROUP)
    e_logits = jnp.take_along_axis(e_logits, g_idx[:, :, None], axis=1)[:, 0]
    e_gate, e_idx = lax.top_k(jax.nn.softmax(e_logits, axis=-1), MOE_TOP_K)
    weights = g_gate * e_gate / jnp.sum(e_gate, -1, keepdims=True)
    expert = g_idx * MOE_PER_GROUP + e_idx
    A = T * MOE_TOP_K
    n_blocks = -(-(A + MOE_EXPERTS * (MOE_BLOCK - 1)) // MOE_BLOCK)
    flat_e = expert.reshape(-1)
    flat_tok = jnp.repeat(jnp.arange(T, dtype=jnp.int32), MOE_TOP_K)
    order = jnp.argsort(flat_e)
    sorted_e = flat_e[order]
    counts = jnp.bincount(flat_e, length=MOE_EXPERTS)
    padded = (counts + MOE_BLOCK - 1) // MOE_BLOCK * MOE_BLOCK
    start = jnp.cumsum(counts) - counts
    pad_end = jnp.cumsum(padded)
    pad_start = pad_end - padded
    dest = pad_start[sorted_e] + jnp.arange(A, dtype=jnp.int32) - start[sorted_e]
    slot_tok = jnp.full((n_blocks * MOE_BLOCK,), T, jnp.int32).at[dest].set(flat_tok[order])
    slot_w = jnp.zeros((n_blocks * MOE_BLOCK,), F32).at[dest].set(weights.reshape(-1)[order])
    block_e = jnp.minimum(jnp.searchsorted(pad_end, jnp.arange(n_blocks, dtype=jnp.int32) * MOE_BLOCK, side='right'),
                          MOE_EXPERTS - 1)
    x_slots = jnp.concatenate([t, jnp.zeros((1, D), t.dtype)], axis=0)[slot_tok].reshape(n_blocks, MOE_BLOCK, D)

    def expert_block(args):
        xb, e = args
        return (jax.nn.silu(xb @ p['moe_w1'][e]) * (xb @ p['moe_w3'][e])) @ p['moe_w2'][e]

    y_slots = lax.map(expert_block, (x_slots, block_e)).reshape(-1, D)
    y = jax.ops.segment_sum(y_slots.astype(F32) * slot_w[:, None], slot_tok, num_segments=T + 1)[:T]
    return y.astype(t.dtype)


def hybrid_layer(x_l, x_c, c, c_ctx, p, lam_init, cos, sin, ctx_out):
    B, S, D = x_l.shape
    mod_l = jax.nn.silu(c) @ p['w_ada'] + p['b_ada']
    mod_c = jax.nn.silu(c_ctx) @ p['w_ada'] + p['b_ada']
    sh1_l, sc1_l, g1_l, sh2_l, sc2_l, g2_l = jnp.split(mod_l[:, None, :], 6, axis=-1)
    sh1_c, sc1_c, g1_c, sh2_c, sc2_c, g2_c = jnp.split(mod_c, 6, axis=-1)

    y_l, y_c = token_mixers(modulate(x_l, sh1_l, sc1_l), modulate(x_c, sh1_c, sc1_c), p, lam_init, cos, sin, ctx_out)
    x_l = post_norm(DN_ALPHA * x_l + g1_l * y_l, p['ln_mix_g'], p['ln_mix_b'])
    f_l = modulate(x_l, sh2_l, sc2_l).reshape(B * S, D)
    if ctx_out:
        x_c = post_norm(DN_ALPHA * x_c + g1_c * y_c, p['ln_mix_g'], p['ln_mix_b'])
        f_c = modulate(x_c, sh2_c, sc2_c).reshape(-1, D)
        y = hier_moe(jnp.concatenate([f_l, f_c], axis=0), p)
        y_l, y_c = y[:B * S], y[B * S:]
        x_c = post_norm(DN_ALPHA * x_c + g2_c * y_c.reshape(x_c.shape), p['ln_ffn_g'], p['ln_ffn_b'])
    else:
        y_l = hier_moe(f_l, p)
    x_l = post_norm(DN_ALPHA * x_l + g2_l * y_l.reshape(B, S, D), p['ln_ffn_g'], p['ln_ffn_b'])
    return x_l, x_c


def setup_inputs(seed: int = 0) -> dict:
    key = jax.random.key(seed)
    ks = iter(jax.random.split(key, 32))
    nrm = lambda shape, std: std * jax.random.normal(next(ks), shape, F32)
    D = D_MODEL
    return {
        'x': nrm((BATCH, SEQ, D), 1.0),
        'c': nrm((BATCH, D), 1.0),
        'ctx': nrm((BATCH, CTX_LEN, D), 1.0),
        'c_ctx': nrm((D,), 1.0),
        'w_ada': nrm((DEPTH, D, 6 * D), ADA_STD * D ** -0.5),
        'b_ada': nrm((DEPTH, 6 * D), 0.02),
        'w_in': nrm((DEPTH, D, IN_W), D ** -0.5),
        'da_lambda': nrm((DEPTH, 4, DA_DH), 0.1),
        'da_norm': 1.0 + nrm((DEPTH, DA_DV), 0.05),
        'ml_conv_w': nrm((DEPTH, ML_CONV, 2 * ML_W), ML_CONV ** -0.5),
        'ml_conv_b': nrm((DEPTH, 2 * ML_W), 0.02),
        'ml_ib': nrm((DEPTH, 2, ML_HEADS), 0.1),
        'ml_fb': jnp.linspace(3.0, 6.0, ML_HEADS, dtype=F32) + nrm((DEPTH, 2, ML_HEADS), 0.1),
        'ml_norm': 1.0 + nrm((DEPTH, ML_DH), 0.05),
        'gl_wa': nrm((DEPTH, 2, GL_RANK, GL_HEADS * GL_DK), GL_RANK ** -0.5),
        'gl_ba': nrm((DEPTH, 2, GL_HEADS * GL_DK), 0.1),
        'gl_norm': 1.0 + nrm((DEPTH, GL_DV), 0.05),
        'w_out': nrm((DEPTH, MIX_W, D), DN_BETA * MIX_W ** -0.5),
        'ln_mix_g': 1.0 + nrm((DEPTH, D), 0.05),
        'ln_mix_b': nrm((DEPTH, D), 0.02),
        'ln_ffn_g': 1.0 + nrm((DEPTH, D), 0.05),
        'ln_ffn_b': nrm((DEPTH, D), 0.02),
        'moe_wg': nrm((DEPTH, D, MOE_GROUPS), D ** -0.5),
        'moe_we': nrm((DEPTH, D, MOE_EXPERTS), D ** -0.5),
        'moe_w1': nrm((DEPTH, MOE_EXPERTS, D, MOE_HIDDEN), D ** -0.5),
        'moe_w3': nrm((DEPTH, MOE_EXPERTS, D, MOE_HIDDEN), D ** -0.5),
        'moe_w2': nrm((DEPTH, MOE_EXPERTS, MOE_HIDDEN, D), DN_BETA * MOE_HIDDEN ** -0.5),
    }


def reference(x, c, ctx, c_ctx, w_ada, b_ada, w_in, da_lambda, da_norm, ml_conv_w, ml_conv_b, ml_ib, ml_fb,
              ml_norm, gl_wa, gl_ba, gl_norm, w_out, ln_mix_g, ln_mix_b, ln_ffn_g, ln_ffn_b,
              moe_wg, moe_we, moe_w1, moe_w3, moe_w2):
    n_latent = x.shape[1]
    rows = n_latent // GRID_W
    cos, sin = rope_2d(rows, DA_DH)
    x_l, x_c = x, ctx
    for i in range(DEPTH):
        p = dict(w_ada=w_ada[i], b_ada=b_ada[i], w_in=w_in[i], da_lambda=da_lambda[i], da_norm=da_norm[i],
                 ml_conv_w=ml_conv_w[i], ml_conv_b=ml_conv_b[i], ml_ib=ml_ib[i], ml_fb=ml_fb[i],
                 ml_norm=ml_norm[i], gl_wa=gl_wa[i], gl_ba=gl_ba[i], gl_norm=gl_norm[i], w_out=w_out[i],
                 ln_mix_g=ln_mix_g[i], ln_mix_b=ln_mix_b[i], ln_ffn_g=ln_ffn_g[i], ln_ffn_b=ln_ffn_b[i],
                 moe_wg=moe_wg[i], moe_we=moe_we[i], moe_w1=moe_w1[i], moe_w3=moe_w3[i], moe_w2=moe_w2[i])
        lam_init = 0.8 - 0.6 * math.exp(-0.3 * i)
        x_l, x_c = hybrid_layer(x_l, x_c, c, c_ctx, p, lam_init, cos, sin, ctx_out=(i < DEPTH - 1))
    return x_l
```

```python
import math
import contextlib
import numpy as np
import ml_dtypes
import concourse.bass as bass
import concourse.mybir as mybir
from concourse.bass_utils import run_bass_kernel_spmd

F32 = mybir.dt.float32
BF16 = mybir.dt.bfloat16
ALU = mybir.AluOpType
AF = mybir.ActivationFunctionType
AX = mybir.AxisListType

D = 1024
NTOK = 2304
NT = 18
NCTX_T = 2
DEPTH = 2
IN_W = 3376
LN_EPS = 1e-6
DN_ALPHA = (2 * DEPTH) ** 0.25
NE = 32
CAP = 640
NST = CAP // 128
U32 = mybir.dt.uint32
TB = [(0, 512), (512, 512), (1024, 512), (1536, 512), (2048, 256)]

COMPUTE = ("tensor", "vector", "scalar", "gpsimd")
ENGS = ("tensor", "vector", "scalar", "gpsimd", "sync")
NDMASEM = 8


class Prog:
    def __init__(self, nc):
        self.nc = nc
        self.ops = []
        self.sb_base = 16512
        self.sb_top = 16512
        self.sb_limit = 229376 - 64
        self.uid = 0
        self.psum_names = set()
        self.label = ""

    def mark(self):
        return self.sb_top

    def release(self, m):
        self.sb_top = m

    def sb(self, name, shape, dt):
        nbytes = int(np.prod(shape[1:])) * (2 if dt == BF16 else 4)
        nbytes = (nbytes + 63) // 64 * 64
        off = self.sb_top
        assert off + nbytes <= self.sb_limit, f"SBUF overflow {name} {off}+{nbytes}"
        self.sb_top = off + nbytes
        self.uid += 1
        return self.nc.alloc_sbuf_tensor_at(f"{name}_{self.uid}", list(shape), dt, offset=off)

    @staticmethod
    def _keys(lst):
        out = []
        for a in lst:
            if a is None:
                continue
            if isinstance(a, (str, tuple)):
                out.append(a)
            else:
                t = a.tensor if hasattr(a, "tensor") else a
                out.append(t.name)
        return out

    def op(self, eng, fn, reads=(), writes=()):
        self.ops.append(dict(eng=eng, fn=fn, r=self._keys(reads), w=self._keys(writes), dma=False, bar=None, lab=self.label))

    def dma(self, eng, out, in_, reads=None, writes=None, **kw):
        r = self._keys(reads if reads is not None else [in_])
        w = self._keys(writes if writes is not None else [out])
        self.ops.append(dict(eng=eng, fn=lambda e: e.dma_start(out=out, in_=in_, **kw), r=r, w=w, dma=True, bar=None))

    def barrier(self):
        for e in ENGS:
            self.ops.append(dict(eng=e, fn=None, r=[], w=[], dma=False, bar=True))

    def emit(self):
        nc = self.nc
        ops = self.ops
        n = len(ops)
        last_w = {}
        readers = {}
        deps = [None] * n
        pending_dma = []
        last_real = {}
        for i, o in enumerate(ops):
            dd = {}
            if o["bar"]:
                for j in pending_dma:
                    dd[j] = True
                for e2 in ENGS:
                    if e2 != o["eng"] and last_real.get(e2) is not None:
                        dd[last_real[e2]] = True
                if o["eng"] == ENGS[-1]:
                    pending_dma = []
                deps[i] = sorted(dd)
                continue
            d = set()
            for k in o["r"]:
                if k in last_w:
                    d.add((last_w[k], "raw"))
                if k in self.psum_names:
                    for j in readers.get(k, ()):
                        if ops[j]["eng"] != o["eng"]:
                            d.add((j, "rar"))
            for k in o["w"]:
                if k in last_w:
                    d.add((last_w[k], "waw"))
                for j in readers.get(k, ()):
                    d.add((j, "war"))
            for k in o["r"]:
                readers.setdefault(k, []).append(i)
            for k in o["w"]:
                last_w[k] = i
                readers[k] = []
            for j, kind in d:
                if j == i:
                    continue
                oj = ops[j]
                if (not oj["dma"]) and (not o["dma"]) and oj["eng"] == o["eng"]:
                    if o["eng"] == "tensor":
                        continue
                dd[j] = True
            deps[i] = sorted(dd)
            if o["dma"]:
                pending_dma.append(i)
            else:
                last_real[o["eng"]] = i
        signal = [False] * n
        for i in range(n):
            for j in deps[i]:
                signal[j] = True
        cnt = {e: 0 for e in ENGS}
        dcnt = {e: 0 for e in ENGS}
        ev = [None] * n
        for i, o in enumerate(ops):
            e = o["eng"]
            if o["dma"]:
                k = dcnt[e] % NDMASEM
                m = dcnt[e] // NDMASEM + 1
                dcnt[e] += 1
                ev[i] = (("d", e, k), 16 * m)
            elif signal[i]:
                cnt[e] += 1
                ev[i] = (("c", e), cnt[e])
        self.stats = dict(n=n, sig=dict(cnt), dma=dict(dcnt))
        self.vlabels = [o.get("lab", "") for o in ops if o["eng"] == "vector" and o["fn"] is not None and not o["dma"]]
        semkeys = sorted(set(v[0] for v in ev if v is not None), key=str)
        with contextlib.ExitStack() as st:
            sems = {}
            for sk in semkeys:
                sems[sk] = st.enter_context(nc.semaphore("s_" + "_".join(str(x) for x in sk)))
            block = st.enter_context(nc.Block())
            per = {e: [] for e in ENGS}
            for i, o in enumerate(ops):
                per[o["eng"]].append(i)
            final_waits = {}
            for i, o in enumerate(ops):
                if o["dma"]:
                    final_waits[ev[i][0]] = max(final_waits.get(ev[i][0], 0), ev[i][1])

            def run_engine(ename, eobj):
                waited = {}
                for i in per[ename]:
                    o = ops[i]
                    need = {}
                    for j in deps[i]:
                        sk, val = ev[j]
                        need[sk] = max(need.get(sk, 0), val)
                    if o["dma"]:
                        sk, val = ev[i]
                        if val > 16:
                            need[sk] = max(need.get(sk, 0), val - 16)
                    for sk, val in need.items():
                        if waited.get(sk, 0) >= val:
                            continue
                        eobj.wait_ge(sems[sk], val)
                        waited[sk] = val
                    if o["fn"] is None:
                        continue
                    ins = o["fn"](eobj)
                    if ev[i] is not None:
                        sk, val = ev[i]
                        ins.then_inc(sems[sk], 16 if o["dma"] else 1)
                if ename == "sync":
                    for sk, val in final_waits.items():
                        if waited.get(sk, 0) < val:
                            eobj.wait_ge(sems[sk], val)
                    for e2 in COMPUTE:
                        if cnt[e2] > 0:
                            eobj.wait_ge(sems[("c", e2)], cnt[e2])

            block.tensor(lambda e: run_engine("tensor", e))
            block.vector(lambda e: run_engine("vector", e))
            block.scalar(lambda e: run_engine("scalar", e))
            block.gpsimd(lambda e: run_engine("gpsimd", e))
            block.sync(lambda e: run_engine("sync", e))
        return self.stats


def make_consts():
    c = {}
    c["ident_bf"] = np.eye(128, dtype=np.float32).astype(ml_dtypes.bfloat16)
    c["ident_f"] = np.eye(128, dtype=np.float32)
    s = np.arange(128)[:, None]
    l = np.arange(128)[None, :]
    c["tri_f"] = (s <= l).astype(np.float32)
    c["tri_b"] = (s >= l).astype(np.float32)
    n_freq = 16
    inv_freq = (10000.0 ** (-np.arange(n_freq, dtype=np.float32) / n_freq)).astype(np.float32)
    t = np.arange(2048)
    row = (t // 64).astype(np.float32)
    col = (t % 64).astype(np.float32)
    ang_r = row[:, None] * inv_freq
    ang_c = col[:, None] * inv_freq
    ang = np.concatenate([ang_r, ang_r, ang_c, ang_c], axis=-1).astype(np.float32)
    cos = np.cos(ang).astype(np.float32)
    sin = np.sin(ang).astype(np.float32)
    sgn = np.concatenate([-np.ones(16), np.ones(16), -np.ones(16), np.ones(16)]).astype(np.float32)
    cosT = np.ones((128, NTOK), np.float32)
    sinT = np.zeros((128, NTOK), np.float32)
    for m in range(2):
        cosT[64 * m:64 * m + 64, 256:] = cos.T
        sinT[64 * m:64 * m + 64, 256:] = (sin * sgn[None, :]).T
    c["cosT"] = cosT
    c["sinT"] = sinT
    c["ecolC"] = np.tile((np.arange(NE, dtype=np.float32) * CAP)[None, :], (128, 1)).astype(np.float32)
    return c


CONST_DT = {"ident_bf": BF16, "ident_f": F32, "tri_f": F32, "tri_b": F32, "cosT": F32, "sinT": F32, "ecolC": F32}

IN_SHAPES = {
    "xin": ([NTOK, D], F32), "c2": ([D, 2], F32),
    "w_ada": ([DEPTH, D, 6 * D], F32), "b_ada": ([DEPTH, 6 * D], F32), "w_in": ([DEPTH, D, IN_W], F32),
    "da_lambda": ([DEPTH, 4, 64], F32), "da_norm": ([DEPTH, 128], F32),
    "ml_conv_w": ([DEPTH, 3, 512], F32), "ml_conv_b": ([DEPTH, 512], F32),
    "ml_ib": ([DEPTH, 2, 4], F32), "ml_fb": ([DEPTH, 2, 4], F32), "ml_norm": ([DEPTH, 64], F32),
    "gl_wa": ([DEPTH, 2, 16, 128], F32), "gl_ba": ([DEPTH, 2, 128], F32), "gl_norm": ([DEPTH, 64], F32),
    "w_out": ([DEPTH, D, D], F32),
    "ln_mix_g": ([DEPTH, D], F32), "ln_mix_b": ([DEPTH, D], F32),
    "ln_ffn_g": ([DEPTH, D], F32), "ln_ffn_b": ([DEPTH, D], F32),
    "moe_wg": ([DEPTH, D, 4], F32), "moe_we": ([DEPTH, D, 32], F32),
    "moe_w1": ([DEPTH, NE, D, 512], F32), "moe_w3": ([DEPTH, NE, D, 512], F32), "moe_w2": ([DEPTH, NE, 512, D], F32),
}


def build(debug=False, n_layers=DEPTH, stop_after=None):
    nc = bass.Bass("TRN2", target_bir_lowering=False)
    P = Prog(nc)
    I = {}
    for k, (shp, dt) in IN_SHAPES.items():
        I[k] = nc.dram_tensor(k, shp, dt, kind="ExternalInput")
    consts = make_consts()
    for k, v in consts.items():
        I[k] = nc.dram_tensor(k, list(v.shape), CONST_DT[k], kind="ExternalInput")
    out_d = nc.dram_tensor("out", [2048, D], F32, kind="ExternalOutput")
    skind = "ExternalOutput" if debug else "Internal"
    S = {}

    def scratch(name, shape, dt):
        S[name] = nc.dram_tensor(name, list(shape), dt, kind=skind)
        return S[name]

    scratch("modrow", [2, 6 * D], F32)
    scratch("da_qk", [8, 128, NTOK], BF16)
    scratch("da_v", [NTOK, 4 * 130], BF16)
    scratch("ml_qk", [4, 128, NTOK], BF16)
    scratch("ml_v", [NTOK, 4 * 66], BF16)
    scratch("ml_o", [NTOK, 256], F32)
    scratch("ml_la", [2, 2, 128, NTOK], F32)
    scratch("ml_ig", [2, 2, 128, NTOK], F32)
    scratch("gl_qk", [4, 128, NTOK], BF16)
    scratch("gl_v", [NTOK, 256], BF16)
    scratch("gl_r", [NTOK, 256], F32)
    scratch("gl_la", [2, 2, 128, NTOK], F32)
    scratch("mixT", [8, 128, NTOK], BF16)
    scratch("x1", [NTOK, D], F32)
    scratch("xnext", [NTOK, D], F32)
    scratch("xslots", [NE * CAP, D], BF16)
    scratch("yslots", [NE * CAP, D], BF16)
    if debug:
        scratch("dbg_hT", [8, 128, NTOK], BF16)
        scratch("dbg_y", [NTOK, D], F32)
        scratch("dbg_fT", [8, 128, NTOK], BF16)
        scratch("dbg_W", [NTOK, 32], F32)
        scratch("dbg_moe", [NTOK, D], F32)

    pb = [nc.alloc_psum_tensor(f"pb{i}", [128, 512], F32) for i in range(8)]
    P.psum_names = set(t.name for t in pb)

    ident_bf = P.sb("ident_bf", [128, 128], BF16)
    ident_f = P.sb("ident_f", [128, 128], F32)
    tri_f = P.sb("tri_f", [128, 128], F32)
    tri_b = P.sb("tri_b", [128, 128], F32)
    ones_f = P.sb("ones_f", [128, 128], F32)
    modcol = P.sb("modcol", [128, 48, 2], F32)
    nlam = P.sb("nlam", [128, 1], F32)
    Wt = P.sb("Wt", [128, NT, 32], F32)
    idxs = P.sb("idxs", [128, NT, 2], U32)
    wsel = P.sb("wsel", [128, NT, 2], F32)
    ecolC = P.sb("ecolC", [128, 32], F32)
    cntE = P.sb("cntE", [128, 32], F32)
    zt = P.sb("zt", [128, NST, D], BF16)
    persist_mark = P.mark()

    P.dma("sync", ident_bf[:, :], I["ident_bf"][:, :])
    P.dma("sync", ident_f[:, :], I["ident_f"][:, :])
    P.dma("sync", tri_f[:, :], I["tri_f"][:, :])
    P.dma("sync", tri_b[:, :], I["tri_b"][:, :])
    P.dma("sync", ecolC[:, :], I["ecolC"][:, :])
    P.op("vector", lambda e: e.memset(ones_f[:, :], 1.0), [], [ones_f])
    P.op("gpsimd", lambda e: e.memset(zt[:, :, :], 0.0), [], [zt])

    regs = {}

    def get_bc(e):
        if "bc" not in regs:
            regs["bc"] = e.alloc_register("bcreg")
            e.reg_mov(regs["bc"], NE * CAP - 1)
        return regs["bc"]

    def V(fn, r, w):
        P.op("vector", fn, r, w)

    def A(fn, r, w):
        P.op("scalar", fn, r, w)

    def G(fn, r, w):
        P.op("gpsimd", fn, r, w)

    def T(fn, r, w):
        P.op("tensor", fn, r, w)

    def mm(out, lhsT, rhs, start, stop, r=None, w=None):
        T(lambda e: e.matmul(out, lhsT, rhs, start=start, stop=stop), r if r is not None else [lhsT, rhs],
          w if w is not None else [out])

    def tr(out, in_, ident, r=None, w=None):
        T(lambda e: e.transpose(out, in_, ident), r if r is not None else [in_, ident], w if w is not None else [out])

    def ln_stats(xt, tagbuf):
        st, mv, rstd = tagbuf
        V(lambda e: e.bn_stats(st[:, 0, :], xt[:, 0:512]), [xt], [st])
        V(lambda e: e.bn_stats(st[:, 1, :], xt[:, 512:1024]), [xt], [st])
        V(lambda e: e.bn_aggr(mv[:, :], st[:, :, :].rearrange("p a b -> p (a b)")), [st], [mv])
        A(lambda e: e.activation(rstd[:, :], mv[:, 1:2], AF.Sqrt, bias=eps_col[:, :], scale=1.0), [mv, eps_col], [rstd])
        V(lambda e: e.reciprocal(rstd[:, :], rstd[:, :]), [rstd], [rstd])
        return mv[:, 0:1], rstd[:, 0:1]

    eps_col = P.sb("eps_col", [128, 1], F32)
    V(lambda e: e.memset(eps_col[:, :], LN_EPS), [], [eps_col])
    one_col = P.sb("one_col", [128, 1], F32)
    V(lambda e: e.memset(one_col[:, :], 1.0), [], [one_col])
    persist_mark = P.mark()

    def layer(L, x_cur):
        lam_init = 0.8 - 0.6 * math.exp(-0.3 * L)
        last = (L == DEPTH - 1)

        P.label = f"L{L}_P0"
        P.barrier()
        P.release(persist_mark)
        c2 = P.sb("c2", [128, 8, 2], F32)
        c2b = P.sb("c2b", [128, 8, 2], BF16)
        brow = P.sb("brow", [2, 6 * D], F32)
        mrow = P.sb("mrow", [2, 6 * D], F32)
        wa = [P.sb(f"wa{i}", [128, 8, 512], BF16) for i in range(2)]
        P.dma("sync", c2[:, :, :], I["c2"].ap().rearrange("(k p) r -> p k r", p=128))
        A(lambda e: e.activation(c2b[:, :, :], c2[:, :, :], AF.Silu), [c2], [c2b])
        for r in range(2):
            P.dma("sync", brow[r:r + 1, :], I["b_ada"][L:L + 1, :])
        for cb in range(12):
            w = wa[cb % 2]
            P.dma("gpsimd", w[:, :, :], I["w_ada"][L, :, cb * 512:(cb + 1) * 512].rearrange("(k p) n -> p k n", p=128))
            for k in range(8):
                mm(pb[cb % 2][0:2, :], c2b[:, k, :], w[:, k, :], k == 0, k == 7)
            V(lambda e, cb=cb: e.tensor_tensor(mrow[:, cb * 512:(cb + 1) * 512], pb[cb % 2][0:2, :],
                                               brow[:, cb * 512:(cb + 1) * 512], ALU.add),
              [pb[cb % 2], brow], [mrow])
        P.dma("sync", S["modrow"][:, :], mrow[:, :])
        for r in range(2):
            P.dma("sync", modcol[:, :, r], S["modrow"][r].rearrange("(c p) -> p c", p=128),
                  allow_slow_non_contiguous=True)
        V(lambda e: e.tensor_scalar_add(modcol[:, 8:16, :], modcol[:, 8:16, :], 1.0), [modcol], [modcol])
        V(lambda e: e.tensor_scalar_add(modcol[:, 32:40, :], modcol[:, 32:40, :], 1.0), [modcol], [modcol])
        lamt = P.sb("lamt", [128, 4, 64], F32)
        lamp = P.sb("lamp", [128, 2, 64], F32)
        lamd = P.sb("lamd", [128, 2], F32)
        P.dma("sync", lamt[:, :, :], I["da_lambda"][L:L + 1, :, :].broadcast_to([128, 4, 64]))
        V(lambda e: e.tensor_tensor(lamp[:, 0, :], lamt[:, 0, :], lamt[:, 1, :], ALU.mult), [lamt], [lamp])
        V(lambda e: e.tensor_tensor(lamp[:, 1, :], lamt[:, 2, :], lamt[:, 3, :], ALU.mult), [lamt], [lamp])
        V(lambda e: e.tensor_reduce(lamd[:, :], lamp[:, :, :], AX.X, ALU.add), [lamp], [lamd])
        A(lambda e: e.activation(lamd[:, :], lamd[:, :], AF.Exp), [lamd], [lamd])
        V(lambda e: e.scalar_tensor_tensor(nlam[:, :], lamd[:, 1:2], -lam_init, lamd[:, 0:1], ALU.add, ALU.subtract),
          [lamd], [nlam])

        P.label = f"L{L}_P1"
        P.barrier()
        P.release(persist_mark)
        hT = P.sb("hT", [128, 8, NTOK], BF16)
        m1 = P.mark()
        xt = [P.sb(f"xt{i}", [128, D], F32) for i in range(4)]
        xn = [P.sb(f"xn{i}", [128, D], BF16) for i in range(4)]
        stb = [(P.sb(f"st{i}", [128, 2, 6], F32), P.sb(f"mv{i}", [128, 2], F32), P.sb(f"rs{i}", [128, 1], F32))
               for i in range(4)]

        def p1_s1(t):
            xb = xt[t % 4]
            st, mv, rstd = stb[t % 4]
            P.dma("sync", xb[:, :], x_cur[t * 128:(t + 1) * 128, :])
            V(lambda e: e.bn_stats(st[:, 0, :], xb[:, 0:512]), [xb], [st])
            V(lambda e: e.bn_stats(st[:, 1, :], xb[:, 512:1024]), [xb], [st])
            V(lambda e: e.bn_aggr(mv[:, :], st[:, :, :].rearrange("p a b -> p (a b)")), [st], [mv])

        def p1_s2(t):
            xb = xt[t % 4]
            st, mv, rstd = stb[t % 4]
            xnb = xn[t % 4]
            A(lambda e: e.activation(rstd[:, :], mv[:, 1:2], AF.Sqrt, bias=eps_col[:, :], scale=1.0), [mv, eps_col], [rstd])
            V(lambda e: e.reciprocal(rstd[:, :], rstd[:, :]), [rstd], [rstd])
            V(lambda e: e.tensor_scalar(xnb[:, :], xb[:, :], mv[:, 0:1], rstd[:, 0:1], ALU.subtract, ALU.mult), [xb, mv, rstd], [xnb])

        def p1_s3(t):
            b = t % 2
            xnb = xn[t % 4]
            for ch in range(8):
                bank = 4 + 2 * b + ch // 4
                pT = pb[bank][:, :].bitcast(BF16)
                tr(pT[:, (ch % 4) * 128:(ch % 4 + 1) * 128], xnb[:, ch * 128:(ch + 1) * 128], ident_bf[:, :],
                   [xnb, ident_bf], [pb[bank]])

        def p1_s4(t):
            b = t % 2
            r = 1 if t < NCTX_T else 0
            for ch in range(8):
                bank = 4 + 2 * b + ch // 4
                pT = pb[bank][:, :].bitcast(BF16)
                o = hT[:, ch, t * 128:(t + 1) * 128]
                i_ = pT[:, (ch % 4) * 128:(ch % 4 + 1) * 128]
                if ch < 4:
                    A(lambda e, o=o, i_=i_, ch=ch, r=r: e.activation(o, i_, AF.Identity, bias=modcol[:, ch, r:r + 1],
                                                                     scale=modcol[:, 8 + ch, r:r + 1]),
                      [pb[bank], modcol], [("hT", t)])
                else:
                    V(lambda e, o=o, i_=i_, ch=ch, r=r: e.tensor_scalar(o, i_, modcol[:, 8 + ch, r:r + 1],
                                                                        modcol[:, ch, r:r + 1], ALU.mult, ALU.add),
                      [pb[bank], modcol], [("hT", t)])

        for i_ in range(NT + 3):
            if i_ < NT:
                p1_s1(i_)
            if 0 <= i_ - 1 < NT:
                p1_s2(i_ - 1)
            if 0 <= i_ - 2 < NT:
                p1_s3(i_ - 2)
            if 0 <= i_ - 3 < NT:
                p1_s4(i_ - 3)
        hT_all = [("hT", t) for t in range(NT)]
        if debug:
            for ch in range(8):
                P.dma("sync", S["dbg_hT"][ch], hT[:, ch, :], reads=hT_all)
        if stop_after == "P1":
            return None

        P.label = f"L{L}_P2"
        P.barrier()
        P.release(m1)
        win = P.sb("win", [128, 8, IN_W], BF16)
        for (c0, c1) in ((0, 844), (844, 1688), (1688, 2532), (2532, IN_W)):
            P.dma("gpsimd", win[:, :, c0:c1], I["w_in"][L, :, c0:c1].rearrange("(k p) n -> p k n", p=128))
        wrot = P.sb("wrot", [128, 8, 1024], BF16)
        wv = win[:, :, 0:1024].rearrange("p k (g h s) -> p k g h s", h=2, s=16)
        rv = wrot[:, :, :].rearrange("p k (g h s) -> p k g h s", h=2, s=16)
        for k in range(8):
            V(lambda e, k=k: e.tensor_copy(rv[:, k, :, 0, :], wv[:, k, :, 1, :]), [win], [wrot])
            G(lambda e, k=k: e.tensor_copy(rv[:, k, :, 1, :], wv[:, k, :, 0, :]), [win], [wrot])
        cosT = P.sb("cosT", [128, NTOK], F32)
        sinT = P.sb("sinT", [128, NTOK], F32)
        P.dma("sync", cosT[:, :], I["cosT"][:, :])
        P.dma("sync", sinT[:, :], I["sinT"][:, :])
        m2 = P.mark()

        def fm_proj(bank, lhs_fn, M, tb, extra_r=()):
            t0, tn = TB[tb]
            for k in range(8):
                mm(pb[bank][0:M, 0:tn], lhs_fn(k), hT[:, k, t0:t0 + tn], k == 0, k == 7,
                   r=[win, wrot] + hT_all + list(extra_r), w=[pb[bank]])

        P.label = f"L{L}_P2a_daqk"
        stg = [P.sb(f"stg{i}", [128, NTOK], BF16) for i in range(2)]
        t1 = [P.sb(f"t1_{i}", [128, 512], F32) for i in range(2)]
        t2 = [P.sb(f"t2_{i}", [128, 512], F32) for i in range(2)]
        for ch in range(8):
            sg = stg[ch % 2]
            for tb in range(5):
                t0, tn = TB[tb]
                fm_proj(0, lambda k, ch=ch: win[:, k, ch * 128:(ch + 1) * 128], 128, tb)
                fm_proj(1, lambda k, ch=ch: wrot[:, k, ch * 128:(ch + 1) * 128], 128, tb)
                a1, a2 = t1[tb % 2], t2[tb % 2]
                V(lambda e, a1=a1, t0=t0, tn=tn: e.tensor_tensor(a1[:, 0:tn], pb[0][:, 0:tn], cosT[:, t0:t0 + tn], ALU.mult),
                  [pb[0], cosT], [a1])
                V(lambda e, a2=a2, t0=t0, tn=tn: e.tensor_tensor(a2[:, 0:tn], pb[1][:, 0:tn], sinT[:, t0:t0 + tn], ALU.mult),
                  [pb[1], sinT], [a2])
                G(lambda e, a1=a1, a2=a2, sg=sg, t0=t0, tn=tn: e.tensor_tensor(sg[:, t0:t0 + tn], a1[:, 0:tn], a2[:, 0:tn], ALU.add),
                  [a1, a2], [sg])
            P.dma("sync", S["da_qk"][ch], sg[:, :])
        P.barrier()
        P.release(m2)

        P.label = f"L{L}_P2b_mlqk"
        cw = P.sb("cw", [128, 4, 3], F32)
        cbias = P.sb("cbias", [128, 4], F32)
        for j_ in range(3):
            P.dma("sync", cw[:, :, j_], I["ml_conv_w"][L, j_].rearrange("(c p) -> p c", p=128), allow_slow_non_contiguous=True)
        P.dma("sync", cbias[:, :], I["ml_conv_b"][L].rearrange("(c p) -> p c", p=128), allow_slow_non_contiguous=True)
        pre = [P.sb(f"pre{i}", [128, NTOK], F32) for i in range(2)]
        acc = [P.sb(f"acc{i}", [128, NTOK], F32) for i in range(2)]
        stg = [P.sb(f"stgm{i}", [128, NTOK], BF16) for i in range(2)]
        for ch in range(4):
            pr, ac, sg = pre[ch % 2], acc[ch % 2], stg[ch % 2]
            for tb in range(5):
                t0, tn = TB[tb]
                bank = tb % 2
                fm_proj(bank, lambda k, ch=ch: win[:, k, 1536 + ch * 128:1536 + (ch + 1) * 128], 128, tb)
                A(lambda e, pr=pr, bank=bank, t0=t0, tn=tn: e.copy(pr[:, t0:t0 + tn], pb[bank][:, 0:tn]), [pb[bank]], [pr])
            V(lambda e, pr=pr, ac=ac, ch=ch: e.tensor_scalar(ac[:, :], pr[:, :], cw[:, ch, 1:2], cbias[:, ch:ch + 1],
                                                             ALU.mult, ALU.add), [pr, cw, cbias], [ac])
            for (s0, s1) in ((0, 256), (256, NTOK)):
                V(lambda e, pr=pr, ac=ac, ch=ch, s0=s0, s1=s1: e.scalar_tensor_tensor(
                    ac[:, s0 + 1:s1], pr[:, s0:s1 - 1], cw[:, ch, 0:1], ac[:, s0 + 1:s1], ALU.mult, ALU.add),
                  [pr, cw, ac], [ac])
                V(lambda e, pr=pr, ac=ac, ch=ch, s0=s0, s1=s1: e.scalar_tensor_tensor(
                    ac[:, s0:s1 - 1], pr[:, s0 + 1:s1], cw[:, ch, 2:3], ac[:, s0:s1 - 1], ALU.mult, ALU.add),
                  [pr, cw, ac], [ac])
            A(lambda e, ac=ac, sg=sg: e.activation(sg[:, :], ac[:, :], AF.Silu), [ac], [sg])
            P.dma("sync", S["ml_qk"][ch], sg[:, :])
        P.barrier()
        P.release(m2)

        P.label = f"L{L}_P2c_gates"
        wrep = P.sb("wrep", [128, 8, 128], BF16)
        gcol = P.sb("gcol", [128, 2, 2, 2], F32)
        for ty, nm in ((0, "ml_ib"), (1, "ml_fb")):
            for d in range(2):
                for h in range(4):
                    P.dma("sync", gcol[64 * (h % 2):64 * (h % 2) + 64, ty, d, h // 2:h // 2 + 1],
                          I[nm][L, d:d + 1, h:h + 1].broadcast_to([64, 1]))
        ngfb = P.sb("ngfb", [128, 2, 2], F32)
        V(lambda e: e.tensor_scalar_mul(ngfb[:, :, :], gcol[:, 1, :, :], -1.0), [gcol], [ngfb])
        gst = [P.sb(f"gst{i}", [128, NTOK], F32) for i in range(2)]
        gi = 0
        for ty in range(2):
            for d in range(2):
                for cc in range(2):
                    for hh in range(2):
                        colx = 2560 + 8 * ty + 4 * d + 2 * cc + hh
                        V(lambda e, hh=hh, colx=colx: e.tensor_copy(
                            wrep[:, :, 64 * hh:64 * hh + 64], win[:, :, colx:colx + 1].broadcast_to([128, 8, 64])),
                          [win], [wrep])
                    sg = gst[gi % 2]
                    gi += 1
                    for tb in range(5):
                        t0, tn = TB[tb]
                        bank = tb % 2
                        fm_proj(bank, lambda k: wrep[:, k, :], 128, tb, extra_r=[wrep])
                        if ty == 0:
                            A(lambda e, sg=sg, bank=bank, t0=t0, tn=tn, d=d, cc=cc: e.activation(
                                sg[:, t0:t0 + tn], pb[bank][:, 0:tn], AF.Identity, bias=gcol[:, 0, d, cc:cc + 1], scale=1.0),
                              [pb[bank], gcol], [sg])
                        else:
                            A(lambda e, sg=sg, bank=bank, t0=t0, tn=tn, d=d, cc=cc: e.activation(
                                sg[:, t0:t0 + tn], pb[bank][:, 0:tn], AF.Exp, bias=ngfb[:, d, cc:cc + 1], scale=-1.0),
                              [pb[bank], ngfb], [sg])
                    if ty == 1:
                        A(lambda e, sg=sg: e.activation(sg[:, :], sg[:, :], AF.Ln, bias=one_col[:, :], scale=1.0), [sg, one_col], [sg])
                        V(lambda e, sg=sg: e.tensor_scalar_mul(sg[:, :], sg[:, :], -1.0), [sg], [sg])
                    P.dma("sync", S["ml_ig" if ty == 0 else "ml_la"][d, cc], sg[:, :])
        P.barrier()
        P.release(m2)

        P.label = f"L{L}_P2d_gl"
        stg = [P.sb(f"stgg{i}", [128, NTOK], BF16) for i in range(2)]
        wpad = P.sb("wpad", [128, 8, 128], BF16)
        gi = 0
        for qk_ in range(2):
            for cc in range(2):
                sg = stg[gi % 2]
                gi += 1
                G(lambda e: e.memset(wpad[:, :, :], 0.0), [], [wpad])
                for hh in range(2):
                    c0 = 2576 + 128 * qk_ + (2 * cc + hh) * 32
                    G(lambda e, hh=hh, c0=c0: e.tensor_copy(wpad[:, :, 64 * hh:64 * hh + 32], win[:, :, c0:c0 + 32]), [win], [wpad])
                for tb in range(5):
                    t0, tn = TB[tb]
                    bank = tb % 2
                    fm_proj(bank, lambda k: wpad[:, k, :], 128, tb, extra_r=[wpad])
                    A(lambda e, sg=sg, bank=bank, t0=t0, tn=tn: e.copy(sg[:, t0:t0 + tn], pb[bank][:, 0:tn]), [pb[bank]], [sg])
                P.dma("sync", S["gl_qk"][2 * qk_ + cc], sg[:, :])
        aT = P.sb("aT", [32, NTOK], BF16)
        for tb in range(5):
            t0, tn = TB[tb]
            bank = tb % 2
            fm_proj(bank, lambda k: win[:, k, 3344:3376], 32, tb)
            A(lambda e, bank=bank, t0=t0, tn=tn: e.copy(aT[:, t0:t0 + tn], pb[bank][0:32, 0:tn]), [pb[bank]], [aT])
        wap = P.sb("wap", [32, 2, 2, 128], BF16)
        nba = P.sb("nba", [128, 2, 2], F32)
        G(lambda e: e.memset(wap[:, :, :, :], 0.0), [], [wap])
        G(lambda e: e.memset(nba[:, :, :], 0.0), [], [nba])
        for d in range(2):
            for cc in range(2):
                for hh in range(2):
                    h0 = (2 * cc + hh) * 32
                    P.dma("gpsimd", wap[16 * d:16 * d + 16, d, cc, 64 * hh:64 * hh + 32], I["gl_wa"][L, d, :, h0:h0 + 32])
                    P.dma("sync", nba[64 * hh:64 * hh + 32, d, cc:cc + 1], I["gl_ba"][L, d, h0:h0 + 32].rearrange("(p o) -> p o", o=1),
                          allow_slow_non_contiguous=True)
        V(lambda e: e.tensor_scalar_mul(nba[:, :, :], nba[:, :, :], -1.0), [nba], [nba])
        gls = [P.sb(f"gls{i}", [128, NTOK], F32) for i in range(2)]
        gi = 0
        for d in range(2):
            for cc in range(2):
                sg = gls[gi % 2]
                gi += 1
                for tb in range(5):
                    t0, tn = TB[tb]
                    bank = 2 + tb % 2
                    mm(pb[bank][:, 0:tn], wap[:, d, cc, :], aT[:, t0:t0 + tn], True, True)
                    A(lambda e, sg=sg, bank=bank, t0=t0, tn=tn, d=d, cc=cc: e.activation(
                        sg[:, t0:t0 + tn], pb[bank][:, 0:tn], AF.Exp, bias=nba[:, d, cc:cc + 1], scale=-1.0), [pb[bank], nba], [sg])
                A(lambda e, sg=sg: e.activation(sg[:, :], sg[:, :], AF.Ln, bias=one_col[:, :], scale=1.0), [sg, one_col], [sg])
                V(lambda e, sg=sg: e.tensor_scalar_mul(sg[:, :], sg[:, :], -1.0 / 16.0), [sg], [sg])
                P.dma("sync", S["gl_la"][d, cc], sg[:, :])
        P.barrier()
        P.release(m2)

        P.label = f"L{L}_P2e_tm"
        vst = [P.sb(f"vst{i}", [128, 4, 130], BF16) for i in range(2)]
        mvst = [P.sb(f"mvst{i}", [128, 4, 66], BF16) for i in range(2)]
        ost = [P.sb(f"ost{i}", [128, 256], F32) for i in range(2)]
        gvst = [P.sb(f"gvst{i}", [128, 256], BF16) for i in range(2)]
        rst = [P.sb(f"rst{i}", [128, 256], F32) for i in range(2)]
        for i in range(2):
            G(lambda e, i=i: e.memset(vst[i][:, :, :], 0.0), [], [vst[i]])
            G(lambda e, i=i: e.memset(vst[i][:, :, 128:129], 1.0), [], [vst[i]])
            G(lambda e, i=i: e.memset(mvst[i][:, :, :], 0.0), [], [mvst[i]])
            G(lambda e, i=i: e.memset(mvst[i][:, :, 64:65], 1.0), [], [mvst[i]])

        def tm_proj(bank, t, c0, ncol):
            for k in range(8):
                mm(pb[bank][:, 0:ncol], hT[:, k, t * 128:(t + 1) * 128], win[:, k, c0:c0 + ncol], k == 0, k == 7,
                   r=[win, ("hT", t)], w=[pb[bank]])

        for t in range(NT):
            b = t % 2
            ts_ = slice(t * 128, (t + 1) * 128)
            B0, B1, B2 = 3 * b, 3 * b + 1, 3 * b + 2
            tm_proj(B0, t, 1024, 512)
            V(lambda e, b=b, B0=B0: e.tensor_copy(vst[b][:, :, 0:128], pb[B0][:, :].rearrange("p (h d) -> p h d", h=4)), [pb[B0]], [vst[b]])
            P.dma("sync", S["da_v"][ts_, :], vst[b][:, :, :].rearrange("p h d -> p (h d)"))
            tm_proj(B1, t, 2048, 512)
            V(lambda e, b=b, B1=B1: e.tensor_copy(mvst[b][:, :, 0:64], pb[B1][:, 0:256].rearrange("p (h d) -> p h d", h=4)), [pb[B1]], [mvst[b]])
            A(lambda e, b=b, B1=B1: e.activation(ost[b][:, :], pb[B1][:, 256:512], AF.Sigmoid), [pb[B1]], [ost[b]])
            P.dma("sync", S["ml_v"][ts_, :], mvst[b][:, :, :].rearrange("p h d -> p (h d)"))
            P.dma("sync", S["ml_o"][ts_, :], ost[b][:, :])
            tm_proj(B2, t, 2832, 512)
            V(lambda e, b=b, B2=B2: e.tensor_copy(gvst[b][:, :], pb[B2][:, 0:256]), [pb[B2]], [gvst[b]])
            A(lambda e, b=b, B2=B2: e.activation(rst[b][:, :], pb[B2][:, 256:512], AF.Silu), [pb[B2]], [rst[b]])
            P.dma("sync", S["gl_v"][ts_, :], gvst[b][:, :])
            P.dma("sync", S["gl_r"][ts_, :], rst[b][:, :])
        if stop_after == "P2":
            return None

        P.label = f"L{L}_P3"
        P.barrier()
        P.release(persist_mark)
        qk = P.sb("qk", [128, 4, NTOK], BF16)
        kz = P.sb("kz", [128, 2, 4, NTOK], BF16)
        vv = P.sb("vv", [128, NT, 520], BF16)
        for m in range(2):
            G(lambda e, m=m: e.memset(kz[64 * (1 - m):64 * (1 - m) + 64, m, :, :], 0.0), [], [kz])
        for ch in range(4):
            P.dma("sync", qk[:, ch, :], S["da_qk"][ch])
            for m in range(2):
                P.dma("sync", kz[64 * m:64 * m + 64, m, ch, :], S["da_qk"][4 + ch, 64 * m:64 * m + 64, :])
        for t in range(NT):
            P.dma("sync", vv[:, t, :], S["da_v"][t * 128:(t + 1) * 128, :])
        for ex_ in (range(NE) if L == 0 else []):
            P.dma("sync", S["xslots"][ex_ * CAP:(ex_ + 1) * CAP, :].rearrange("(s p) d -> p s d", p=128), zt[:, :, :],
                  writes=[("xslots", t_, k__) for t_ in range(NT) for k__ in range(2)])
        gda = P.sb("gda", [128, 128], F32)
        P.dma("sync", gda[:, :], I["da_norm"][L:L + 1, :].broadcast_to([128, 128]))
        V(lambda e: e.tensor_scalar_mul(gda[:, :], gda[:, :], 1.0 - lam_init), [gda], [gda])
        Eb = [P.sb(f"Eb{i}", [128, 512], BF16) for i in range(3)]
        osb = [P.sb(f"osb{i}", [128, 128], F32) for i in range(2)]
        o2 = [P.sb(f"o2{i}", [128, 128], F32) for i in range(2)]
        sq = [P.sb(f"sq{i}", [128, 128], F32) for i in range(2)]
        rc = [P.sb(f"rc{i}", [128, 4], F32) for i in range(2)]
        oall = P.sb("oall", [128, NT, 4, 128], BF16)
        vvh = vv[:, :, :].rearrange("p t (h d) -> p t h d", h=4)
        qblocks = [(0, 256, [0, 1])] + [(256 + 512 * i, 512, list(range(NT))) for i in range(4)]
        nonlocal_ei = [0]
        oi = 0
        rnd = 0
        for h in range(4):
            for (q0, qn, kts) in qblocks:
                nqs = qn // 128
                ob = 2 + 3 * (rnd % 2)
                rnd += 1
                touched = set()
                seq = [(m, kt, qs) for m in range(2) for kt in kts for qs in range(nqs)]
                lastt = {}
                for (m, kt, qs) in seq:
                    lastt[(qs * 2 + m) // 3] = (m, kt, qs)
                steps = [(m, kt) for m in range(2) for kt in kts]

                def issue_scores(i):
                    m, kt = steps[i]
                    sbank = i % 2
                    mm(pb[sbank][:, 0:qn], kz[:, m, h, kt * 128:(kt + 1) * 128], qk[:, h, q0:q0 + qn], True, True,
                       r=[kz, qk], w=[pb[sbank]])
                    nonlocal_ei[0] += 1
                    E = Eb[nonlocal_ei[0] % 3]
                    A(lambda e, E=E, sbank=sbank, qn=qn: e.activation(E[:, 0:qn], pb[sbank][:, 0:qn], AF.Exp, scale=0.125),
                      [pb[sbank]], [E])
                    return E

                Es = {0: issue_scores(0)}
                for i, (m, kt) in enumerate(steps):
                    if i + 1 < len(steps):
                        Es[i + 1] = issue_scores(i + 1)
                    E = Es.pop(i)
                    for qs in range(nqs):
                        a = qs * 2 + m
                        bank = ob + a // 3
                        c0 = 130 * (a % 3)
                        st_ = bank not in touched
                        touched.add(bank)
                        sp_ = lastt[a // 3] == (m, kt, qs)
                        mm(pb[bank][:, c0:c0 + 129], E[:, qs * 128:(qs + 1) * 128], vvh[:, kt, h, 0:129], st_, sp_,
                           r=[E, vv], w=[pb[bank]])
                for qs in range(nqs):
                    j = oi % 2
                    oi += 1
                    a0, a1 = qs * 2, qs * 2 + 1
                    b0, c0 = ob + a0 // 3, 130 * (a0 % 3)
                    b1, c1 = ob + a1 // 3, 130 * (a1 % 3)
                    tq = (q0 + qs * 128) // 128
                    V(lambda e, j=j, b0=b0, c0=c0: e.reciprocal(rc[j][:, 0:1], pb[b0][:, c0 + 128:c0 + 129]), [pb[b0]], [rc[j]])
                    V(lambda e, j=j, b1=b1, c1=c1: e.reciprocal(rc[j][:, 1:2], pb[b1][:, c1 + 128:c1 + 129]), [pb[b1]], [rc[j]])
                    V(lambda e, j=j: e.tensor_tensor(rc[j][:, 1:2], rc[j][:, 1:2], nlam[:, :], ALU.mult), [rc[j], nlam], [rc[j]])
                    V(lambda e, j=j, b0=b0, c0=c0: e.tensor_scalar(osb[j][:, :], pb[b0][:, c0:c0 + 128], rc[j][:, 0:1], None, ALU.mult),
                      [pb[b0], rc[j]], [osb[j]])
                    V(lambda e, j=j, b1=b1, c1=c1: e.scalar_tensor_tensor(o2[j][:, :], pb[b1][:, c1:c1 + 128], rc[j][:, 1:2],
                                                                         osb[j][:, :], ALU.mult, ALU.add),
                      [pb[b1], rc[j], osb[j]], [o2[j]])
                    G(lambda e, j=j: e.tensor_tensor(sq[j][:, :], o2[j][:, :], o2[j][:, :], ALU.mult), [o2[j]], [sq[j]])
                    V(lambda e, j=j: e.tensor_reduce(rc[j][:, 2:3], sq[j][:, :], AX.X, ALU.add), [sq[j]], [rc[j]])
                    A(lambda e, j=j: e.activation(rc[j][:, 2:3], rc[j][:, 2:3], AF.Sqrt, bias=eps_col[:, :], scale=1.0 / 128.0),
                      [rc[j], eps_col], [rc[j]])
                    V(lambda e, j=j: e.reciprocal(rc[j][:, 3:4], rc[j][:, 2:3]), [rc[j]], [rc[j]])
                    V(lambda e, j=j, tq=tq, h=h: e.scalar_tensor_tensor(oall[:, tq, h, :], o2[j][:, :], rc[j][:, 3:4], gda[:, :], ALU.mult, ALU.mult),
                      [o2[j], rc[j], gda], [("oall", tq, h)])
        mixst = [P.sb(f"mixst{i}", [128, NTOK], BF16) for i in range(2)]
        ti = 0
        for h in range(4):
            mst = mixst[h % 2]
            for t0_ in range(0, NT, 4):
                nt_ = min(4, NT - t0_)
                bank = ti % 2
                ti += 1
                pT = pb[bank][:, :].bitcast(BF16)
                for k_ in range(nt_):
                    tr(pT[:, k_ * 128:(k_ + 1) * 128], oall[:, t0_ + k_, h, :], ident_bf[:, :], [("oall", t0_ + k_, h), ident_bf], [pb[bank]])
                if bank == 0:
                    A(lambda e, pT=pT, mst=mst, t0_=t0_, nt_=nt_: e.copy(mst[:, t0_ * 128:(t0_ + nt_) * 128], pT[:, 0:nt_ * 128]), [pb[bank]], [mst])
                else:
                    V(lambda e, pT=pT, mst=mst, t0_=t0_, nt_=nt_: e.tensor_copy(mst[:, t0_ * 128:(t0_ + nt_) * 128], pT[:, 0:nt_ * 128]), [pb[bank]], [mst])
            P.dma("sync", S["mixT"][h], mst[:, :])
        if stop_after == "P3":
            return None

        P.label = f"L{L}_P4/P5"
        def decay_attn(kind):
            P.barrier()
            P.release(persist_mark)
            ml = (kind == "ml")
            P.label = f"L{L}_P45_{kind}"
            ncc = 2
            Hc = 2
            dk = 64
            dva = 65 if ml else 64
            vstride = 66 if ml else 64
            qscale = (64 if ml else 32) ** -0.5
            qT = P.sb("qT", [128, ncc, NTOK], BF16)
            kT = P.sb("kT", [128, ncc, NTOK], BF16)
            for cc in range(ncc):
                P.dma("sync", qT[:, cc, :], S["ml_qk" if ml else "gl_qk"][cc])
                P.dma("sync", kT[:, cc, :], S["ml_qk" if ml else "gl_qk"][2 + cc])
            Vt = P.sb("Vt", [128, NT, 4 * vstride], BF16)
            for t in range(NT):
                P.dma("sync", Vt[:, t, :], S["ml_v" if ml else "gl_v"][t * 128:(t + 1) * 128, :])
            Vh = Vt[:, :, :].rearrange("p t (h d) -> p t h d", h=4)
            Hsum = P.sb("Hsum", [128, NT, 256], F32)
            Hs4 = Hsum[:, :, :].rearrange("p t (h d) -> p t h d", h=4)
            Hsum2 = P.sb("Hsum2", [128, NT, 256], F32)
            Hb4 = Hsum2[:, :, :].rearrange("p t (h d) -> p t h d", h=4)
            chains = [(d, cc) for d in range(2) for cc in range(ncc)]
            laC = {c: P.sb(f"la{c[0]}{c[1]}", [128, NTOK], F32) for c in chains}
            igC = {c: (P.sb(f"ig{c[0]}{c[1]}", [128, NTOK], F32) if ml else None) for c in chains}
            SstC = {c: P.sb(f"Sst{c[0]}{c[1]}", [128, dva], F32) for c in chains}
            SbfC = {c: P.sb(f"Sbf{c[0]}{c[1]}", [128, dva], BF16) for c in chains}
            NB = 4
            bT = [P.sb(f"bT{i}", [128, 128], F32) for i in range(NB)]
            pfx = [P.sb(f"pfx{i}", [128, 128], F32) for i in range(NB)]
            arg = [P.sb(f"arg{i}", [128, 128], F32) for i in range(NB)]
            eq = [P.sb(f"eq{i}", [128, 128], F32) for i in range(NB)]
            ek = [P.sb(f"ek{i}", [128, 128], F32) for i in range(NB)]
            ekh = [P.sb(f"ekh{i}", [128, 128], F32) for i in range(NB)]
            gam = [P.sb(f"gam{i}", [128, 1], F32) for i in range(NB)]
            qt_ = [P.sb(f"qt_{i}", [128, 128], BF16) for i in range(NB)]
            kt_ = [P.sb(f"kt_{i}", [128, 128], BF16) for i in range(NB)]
            kh_ = [P.sb(f"kh_{i}", [128, 128], BF16) for i in range(NB)]
            khT = [P.sb(f"khT{i}", [128, 128], BF16) for i in range(NB)]
            PT = [P.sb(f"PT{i}", [128, Hc, 128], BF16) for i in range(NB)]
            den = [P.sb(f"den{i}", [128, Hc, 1], F32) for i in range(NB)]
            for c in chains:
                P.dma("sync", laC[c][:, :], S["ml_la" if ml else "gl_la"][c[0], c[1]])
                if ml:
                    P.dma("sync", igC[c][:, :], S["ml_ig"][c[0], c[1]])
                V(lambda e, c=c: e.memset(SstC[c][:, :], 0.0), [], [SstC[c]])
                V(lambda e, c=c: e.memset(SbfC[c][:, :], 0.0), [], [SbfC[c]])
            orders = {0: list(range(NT)), 1: [1, 0] + list(range(NT - 1, 1, -1))}
            it = 0
            qz = [[P.sb(f"qz{i}_{hh}", [128, 128], BF16) for hh in range(Hc)] for i in range(NB)]
            for i in range(NB):
                for hh in range(Hc):
                    G(lambda e, i=i, hh=hh: e.memset(qz[i][hh][:, :], 0.0), [], [qz[i][hh]])
            G(lambda e: e.tensor_scalar_mul(qT[:, :, :], qT[:, :, :], qscale), [qT], [qT])
            for step in range(NT):
                ctxs = []
                for ci, (d, cc) in enumerate(chains):
                    la, ig = laC[(d, cc)], igC[(d, cc)]
                    t = orders[d][step]
                    j = ci
                    tsl = slice(t * 128, (t + 1) * 128)
                    if d == 0:
                        V(lambda e, j=j, tsl=tsl, la=la: e.tensor_tensor_scan(bT[j][:, :], ones_f[:, :], la[:, tsl], 0.0, ALU.mult, ALU.add),
                          [ones_f, la], [bT[j]])
                        tot = bT[j][:, 127:128]
                    else:
                        V(lambda e, j=j, tsl=tsl, la=la: e.tensor_tensor_scan(pfx[j][:, :], ones_f[:, :], la[:, tsl], 0.0, ALU.mult, ALU.add),
                          [ones_f, la], [pfx[j]])
                        V(lambda e, j=j, tsl=tsl, la=la: e.scalar_tensor_tensor(bT[j][:, :], la[:, tsl], pfx[j][:, 127:128], pfx[j][:, :],
                                                                                 ALU.add, ALU.subtract), [la, pfx[j]], [bT[j]])
                        tot = bT[j][:, 0:1]
                    A(lambda e, j=j: e.activation(eq[j][:, :], bT[j][:, :], AF.Exp), [bT[j]], [eq[j]])
                    for hh in range(Hc):
                        G(lambda e, j=j, cc=cc, tsl=tsl, hh=hh: e.tensor_tensor(
                            qz[j][hh][64 * hh:64 * hh + 64, :], qT[64 * hh:64 * hh + 64, cc, tsl], eq[j][64 * hh:64 * hh + 64, :],
                            ALU.mult), [qT, eq[j]], [qz[j][hh]])
                    if ml:
                        G(lambda e, j=j, tsl=tsl, ig=ig: e.tensor_tensor(arg[j][:, :], ig[:, tsl], bT[j][:, :], ALU.subtract), [ig, bT[j]], [arg[j]])
                        A(lambda e, j=j: e.activation(ek[j][:, :], arg[j][:, :], AF.Exp), [arg[j]], [ek[j]])
                        A(lambda e, j=j, tot=tot: e.activation(ekh[j][:, :], arg[j][:, :], AF.Exp, bias=tot, scale=1.0), [arg[j], bT[j]], [ekh[j]])
                    else:
                        A(lambda e, j=j: e.activation(ek[j][:, :], bT[j][:, :], AF.Exp, scale=-1.0), [bT[j]], [ek[j]])
                        A(lambda e, j=j, tot=tot: e.activation(ekh[j][:, :], bT[j][:, :], AF.Exp, bias=tot, scale=-1.0), [bT[j]], [ekh[j]])
                    A(lambda e, j=j, tot=tot: e.activation(gam[j][:, :], tot, AF.Exp), [bT[j]], [gam[j]])
                    G(lambda e, j=j, cc=cc, tsl=tsl: e.tensor_tensor(kt_[j][:, :], kT[:, cc, tsl], ek[j][:, :], ALU.mult), [kT, ek[j]], [kt_[j]])
                    G(lambda e, j=j, cc=cc, tsl=tsl: e.tensor_tensor(kh_[j][:, :], kT[:, cc, tsl], ekh[j][:, :], ALU.mult), [kT, ekh[j]], [kh_[j]])
                    pTk = pb[7][:, :].bitcast(BF16)
                    tr(pTk[:, 0:128], kh_[j][:, :], ident_bf[:, :], [kh_[j], ident_bf], [pb[7]])
                    A(lambda e, j=j, pTk=pTk: e.copy(khT[j][:, :], pTk[:, 0:128]), [pb[7]], [khT[j]])
                    for hh in range(Hc):
                        mm(pb[ci][:, hh * 128:(hh + 1) * 128], kt_[j][:, :], qz[j][hh][:, :], hh == 0, hh == Hc - 1,
                           r=[kt_[j], qz[j][hh]], w=[pb[ci]])
                    ctxs.append((d, cc, t, j, ci))
                for (d, cc, t, j, ci) in ctxs:
                    tri = tri_f if d == 0 else tri_b
                    Sbf = SbfC[(d, cc)]
                    Sst = SstC[(d, cc)]
                    hb = 4 + ci % 2
                    V(lambda e, j=j, tri=tri, ci=ci: e.tensor_tensor(
                        PT[j][:, :, :], pb[ci][:, 0:Hc * 128].rearrange("p (h l) -> p h l", h=Hc),
                        tri[:, :].unsqueeze(1).broadcast_to([128, Hc, 128]), ALU.mult), [pb[ci], tri], [PT[j]])
                    for hh in range(Hc):
                        head = Hc * cc + hh
                        hc0 = hh * 128
                        mm(pb[hb][:, hc0:hc0 + dva], PT[j][:, hh, :], Vh[:, t, head, 0:dva], hh == 0, False, r=[PT[j], Vt], w=[pb[hb]])
                        mm(pb[hb][:, hc0:hc0 + dva], qz[j][hh][:, :], Sbf[:, :], False, hh == Hc - 1, r=[qz[j][hh], Sbf], w=[pb[hb]])
                    Hx4 = Hs4 if d == 0 else Hb4
                    hkey = ("Hsum", t) if d == 0 else ("Hsum2", t)
                    if ml:
                        A(lambda e, j=j, hb=hb: e.activation(den[j][:, :, :], pb[hb][:, 0:Hc * 128].rearrange("p (h c) -> p h c", h=Hc)[:, :, 64:65],
                                                             AF.Abs), [pb[hb]], [den[j]])
                        V(lambda e, j=j: e.tensor_scalar_max(den[j][:, :, :], den[j][:, :, :], 1.0), [den[j]], [den[j]])
                        V(lambda e, j=j: e.reciprocal(den[j][:, :, :], den[j][:, :, :]), [den[j]], [den[j]])
                    for hh in range(Hc):
                        head = Hc * cc + hh
                        hc0 = hh * 128
                        if ml:
                            A(lambda e, j=j, hh=hh, hb=hb, hc0=hc0, head=head, t=t, Hx4=Hx4: e.activation(
                                Hx4[:, t, head, :], pb[hb][:, hc0:hc0 + 64], AF.Identity, scale=den[j][:, hh, :]), [pb[hb], den[j]], [hkey])
                        else:
                            A(lambda e, t=t, head=head, hb=hb, hc0=hc0, Hx4=Hx4: e.copy(Hx4[:, t, head, :], pb[hb][:, hc0:hc0 + 64]), [pb[hb]], [hkey])
                    for hh in range(Hc):
                        head = Hc * cc + hh
                        mm(pb[6][dk * hh:dk * (hh + 1), 0:dva], khT[j][:, dk * hh:dk * (hh + 1)], Vh[:, t, head, 0:dva], True, True,
                           r=[khT[j], Vt], w=[pb[6]])
                    V(lambda e, j=j, Sst=Sst: e.scalar_tensor_tensor(Sst[:, :], Sst[:, :], gam[j][:, :], pb[6][:, 0:dva], ALU.mult, ALU.add),
                      [Sst, gam[j], pb[6]], [Sst])
                    A(lambda e, Sst=Sst, Sbf=Sbf: e.copy(Sbf[:, :], Sst[:, :]), [Sst], [Sbf])
            P.label = f"L{L}_P45_{kind}_fin"
            gnm = P.sb("gnm", [128, 64], F32)
            P.dma("sync", gnm[:, :], I["ml_norm" if ml else "gl_norm"][L:L + 1, :].broadcast_to([128, 64]))
            gate = [P.sb(f"gate{i}", [128, 256], F32) for i in range(2)]
            sqh = [P.sb(f"sqh{i}", [128, 256], F32) for i in range(2)]
            ssq = [P.sb(f"ssq{i}", [128, 4], F32) for i in range(2)]
            obm = [P.sb(f"obm{i}", [128, 256], BF16) for i in range(2)]
            mst = [P.sb(f"mstm{i}", [128, NTOK], BF16) for i in range(2)]
            for t in range(NT):
                j = t % 2
                P.dma("sync", gate[j][:, :], S["ml_o" if ml else "gl_r"][t * 128:(t + 1) * 128, :])
                G(lambda e, j=j: e.tensor_tensor(gate[j][:, :].rearrange("p (h d) -> p h d", h=4),
                                                 gate[j][:, :].rearrange("p (h d) -> p h d", h=4),
                                                 gnm[:, :].unsqueeze(1).broadcast_to([128, 4, 64]), ALU.mult), [gate[j], gnm], [gate[j]])
                G(lambda e, t=t: e.tensor_tensor(Hsum[:, t, :], Hsum[:, t, :], Hsum2[:, t, :], ALU.add), [("Hsum", t), ("Hsum2", t)], [("Hsum", t)])
                V(lambda e, j=j, t=t: e.tensor_tensor(sqh[j][:, :], Hsum[:, t, :], Hsum[:, t, :], ALU.mult), [("Hsum", t)], [sqh[j]])
                V(lambda e, j=j: e.tensor_reduce(ssq[j][:, :], sqh[j][:, :].rearrange("p (h d) -> p h d", h=4), AX.X, ALU.add), [sqh[j]], [ssq[j]])
                A(lambda e, j=j: e.activation(ssq[j][:, :], ssq[j][:, :], AF.Sqrt, bias=eps_col[:, :], scale=1.0 / 64.0), [ssq[j], eps_col], [ssq[j]])
                V(lambda e, j=j: e.reciprocal(ssq[j][:, :], ssq[j][:, :]), [ssq[j]], [ssq[j]])
                for hh in range(4):
                    V(lambda e, j=j, t=t, hh=hh: e.scalar_tensor_tensor(obm[j][:, hh * 64:(hh + 1) * 64], Hs4[:, t, hh, :], ssq[j][:, hh:hh + 1],
                                                                        gate[j][:, hh * 64:(hh + 1) * 64], ALU.mult, ALU.mult),
                      [("Hsum", t), ssq[j], gate[j]], [obm[j]])
                pT = pb[5][:, :].bitcast(BF16)
                for c2_ in range(2):
                    tr(pT[:, c2_ * 128:(c2_ + 1) * 128], obm[j][:, c2_ * 128:(c2_ + 1) * 128], ident_bf[:, :], [obm[j], ident_bf], [pb[5]])
                    A(lambda e, c2_=c2_, t=t, pT=pT: e.copy(mst[c2_][:, t * 128:(t + 1) * 128], pT[:, c2_ * 128:(c2_ + 1) * 128]), [pb[5]], [mst[c2_]])
            base = 4 if ml else 6
            for c2_ in range(2):
                P.dma("sync", S["mixT"][base + c2_], mst[c2_][:, :])

        decay_attn("ml")
        if stop_after == "P4":
            return None
        decay_attn("gl")
        if stop_after == "P5":
            return None

        P.label = f"L{L}_P6"
        P.barrier()
        P.release(persist_mark)
        m6 = P.mark()
        mixT = P.sb("mixTs", [128, 8, NTOK], BF16)
        for ch in range(8):
            P.dma("sync", mixT[:, ch, :], S["mixT"][ch])
        wo = P.sb("wo", [128, 8, D], BF16)
        P.dma("gpsimd", wo[:, :, :], I["w_out"][L].rearrange("(k p) n -> p k n", p=128))
        g1bc = P.sb("g1bc", [128, 2, D], F32)
        for r in range(2):
            P.dma("sync", g1bc[:, r, :], S["modrow"][r:r + 1, 2 * D:3 * D].broadcast_to([128, D]))
        lng6 = P.sb("lng", [128, D], F32)
        lnb6 = P.sb("lnb", [128, D], F32)
        P.dma("sync", lng6[:, :], I["ln_mix_g"][L:L + 1, :].broadcast_to([128, D]))
        P.dma("sync", lnb6[:, :], I["ln_mix_b"][L:L + 1, :].broadcast_to([128, D]))
        s2bc = P.sb("s2bc", [128, 2, 2, D], F32)
        for r in range(2):
            P.dma("sync", s2bc[:, 0, r, :], S["modrow"][r:r + 1, 3 * D:4 * D].broadcast_to([128, D]))
            P.dma("sync", s2bc[:, 1, r, :], S["modrow"][r:r + 1, 4 * D:5 * D].broadcast_to([128, D]))
        V(lambda e: e.tensor_scalar_add(s2bc[:, 1, :, :], s2bc[:, 1, :, :], 1.0), [s2bc], [s2bc])
        V(lambda e: e.memset(cntE[:, :], 0.0), [], [cntE])
        fTM = [P.sb(f"fTM{i}", [128, D], BF16) for i in range(3)]
        ftmp = [P.sb(f"ftmp{i}", [128, D], F32) for i in range(3)]
        wr = P.sb("wr", [128, 8, 36], F32)
        P.dma("sync", wr[:, :, 0:4], I["moe_wg"][L].rearrange("(k p) n -> p k n", p=128))
        P.dma("sync", wr[:, :, 4:36], I["moe_we"][L].rearrange("(k p) n -> p k n", p=128))
        xt6v = [P.sb(f"xt6{i}", [128, D], F32) for i in range(3)]
        u6v = [P.sb(f"u6{i}", [128, D], F32) for i in range(3)]
        xn6v = [P.sb(f"xn6{i}", [128, D], F32) for i in range(3)]
        fTf = [P.sb(f"fTf{i}", [128, 8, 128], F32) for i in range(3)]
        stb6 = [(P.sb(f"st6{i}", [128, 2, 6], F32), P.sb(f"mv6{i}", [128, 2], F32), P.sb(f"rs6{i}", [128, 1], F32)) for i in range(3)]
        stc = [(P.sb(f"st7{i}", [128, 2, 6], F32), P.sb(f"mv7{i}", [128, 2], F32), P.sb(f"rs7{i}", [128, 1], F32)) for i in range(3)]
        rt2 = [dict(oh1=P.sb(f"oh1{i}", [128, 32], F32), oh2=P.sb(f"oh2{i}", [128, 32], F32), slot=P.sb(f"slot{i}", [128, 32], F32),
                    dst=P.sb(f"dst{i}", [128, 32], F32), ovm=P.sb(f"ovm{i}", [128, 32], F32), tm=P.sb(f"tm{i}", [128, 32], F32),
                    wvv=P.sb(f"wv{i}", [128, 32], F32), c4=P.sb(f"c4{i}", [128, 4], F32)) for i in range(3)]
        rt = [dict(lg=P.sb(f"lg{i}", [128, 36], F32), s1=P.sb(f"s1{i}", [128, 8], F32), oh=P.sb(f"oh{i}", [128, 4], F32),
                   ml_=P.sb(f"mlg{i}", [128, 32], F32), t32=P.sb(f"t32{i}", [128, 32], F32), ex=P.sb(f"ex{i}", [128, 32], F32),
                   e4=P.sb(f"e4{i}", [128, 4], F32)) for i in range(3)]
        def stA1(t):
            b = t % 3
            r = 1 if t < NCTX_T else 0
            tsl = slice(t * 128, (t + 1) * 128)
            P.dma("sync", xt6v[b][:, :], x_cur[tsl, :])
            for half in range(2):
                for k in range(8):
                    mm(pb[half][:, :], mixT[:, k, tsl], wo[:, k, half * 512:(half + 1) * 512], k == 0, k == 7, r=[mixT, wo], w=[pb[half]])
                V(lambda e, b=b, half=half, r=r: e.tensor_tensor(u6v[b][:, half * 512:(half + 1) * 512], pb[half][:, :],
                                                                  g1bc[:, r, half * 512:(half + 1) * 512], ALU.mult), [pb[half], g1bc], [u6v[b]])
                if debug:
                    pass
            V(lambda e, b=b: e.scalar_tensor_tensor(u6v[b][:, :], xt6v[b][:, :], DN_ALPHA, u6v[b][:, :], ALU.mult, ALU.add), [xt6v[b], u6v[b]], [u6v[b]])
            mean, rstd = ln_stats(u6v[b], stb6[b])
            V(lambda e, b=b, mean=mean, rstd=rstd: e.tensor_scalar(u6v[b][:, :], u6v[b][:, :], mean, rstd, ALU.subtract, ALU.mult),
              [u6v[b], stb6[b][1], stb6[b][2]], [u6v[b]])
            G(lambda e, b=b: e.tensor_tensor(u6v[b][:, :], u6v[b][:, :], lng6[:, :], ALU.mult), [u6v[b], lng6], [u6v[b]])
            G(lambda e, b=b: e.tensor_tensor(u6v[b][:, :], u6v[b][:, :], lnb6[:, :], ALU.add), [u6v[b], lnb6], [u6v[b]])
            P.dma("sync", S["x1"][tsl, :], u6v[b][:, :])

        def stA2(t):
            b = t % 3
            r = 1 if t < NCTX_T else 0
            tsl = slice(t * 128, (t + 1) * 128)
            mean2, rstd2 = ln_stats(u6v[b], stc[b])
            V(lambda e, b=b, mean2=mean2, rstd2=rstd2: e.tensor_scalar(xn6v[b][:, :], u6v[b][:, :], mean2, rstd2, ALU.subtract, ALU.mult),
              [u6v[b], stc[b][1], stc[b][2]], [xn6v[b]])
            for ch in range(8):
                bank = 2 + ch // 4
                tr(pb[bank][:, (ch % 4) * 128:(ch % 4 + 1) * 128], xn6v[b][:, ch * 128:(ch + 1) * 128], ident_f[:, :], [xn6v[b], ident_f], [pb[bank]])
            for ch in range(8):
                bank = 2 + ch // 4
                i_ = pb[bank][:, (ch % 4) * 128:(ch % 4 + 1) * 128]
                A(lambda e, b=b, ch=ch, i_=i_, r=r: e.activation(fTf[b][:, ch, :], i_, AF.Identity, bias=modcol[:, 24 + ch, r:r + 1],
                                                                 scale=modcol[:, 32 + ch, r:r + 1]), [pb[bank], modcol], [fTf[b]])
            G(lambda e, b=b, r=r: e.tensor_tensor(ftmp[b][:, :], xn6v[b][:, :], s2bc[:, 1, r, :], ALU.mult), [xn6v[b], s2bc], [ftmp[b]])
            G(lambda e, b=b, r=r: e.tensor_tensor(fTM[b][:, :], ftmp[b][:, :], s2bc[:, 0, r, :], ALU.add), [ftmp[b], s2bc], [fTM[b]])

        def stB(t):
            b = t % 3
            r = 1 if t < NCTX_T else 0
            tsl = slice(t * 128, (t + 1) * 128)
            for k in range(8):
                mm(pb[4][:, 0:36], fTf[b][:, k, :], wr[:, k, :], k == 0, k == 7, r=[fTf[b], wr], w=[pb[4]])
            R_ = rt[b]
            lg, s1, oh, mlg, t32, ex, e4 = R_["lg"], R_["s1"], R_["oh"], R_["ml_"], R_["t32"], R_["ex"], R_["e4"]
            V(lambda e, lg=lg: e.tensor_copy(lg[:, :], pb[4][:, 0:36]), [pb[4]], [lg])
            V(lambda e, lg=lg, s1=s1: e.tensor_reduce(s1[:, 0:1], lg[:, 0:4], AX.X, ALU.max), [lg], [s1])
            V(lambda e, s1=s1: e.tensor_scalar_mul(s1[:, 1:2], s1[:, 0:1], -1.0), [s1], [s1])
            A(lambda e, lg=lg, s1=s1, e4=e4: e.activation(e4[:, :], lg[:, 0:4], AF.Exp, bias=s1[:, 1:2], scale=1.0), [lg, s1], [e4])
            V(lambda e, s1=s1, e4=e4: e.tensor_reduce(s1[:, 2:3], e4[:, :], AX.X, ALU.add), [e4], [s1])
            V(lambda e, s1=s1: e.reciprocal(s1[:, 2:3], s1[:, 2:3]), [s1], [s1])
            V(lambda e, lg=lg, s1=s1, oh=oh: e.tensor_scalar(oh[:, :], lg[:, 0:4], s1[:, 0:1], None, ALU.is_ge), [lg, s1], [oh])
            V(lambda e, oh=oh: e.tensor_scalar(oh[:, :], oh[:, :], -1.0, 1e30, ALU.add, ALU.mult), [oh], [oh])
            for g in range(4):
                V(lambda e, g=g, lg=lg, oh=oh, mlg=mlg: e.tensor_scalar(mlg[:, g * 8:(g + 1) * 8], lg[:, 4 + g * 8:4 + (g + 1) * 8],
                                                                       oh[:, g:g + 1], None, ALU.add), [lg, oh], [mlg])
            V(lambda e, mlg=mlg, s1=s1: e.tensor_reduce(s1[:, 3:4], mlg[:, :], AX.X, ALU.max), [mlg], [s1])
            V(lambda e, mlg=mlg, s1=s1, t32=t32: e.tensor_scalar(t32[:, :], mlg[:, :], s1[:, 3:4], -1e30, ALU.is_ge, ALU.mult), [mlg, s1], [t32])
            V(lambda e, mlg=mlg, t32=t32: e.tensor_tensor(t32[:, :], t32[:, :], mlg[:, :], ALU.add), [mlg, t32], [t32])
            V(lambda e, t32=t32, s1=s1: e.tensor_reduce(s1[:, 4:5], t32[:, :], AX.X, ALU.max), [t32], [s1])
            V(lambda e, mlg=mlg, s1=s1, t32=t32: e.tensor_scalar(t32[:, :], mlg[:, :], s1[:, 4:5], None, ALU.is_ge), [mlg, s1], [t32])
            V(lambda e, s1=s1: e.tensor_scalar_mul(s1[:, 5:6], s1[:, 3:4], -1.0), [s1], [s1])
            A(lambda e, mlg=mlg, s1=s1, ex=ex: e.activation(ex[:, :], mlg[:, :], AF.Exp, bias=s1[:, 5:6], scale=1.0), [mlg, s1], [ex])
            V(lambda e, ex=ex, t32=t32: e.tensor_tensor(ex[:, :], ex[:, :], t32[:, :], ALU.mult), [ex, t32], [ex])
            V(lambda e, ex=ex, s1=s1: e.tensor_reduce(s1[:, 6:7], ex[:, :], AX.X, ALU.add), [ex], [s1])
            V(lambda e, s1=s1: e.reciprocal(s1[:, 6:7], s1[:, 6:7]), [s1], [s1])
            V(lambda e, s1=s1: e.tensor_tensor(s1[:, 6:7], s1[:, 6:7], s1[:, 2:3], ALU.mult), [s1], [s1])
            V(lambda e, ex=ex, s1=s1, t=t: e.tensor_scalar(Wt[:, t, :], ex[:, :], s1[:, 6:7], None, ALU.mult), [ex, s1], [Wt])
            Q_ = rt2[b]
            oh1, oh2, slot, dst, ovm, tm, wvv, c4 = Q_["oh1"], Q_["oh2"], Q_["slot"], Q_["dst"], Q_["ovm"], Q_["tm"], Q_["wvv"], Q_["c4"]
            V(lambda e, mlg=mlg, s1=s1, oh1=oh1: e.tensor_scalar(oh1[:, :], mlg[:, :], s1[:, 3:4], None, ALU.is_ge), [mlg, s1], [oh1])
            V(lambda e, t32=t32, oh1=oh1, oh2=oh2: e.tensor_tensor(oh2[:, :], t32[:, :], oh1[:, :], ALU.subtract), [t32, oh1], [oh2])
            mm(pb[5][:, 0:32], tri_f[:, :], t32[:, :], True, True, r=[tri_f, t32], w=[pb[5]])
            mm(pb[6][:, 0:32], ones_f[:, :], t32[:, :], True, True, r=[ones_f, t32], w=[pb[6]])
            V(lambda e, slot=slot, t32=t32: e.tensor_tensor(slot[:, :], pb[5][:, 0:32], t32[:, :], ALU.subtract), [pb[5], t32], [slot])
            V(lambda e, slot=slot: e.tensor_tensor(slot[:, :], slot[:, :], cntE[:, :], ALU.add), [slot, cntE], [slot])
            V(lambda e: e.tensor_tensor(cntE[:, :], cntE[:, :], pb[6][:, 0:32], ALU.add), [cntE, pb[6]], [cntE])
            V(lambda e, slot=slot, ovm=ovm: e.tensor_scalar(ovm[:, :], slot[:, :], float(CAP), None, ALU.is_ge), [slot], [ovm])
            V(lambda e, slot=slot, dst=dst: e.tensor_tensor(dst[:, :], slot[:, :], ecolC[:, :], ALU.add), [slot, ecolC], [dst])
            V(lambda e, dst=dst, ovm=ovm: e.scalar_tensor_tensor(dst[:, :], ovm[:, :], 1.0e6, dst[:, :], ALU.mult, ALU.add), [ovm, dst], [dst])
            V(lambda e, t=t, ovm=ovm, wvv=wvv: e.tensor_tensor(wvv[:, :], Wt[:, t, :], ovm[:, :], ALU.mult), [Wt, ovm], [wvv])
            V(lambda e, t=t, wvv=wvv: e.tensor_tensor(wvv[:, :], Wt[:, t, :], wvv[:, :], ALU.subtract), [Wt, wvv], [wvv])
            for k_, oh in ((0, oh1), (1, oh2)):
                V(lambda e, oh=oh, dst=dst, tm=tm: e.tensor_tensor(tm[:, :], oh[:, :], dst[:, :], ALU.mult), [oh, dst], [tm])
                V(lambda e, tm=tm, c4=c4, k_=k_: e.tensor_reduce(c4[:, k_:k_ + 1], tm[:, :], AX.X, ALU.add), [tm], [c4])
                V(lambda e, oh=oh, wvv=wvv, tm=tm: e.tensor_tensor(tm[:, :], oh[:, :], wvv[:, :], ALU.mult), [oh, wvv], [tm])
                V(lambda e, tm=tm, t=t, k_=k_: e.tensor_reduce(wsel[:, t, k_:k_ + 1], tm[:, :], AX.X, ALU.add), [tm], [wsel])
            V(lambda e, c4=c4, t=t: e.tensor_copy(idxs[:, t, :], c4[:, 0:2]), [c4], [idxs])
            for k_ in range(2):
                P.ops.append(dict(eng="gpsimd", dma=True, bar=None, r=P._keys([fTM[b], idxs]), w=[("xslots", t, k_)],
                                  fn=lambda e, b=b, t=t, k_=k_: e.indirect_dma_start(
                                      out=S["xslots"][:, :], out_offset=bass.IndirectOffsetOnAxis(idxs[:, t, k_:k_ + 1], 0),
                                      in_=fTM[b][:, :], in_offset=None, bounds_check=get_bc(e), oob_is_err=False)))
        for i_ in range(NT + 2):
            if i_ < NT:
                stA1(i_)
            if 0 <= i_ - 1 < NT:
                stA2(i_ - 1)
            if 0 <= i_ - 2 < NT:
                stB(i_ - 2)
        if debug:
            for t in range(NT):
                P.dma("sync", S["dbg_W"][t * 128:(t + 1) * 128, :], Wt[:, t, :])
        if stop_after == "P6":
            return None

        P.label = f"L{L}_P7"
        P.barrier()
        P.release(m6)
        yacc = P.sb("yacc", [128, NT, D], F32)
        after_yacc = P.mark()
        w1b = [P.sb(f"w1b{i}", [128, 8, 512], BF16) for i in range(2)]
        w3b = [P.sb(f"w3b{i}", [128, 8, 512], BF16) for i in range(2)]
        w2b = [P.sb(f"w2b{i}", [128, 4, D], BF16) for i in range(2)]
        xs = [P.sb(f"xs{i}", [128, NST, D], BF16) for i in range(2)]
        xT = [P.sb(f"xTs{i}", [128, 8, CAP], BF16) for i in range(2)]
        gTb = [P.sb(f"gTb{i}", [128, 4, CAP], BF16) for i in range(2)]
        s1b = [P.sb(f"s1b{i}", [128, 512], F32) for i in range(2)]
        ysb = [P.sb(f"ysb{i}", [128, D], BF16) for i in range(2)]
        SB = [(0, CAP // 2), (CAP // 2, CAP // 2)] if CAP > 512 else [(0, CAP)]
        yi = 0

        def moe_load(ex_):
            eb = ex_ % 2
            P.dma("gpsimd", w1b[eb][:, :, :], I["moe_w1"][L, ex_].rearrange("(k p) n -> p k n", p=128))
            P.dma("gpsimd", w3b[eb][:, :, :], I["moe_w3"][L, ex_].rearrange("(k p) n -> p k n", p=128))
            P.dma("gpsimd", w2b[eb][:, :, :], I["moe_w2"][L, ex_].rearrange("(k p) n -> p k n", p=128))
            P.dma("sync", xs[eb][:, :, :], S["xslots"][ex_ * CAP:(ex_ + 1) * CAP, :].rearrange("(s p) d -> p s d", p=128),
                  reads=[("xslots", t_, k__) for t_ in range(NT) for k__ in range(2)])

        def moe_transposes(ex_):
            eb = ex_ % 2
            for st in range(NST):
                bank = 6 + st % 2
                pT = pb[bank][:, :].bitcast(BF16)
                for k in range(8):
                    tr(pT[:, k * 128:(k + 1) * 128], xs[eb][:, st, k * 128:(k + 1) * 128], ident_bf[:, :], [xs[eb], ident_bf], [pb[bank]])
                if st % 2 == 0:
                    A(lambda e, eb=eb, st=st, pT=pT: e.copy(xT[eb][:, :, st * 128:(st + 1) * 128], pT[:, :].rearrange("p (k s) -> p k s", k=8)),
                      [pb[bank]], [xT[eb]])
                else:
                    V(lambda e, eb=eb, st=st, pT=pT: e.tensor_copy(xT[eb][:, :, st * 128:(st + 1) * 128], pT[:, :].rearrange("p (k s) -> p k s", k=8)),
                      [pb[bank]], [xT[eb]])

        moe_load(0)
        moe_transposes(0)
        for ex_ in range(NE):
            eb = ex_ % 2
            if ex_ + 1 < NE:
                moe_load(ex_ + 1)
            gT = gTb[eb]
            for (t0, tn) in SB:
                for hc in range(4):
                    b1, b3 = (hc % 2) * 2, (hc % 2) * 2 + 1
                    for k in range(8):
                        mm(pb[b1][:, 0:tn], w1b[eb][:, k, hc * 128:(hc + 1) * 128], xT[eb][:, k, t0:t0 + tn], k == 0, k == 7,
                           r=[w1b[eb], xT[eb]], w=[pb[b1]])
                    for k in range(8):
                        mm(pb[b3][:, 0:tn], w3b[eb][:, k, hc * 128:(hc + 1) * 128], xT[eb][:, k, t0:t0 + tn], k == 0, k == 7,
                           r=[w3b[eb], xT[eb]], w=[pb[b3]])
                    sb_ = s1b[hc % 2]
                    A(lambda e, sb_=sb_, b1=b1, tn=tn: e.activation(sb_[:, 0:tn], pb[b1][:, 0:tn], AF.Silu), [pb[b1]], [sb_])
                    V(lambda e, sb_=sb_, b3=b3, tn=tn, gT=gT, hc=hc, t0=t0: e.tensor_tensor(gT[:, hc, t0:t0 + tn], sb_[:, 0:tn], pb[b3][:, 0:tn], ALU.mult),
                      [sb_, pb[b3]], [gT])
            if ex_ + 1 < NE:
                moe_transposes(ex_ + 1)
            for st in range(NST):
                yb = ysb[yi % 2]
                yi += 1
                for half in range(2):
                    bank = 4 + half
                    for hc in range(4):
                        mm(pb[bank][:, :], gT[:, hc, st * 128:(st + 1) * 128], w2b[eb][:, hc, half * 512:(half + 1) * 512], hc == 0, hc == 3,
                           r=[gT, w2b[eb]], w=[pb[bank]])
                    if half == 0:
                        A(lambda e, yb=yb, bank=bank: e.copy(yb[:, 0:512], pb[bank][:, :]), [pb[bank]], [yb])
                    else:
                        V(lambda e, yb=yb, bank=bank: e.tensor_copy(yb[:, 512:1024], pb[bank][:, :]), [pb[bank]], [yb])
                r0 = ex_ * CAP + st * 128
                P.dma("sync", S["yslots"][r0:r0 + 128, :], yb[:, :], writes=[("yslots", ex_, st)])
        P.label = f"L{L}_P7_combine"
        yg = [P.sb(f"yg{i}", [128, D], BF16) for i in range(4)]
        for i in range(4):
            V(lambda e, i=i: e.memset(yg[i][:, :], 0.0), [], [yg[i]])
        for t in range(NT):
            g0, g1_ = yg[(2 * t) % 4], yg[(2 * t + 1) % 4]
            for k_, gt in ((0, g0), (1, g1_)):
                P.ops.append(dict(eng="gpsimd", dma=True, bar=None, r=[("yslots", e_, s_) for e_ in range(NE) for s_ in range(NST)] + P._keys([idxs]), w=P._keys([gt]),
                                  fn=lambda e, t=t, k_=k_, gt=gt: e.indirect_dma_start(
                                      out=gt[:, :], out_offset=None, in_=S["yslots"][:, :],
                                      in_offset=bass.IndirectOffsetOnAxis(idxs[:, t, k_:k_ + 1], 0),
                                      bounds_check=get_bc(e), oob_is_err=False)))
            V(lambda e, t=t, g0=g0: e.tensor_scalar(yacc[:, t, :], g0[:, :], wsel[:, t, 0:1], None, ALU.mult), [g0, wsel], [("yacc", t)])
            V(lambda e, t=t, g1_=g1_: e.scalar_tensor_tensor(yacc[:, t, :], g1_[:, :], wsel[:, t, 1:2], yacc[:, t, :], ALU.mult, ALU.add),
              [g1_, wsel, ("yacc", t)], [("yacc", t)])
        if debug:
            for t in range(NT):
                P.dma("sync", S["dbg_moe"][t * 128:(t + 1) * 128, :], yacc[:, t, :], reads=[("yacc", t)])
        if stop_after == "P7":
            return None

        P.label = f"L{L}_P8"
        P.barrier()
        P.release(after_yacc)
        g2bc = P.sb("g2bc", [128, 2, D], F32)
        for r in range(2):
            P.dma("sync", g2bc[:, r, :], S["modrow"][r:r + 1, 5 * D:6 * D].broadcast_to([128, D]))
        lng8v = P.sb("lng8", [128, D], F32)
        lnb8v = P.sb("lnb8", [128, D], F32)
        P.dma("sync", lng8v[:, :], I["ln_ffn_g"][L:L + 1, :].broadcast_to([128, D]))
        P.dma("sync", lnb8v[:, :], I["ln_ffn_b"][L:L + 1, :].broadcast_to([128, D]))
        xt8v = [P.sb(f"xt8{i}", [128, D], F32) for i in range(4)]
        u8v = [P.sb(f"u8{i}", [128, D], F32) for i in range(4)]
        stb8 = [(P.sb(f"st8{i}", [128, 2, 6], F32), P.sb(f"mv8{i}", [128, 2], F32), P.sb(f"rs8{i}", [128, 1], F32)) for i in range(4)]
        tiles8 = [t for t in range(NT) if not (last and t < NCTX_T)]

        def p8_s1(t):
            b = t % 4
            r = 1 if t < NCTX_T else 0
            st, mv, rstd = stb8[b]
            xb, ub = xt8v[b], u8v[b]
            P.dma("sync", xb[:, :], S["x1"][t * 128:(t + 1) * 128, :])
            V(lambda e: e.tensor_tensor(ub[:, :], yacc[:, t, :], g2bc[:, r, :], ALU.mult), [("yacc", t), g2bc], [ub])
            V(lambda e: e.scalar_tensor_tensor(ub[:, :], xb[:, :], DN_ALPHA, ub[:, :], ALU.mult, ALU.add), [xb, ub], [ub])
            V(lambda e: e.bn_stats(st[:, 0, :], ub[:, 0:512]), [ub], [st])
            V(lambda e: e.bn_stats(st[:, 1, :], ub[:, 512:1024]), [ub], [st])
            V(lambda e: e.bn_aggr(mv[:, :], st[:, :, :].rearrange("p a b -> p (a b)")), [st], [mv])

        def p8_s2(t):
            b = t % 4
            st, mv, rstd = stb8[b]
            ub = u8v[b]
            A(lambda e: e.activation(rstd[:, :], mv[:, 1:2], AF.Sqrt, bias=eps_col[:, :], scale=1.0), [mv, eps_col], [rstd])
            V(lambda e: e.reciprocal(rstd[:, :], rstd[:, :]), [rstd], [rstd])
            V(lambda e: e.tensor_scalar(ub[:, :], ub[:, :], mv[:, 0:1], rstd[:, 0:1], ALU.subtract, ALU.mult), [ub, mv, rstd], [ub])

        def p8_s3(t):
            b = t % 4
            ub = u8v[b]
            G(lambda e: e.tensor_tensor(ub[:, :], ub[:, :], lng8v[:, :], ALU.mult), [ub, lng8v], [ub])
            G(lambda e: e.tensor_tensor(ub[:, :], ub[:, :], lnb8v[:, :], ALU.add), [ub, lnb8v], [ub])
            if last:
                P.dma("sync", out_d[(t - NCTX_T) * 128:(t - NCTX_T + 1) * 128, :], ub[:, :])
            else:
                P.dma("sync", S["xnext"][t * 128:(t + 1) * 128, :], ub[:, :])

        n8 = len(tiles8)
        for i_ in range(n8 + 2):
            if i_ < n8:
                p8_s1(tiles8[i_])
            if 0 <= i_ - 1 < n8:
                p8_s2(tiles8[i_ - 1])
            if 0 <= i_ - 2 < n8:
                p8_s3(tiles8[i_ - 2])
        return S["xnext"]

    x_cur = I["xin"]
    for L_ in range(n_layers):
        x_cur = layer(L_, x_cur)
        if x_cur is None:
            break

    stats = P.emit()
    global _LAST_VLABELS
    _LAST_VLABELS = P.vlabels
    return nc, stats, consts, list(S.keys())


_CACHE = {}


def make_in_maps(inputs, consts):
    x = np.asarray(inputs["x"], np.float32)
    ctx = np.asarray(inputs["ctx"], np.float32)
    c = np.asarray(inputs["c"], np.float32)
    c_ctx = np.asarray(inputs["c_ctx"], np.float32)
    shared = {k: np.ascontiguousarray(np.asarray(inputs[k], np.float32)) for k in IN_SHAPES if k not in ("xin", "c2")}
    shared.update(consts)
    maps = []
    for b in range(x.shape[0]):
        m = dict(shared)
        m["xin"] = np.ascontiguousarray(np.concatenate([ctx[b], x[b]], axis=0))
        m["c2"] = np.ascontiguousarray(np.stack([c[b], c_ctx], axis=1))
        maps.append(m)
    return maps


def kernel(**inputs):
    if "nc" not in _CACHE:
        nc, stats, consts, _ = build(debug=False)
        _CACHE["nc"] = nc
        _CACHE["consts"] = consts
    nc = _CACHE["nc"]
    maps = make_in_maps(inputs, _CACHE["consts"])
    res = run_bass_kernel_spmd(nc, maps, core_ids=list(range(8)))
    out = np.stack([np.asarray(r["out"], np.float32) for r in res.results], axis=0)
    return out
```

```python
import math
import contextlib
import numpy as np
import ml_dtypes
import concourse.bass as bass
import concourse.mybir as mybir
from concourse.bass_utils import run_bass_kernel_spmd

F32 = mybir.dt.float32
BF16 = mybir.dt.bfloat16
ALU = mybir.AluOpType
AF = mybir.ActivationFunctionType
AX = mybir.AxisListType

D = 1024
NTOK = 2304
NT = 18
NCTX_T = 2
DEPTH = 2
IN_W = 3376
LN_EPS = 1e-6
DN_ALPHA = (2 * DEPTH) ** 0.25
NE = 32
CAP = 640
NST = CAP // 128
U32 = mybir.dt.uint32
TB = [(0, 512), (512, 512), (1024, 512), (1536, 512), (2048, 256)]

COMPUTE = ("tensor", "vector", "scalar", "gpsimd")
ENGS = ("tensor", "vector", "scalar", "gpsimd", "sync")
NDMASEM = 8


class Prog:
    def __init__(self, nc):
        self.nc = nc
        self.ops = []
        self.sb_base = 16512
        self.sb_top = 16512
        self.sb_limit = 229376 - 64
        self.uid = 0
        self.psum_names = set()
        self.label = ""

    def mark(self):
        return self.sb_top

    def release(self, m):
        self.sb_top = m

    def sb(self, name, shape, dt):
        nbytes = int(np.prod(shape[1:])) * (2 if dt == BF16 else 4)
        nbytes = (nbytes + 63) // 64 * 64
        off = self.sb_top
        assert off + nbytes <= self.sb_limit, f"SBUF overflow {name} {off}+{nbytes}"
        self.sb_top = off + nbytes
        self.uid += 1
        return self.nc.alloc_sbuf_tensor_at(f"{name}_{self.uid}", list(shape), dt, offset=off)

    @staticmethod
    def _keys(lst):
        out = []
        for a in lst:
            if a is None:
                continue
            if isinstance(a, (str, tuple)):
                out.append(a)
            else:
                t = a.tensor if hasattr(a, "tensor") else a
                out.append(t.name)
        return out

    def op(self, eng, fn, reads=(), writes=()):
        self.ops.append(dict(eng=eng, fn=fn, r=self._keys(reads), w=self._keys(writes), dma=False, bar=None, lab=self.label))

    def dma(self, eng, out, in_, reads=None, writes=None, **kw):
        r = self._keys(reads if reads is not None else [in_])
        w = self._keys(writes if writes is not None else [out])
        self.ops.append(dict(eng=eng, fn=lambda e: e.dma_start(out=out, in_=in_, **kw), r=r, w=w, dma=True, bar=None))

    def barrier(self):
        for e in ENGS:
            self.ops.append(dict(eng=e, fn=None, r=[], w=[], dma=False, bar=True))

    def emit(self):
        nc = self.nc
        ops = self.ops
        n = len(ops)
        last_w = {}
        readers = {}
        deps = [None] * n
        pending_dma = []
        last_real = {}
        for i, o in enumerate(ops):
            dd = {}
            if o["bar"]:
                for j in pending_dma:
                    dd[j] = True
                for e2 in ENGS:
                    if e2 != o["eng"] and last_real.get(e2) is not None:
                        dd[last_real[e2]] = True
                if o["eng"] == ENGS[-1]:
                    pending_dma = []
                deps[i] = sorted(dd)
                continue
            d = set()
            for k in o["r"]:
                if k in last_w:
                    d.add((last_w[k], "raw"))
                if k in self.psum_names:
                    for j in readers.get(k, ()):
                        if ops[j]["eng"] != o["eng"]:
                            d.add((j, "rar"))
            for k in o["w"]:
                if k in last_w:
                    d.add((last_w[k], "waw"))
                for j in readers.get(k, ()):
                    d.add((j, "war"))
            for k in o["r"]:
                readers.setdefault(k, []).append(i)
            for k in o["w"]:
                last_w[k] = i
                readers[k] = []
            for j, kind in d:
                if j == i:
                    continue
                oj = ops[j]
                if (not oj["dma"]) and (not o["dma"]) and oj["eng"] == o["eng"]:
                    if o["eng"] == "tensor":
                        continue
                dd[j] = True
            deps[i] = sorted(dd)
            if o["dma"]:
                pending_dma.append(i)
            else:
                last_real[o["eng"]] = i
        signal = [False] * n
        for i in range(n):
            for j in deps[i]:
                signal[j] = True
        cnt = {e: 0 for e in ENGS}
        dcnt = {e: 0 for e in ENGS}
        ev = [None] * n
        for i, o in enumerate(ops):
            e = o["eng"]
            if o["dma"]:
                k = dcnt[e] % NDMASEM
                m = dcnt[e] // NDMASEM + 1
                dcnt[e] += 1
                ev[i] = (("d", e, k), 16 * m)
            elif signal[i]:
                cnt[e] += 1
                ev[i] = (("c", e), cnt[e])
        self.stats = dict(n=n, sig=dict(cnt), dma=dict(dcnt))
        self.vlabels = [o.get("lab", "") for o in ops if o["eng"] == "vector" and o["fn"] is not None and not o["dma"]]
        semkeys = sorted(set(v[0] for v in ev if v is not None), key=str)
        with contextlib.ExitStack() as st:
            sems = {}
            for sk in semkeys:
                sems[sk] = st.enter_context(nc.semaphore("s_" + "_".join(str(x) for x in sk)))
            block = st.enter_context(nc.Block())
            per = {e: [] for e in ENGS}
            for i, o in enumerate(ops):
                per[o["eng"]].append(i)
            final_waits = {}
            for i, o in enumerate(ops):
                if o["dma"]:
                    final_waits[ev[i][0]] = max(final_waits.get(ev[i][0], 0), ev[i][1])

            def run_engine(ename, eobj):
                waited = {}
                for i in per[ename]:
                    o = ops[i]
                    need = {}
                    for j in deps[i]:
                        sk, val = ev[j]
                        need[sk] = max(need.get(sk, 0), val)
                    if o["dma"]:
                        sk, val = ev[i]
                        if val > 16:
                            need[sk] = max(need.get(sk, 0), val - 16)
                    for sk, val in need.items():
                        if waited.get(sk, 0) >= val:
                            continue
                        eobj.wait_ge(sems[sk], val)
                        waited[sk] = val
                    if o["fn"] is None:
                        continue
                    ins = o["fn"](eobj)
                    if ev[i] is not None:
                        sk, val = ev[i]
                        ins.then_inc(sems[sk], 16 if o["dma"] else 1)
                if ename == "sync":
                    for sk, val in final_waits.items():
                        if waited.get(sk, 0) < val:
                            eobj.wait_ge(sems[sk], val)
                    for e2 in COMPUTE:
                        if cnt[e2] > 0:
                            eobj.wait_ge(sems[("c", e2)], cnt[e2])

            block.tensor(lambda e: run_engine("tensor", e))
            block.vector(lambda e: run_engine("vector", e))
            block.scalar(lambda e: run_engine("scalar", e))
            block.gpsimd(lambda e: run_engine("gpsimd", e))
            block.sync(lambda e: run_engine("sync", e))
        return self.stats


def make_consts():
    c = {}
    c["ident_bf"] = np.eye(128, dtype=np.float32).astype(ml_dtypes.bfloat16)
    c["ident_f"] = np.eye(128, dtype=np.float32)
    s = np.arange(128)[:, None]
    l = np.arange(128)[None, :]
    c["tri_f"] = (s <= l).astype(np.float32)
    c["tri_b"] = (s >= l).astype(np.float32)
    n_freq = 16
    inv_freq = (10000.0 ** (-np.arange(n_freq, dtype=np.float32) / n_freq)).astype(np.float32)
    t = np.arange(2048)
    row = (t // 64).astype(np.float32)
    col = (t % 64).astype(np.float32)
    ang_r = row[:, None] * inv_freq
    ang_c = col[:, None] * inv_freq
    ang = np.concatenate([ang_r, ang_r, ang_c, ang_c], axis=-1).astype(np.float32)
    cos = np.cos(ang).astype(np.float32)
    sin = np.sin(ang).astype(np.float32)
    sgn = np.concatenate([-np.ones(16), np.ones(16), -np.ones(16), np.ones(16)]).astype(np.float32)
    cosT = np.ones((128, NTOK), np.float32)
    sinT = np.zeros((128, NTOK), np.float32)
    for m in range(2):
        cosT[64 * m:64 * m + 64, 256:] = cos.T
        sinT[64 * m:64 * m + 64, 256:] = (sin * sgn[None, :]).T
    c["cosT"] = cosT
    c["sinT"] = sinT
    c["ecolC"] = np.tile((np.arange(NE, dtype=np.float32) * CAP)[None, :], (128, 1)).astype(np.float32)
    return c


CONST_DT = {"ident_bf": BF16, "ident_f": F32, "tri_f": F32, "tri_b": F32, "cosT": F32, "sinT": F32, "ecolC": F32}

IN_SHAPES = {
    "xin": ([NTOK, D], F32), "c2": ([D, 2], F32),
    "w_ada": ([DEPTH, D, 6 * D], F32), "b_ada": ([DEPTH, 6 * D], F32), "w_in": ([DEPTH, D, IN_W], F32),
    "da_lambda": ([DEPTH, 4, 64], F32), "da_norm": ([DEPTH, 128], F32),
    "ml_conv_w": ([DEPTH, 3, 512], F32), "ml_conv_b": ([DEPTH, 512], F32),
    "ml_ib": ([DEPTH, 2, 4], F32), "ml_fb": ([DEPTH, 2, 4], F32), "ml_norm": ([DEPTH, 64], F32),
    "gl_wa": ([DEPTH, 2, 16, 128], F32), "gl_ba": ([DEPTH, 2, 128], F32), "gl_norm": ([DEPTH, 64], F32),
    "w_out": ([DEPTH, D, D], F32),
    "ln_mix_g": ([DEPTH, D], F32), "ln_mix_b": ([DEPTH, D], F32),
    "ln_ffn_g": ([DEPTH, D], F32), "ln_ffn_b": ([DEPTH, D], F32),
    "moe_wg": ([DEPTH, D, 4], F32), "moe_we": ([DEPTH, D, 32], F32),
    "moe_w1": ([DEPTH, NE, D, 512], F32), "moe_w3": ([DEPTH, NE, D, 512], F32), "moe_w2": ([DEPTH, NE, 512, D], F32),
}


def build(debug=False, n_layers=DEPTH, stop_after=None):
    nc = bass.Bass("TRN2", target_bir_lowering=False)
    P = Prog(nc)
    I = {}
    for k, (shp, dt) in IN_SHAPES.items():
        I[k] = nc.dram_tensor(k, shp, dt, kind="ExternalInput")
    consts = make_consts()
    for k, v in consts.items():
        I[k] = nc.dram_tensor(k, list(v.shape), CONST_DT[k], kind="ExternalInput")
    out_d = nc.dram_tensor("out", [2048, D], F32, kind="ExternalOutput")
    skind = "ExternalOutput" if debug else "Internal"
    S = {}

    def scratch(name, shape, dt):
        S[name] = nc.dram_tensor(name, list(shape), dt, kind=skind)
        return S[name]

    scratch("modrow", [2, 6 * D], F32)
    scratch("da_qk", [8, 128, NTOK], BF16)
    scratch("da_v", [NTOK, 4 * 130], BF16)
    scratch("ml_qk", [4, 128, NTOK], BF16)
    scratch("ml_v", [NTOK, 4 * 66], BF16)
    scratch("ml_o", [NTOK, 256], F32)
    scratch("ml_la", [2, 2, 128, NTOK], F32)
    scratch("ml_ig", [2, 2, 128, NTOK], F32)
    scratch("gl_qk", [4, 128, NTOK], BF16)
    scratch("gl_v", [NTOK, 256], BF16)
    scratch("gl_r", [NTOK, 256], F32)
    scratch("gl_la", [2, 2, 128, NTOK], F32)
    scratch("mixT", [8, 128, NTOK], BF16)
    scratch("x1", [NTOK, D], F32)
    scratch("xnext", [NTOK, D], F32)
    scratch("xslots", [NE * CAP, D], BF16)
    scratch("yslots", [NE * CAP, D], BF16)
    if debug:
        scratch("dbg_hT", [8, 128, NTOK], BF16)
        scratch("dbg_y", [NTOK, D], F32)
        scratch("dbg_fT", [8, 128, NTOK], BF16)
        scratch("dbg_W", [NTOK, 32], F32)
        scratch("dbg_moe", [NTOK, D], F32)

    pb = [nc.alloc_psum_tensor(f"pb{i}", [128, 512], F32) for i in range(8)]
    P.psum_names = set(t.name for t in pb)

    ident_bf = P.sb("ident_bf", [128, 128], BF16)
    ident_f = P.sb("ident_f", [128, 128], F32)
    tri_f = P.sb("tri_f", [128, 128], F32)
    tri_b = P.sb("tri_b", [128, 128], F32)
    ones_f = P.sb("ones_f", [128, 128], F32)
    modcol = P.sb("modcol", [128, 48, 2], F32)
    nlam = P.sb("nlam", [128, 1], F32)
    Wt = P.sb("Wt", [128, NT, 32], F32)
    idxs = P.sb("idxs", [128, NT, 2], U32)
    wsel = P.sb("wsel", [128, NT, 2], F32)
    ecolC = P.sb("ecolC", [128, 32], F32)
    cntE = P.sb("cntE", [128, 32], F32)
    zt = P.sb("zt", [128, NST, D], BF16)
    persist_mark = P.mark()

    P.dma("sync", ident_bf[:, :], I["ident_bf"][:, :])
    P.dma("sync", ident_f[:, :], I["ident_f"][:, :])
    P.dma("sync", tri_f[:, :], I["tri_f"][:, :])
    P.dma("sync", tri_b[:, :], I["tri_b"][:, :])
    P.dma("sync", ecolC[:, :], I["ecolC"][:, :])
    P.op("vector", lambda e: e.memset(ones_f[:, :], 1.0), [], [ones_f])
    P.op("gpsimd", lambda e: e.memset(zt[:, :, :], 0.0), [], [zt])

    regs = {}

    def get_bc(e):
        if "bc" not in regs:
            regs["bc"] = e.alloc_register("bcreg")
            e.reg_mov(regs["bc"], NE * CAP - 1)
        return regs["bc"]

    def V(fn, r, w):
        P.op("vector", fn, r, w)

    def A(fn, r, w):
        P.op("scalar", fn, r, w)

    def G(fn, r, w):
        P.op("gpsimd", fn, r, w)

    def T(fn, r, w):
        P.op("tensor", fn, r, w)

    def mm(out, lhsT, rhs, start, stop, r=None, w=None):
        T(lambda e: e.matmul(out, lhsT, rhs, start=start, stop=stop), r if r is not None else [lhsT, rhs],
          w if w is not None else [out])

    def tr(out, in_, ident, r=None, w=None):
        T(lambda e: e.transpose(out, in_, ident), r if r is not None else [in_, ident], w if w is not None else [out])

    def ln_stats(xt, tagbuf):
        st, mv, rstd = tagbuf
        V(lambda e: e.bn_stats(st[:, 0, :], xt[:, 0:512]), [xt], [st])
        V(lambda e: e.bn_stats(st[:, 1, :], xt[:, 512:1024]), [xt], [st])
        V(lambda e: e.bn_aggr(mv[:, :], st[:, :, :].rearrange("p a b -> p (a b)")), [st], [mv])
        A(lambda e: e.activation(rstd[:, :], mv[:, 1:2], AF.Sqrt, bias=eps_col[:, :], scale=1.0), [mv, eps_col], [rstd])
        V(lambda e: e.reciprocal(rstd[:, :], rstd[:, :]), [rstd], [rstd])
        return mv[:, 0:1], rstd[:, 0:1]

    eps_col = P.sb("eps_col", [128, 1], F32)
    V(lambda e: e.memset(eps_col[:, :], LN_EPS), [], [eps_col])
    one_col = P.sb("one_col", [128, 1], F32)
    V(lambda e: e.memset(one_col[:, :], 1.0), [], [one_col])
    persist_mark = P.mark()

    def layer(L, x_cur):
        lam_init = 0.8 - 0.6 * math.exp(-0.3 * L)
        last = (L == DEPTH - 1)

        P.label = f"L{L}_P0"
        P.barrier()
        P.release(persist_mark)
        c2 = P.sb("c2", [128, 8, 2], F32)
        c2b = P.sb("c2b", [128, 8, 2], BF16)
        brow = P.sb("brow", [2, 6 * D], F32)
        mrow = P.sb("mrow", [2, 6 * D], F32)
        wa = [P.sb(f"wa{i}", [128, 8, 512], BF16) for i in range(2)]
        P.dma("sync", c2[:, :, :], I["c2"].ap().rearrange("(k p) r -> p k r", p=128))
        A(lambda e: e.activation(c2b[:, :, :], c2[:, :, :], AF.Silu), [c2], [c2b])
        for r in range(2):
            P.dma("sync", brow[r:r + 1, :], I["b_ada"][L:L + 1, :])
        for cb in range(12):
            w = wa[cb % 2]
            P.dma("gpsimd", w[:, :, :], I["w_ada"][L, :, cb * 512:(cb + 1) * 512].rearrange("(k p) n -> p k n", p=128))
            for k in range(8):
                mm(pb[cb % 2][0:2, :], c2b[:, k, :], w[:, k, :], k == 0, k == 7)
            V(lambda e, cb=cb: e.tensor_tensor(mrow[:, cb * 512:(cb + 1) * 512], pb[cb % 2][0:2, :],
                                               brow[:, cb * 512:(cb + 1) * 512], ALU.add),
              [pb[cb % 2], brow], [mrow])
        P.dma("sync", S["modrow"][:, :], mrow[:, :])
        for r in range(2):
            P.dma("sync", modcol[:, :, r], S["modrow"][r].rearrange("(c p) -> p c", p=128),
                  allow_slow_non_contiguous=True)
        V(lambda e: e.tensor_scalar_add(modcol[:, 8:16, :], modcol[:, 8:16, :], 1.0), [modcol], [modcol])
        V(lambda e: e.tensor_scalar_add(modcol[:, 32:40, :], modcol[:, 32:40, :], 1.0), [modcol], [modcol])
        lamt = P.sb("lamt", [128, 4, 64], F32)
        lamp = P.sb("lamp", [128, 2, 64], F32)
        lamd = P.sb("lamd", [128, 2], F32)
        P.dma("sync", lamt[:, :, :], I["da_lambda"][L:L + 1, :, :].broadcast_to([128, 4, 64]))
        V(lambda e: e.tensor_tensor(lamp[:, 0, :], lamt[:, 0, :], lamt[:, 1, :], ALU.mult), [lamt], [lamp])
        V(lambda e: e.tensor_tensor(lamp[:, 1, :], lamt[:, 2, :], lamt[:, 3, :], ALU.mult), [lamt], [lamp])
        V(lambda e: e.tensor_reduce(lamd[:, :], lamp[:, :, :], AX.X, ALU.add), [lamp], [lamd])
        A(lambda e: e.activation(lamd[:, :], lamd[:, :], AF.Exp), [lamd], [lamd])
        V(lambda e: e.scalar_tensor_tensor(nlam[:, :], lamd[:, 1:2], -lam_init, lamd[:, 0:1], ALU.add, ALU.subtract),
          [lamd], [nlam])

        P.label = f"L{L}_P1"
        P.barrier()
        P.release(persist_mark)
        hT = P.sb("hT", [128, 8, NTOK], BF16)
        m1 = P.mark()
        xt = [P.sb(f"xt{i}", [128, D], F32) for i in range(4)]
        xn = [P.sb(f"xn{i}", [128, D], BF16) for i in range(4)]
        stb = [(P.sb(f"st{i}", [128, 2, 6], F32), P.sb(f"mv{i}", [128, 2], F32), P.sb(f"rs{i}", [128, 1], F32))
               for i in range(4)]

        def p1_s1(t):
            xb = xt[t % 4]
            st, mv, rstd = stb[t % 4]
            P.dma("sync", xb[:, :], x_cur[t * 128:(t + 1) * 128, :])
            V(lambda e: e.bn_stats(st[:, 0, :], xb[:, 0:512]), [xb], [st])
            V(lambda e: e.bn_stats(st[:, 1, :], xb[:, 512:1024]), [xb], [st])
            V(lambda e: e.bn_aggr(mv[:, :], st[:, :, :].rearrange("p a b -> p (a b)")), [st], [mv])

        def p1_s2(t):
            xb = xt[t % 4]
            st, mv, rstd = stb[t % 4]
            xnb = xn[t % 4]
            A(lambda e: e.activation(rstd[:, :], mv[:, 1:2], AF.Sqrt, bias=eps_col[:, :], scale=1.0), [mv, eps_col], [rstd])
            V(lambda e: e.reciprocal(rstd[:, :], rstd[:, :]), [rstd], [rstd])
            V(lambda e: e.tensor_scalar(xnb[:, :], xb[:, :], mv[:, 0:1], rstd[:, 0:1], ALU.subtract, ALU.mult), [xb, mv, rstd], [xnb])

        def p1_s3(t):
            b = t % 2
            xnb = xn[t % 4]
            for ch in range(8):
                bank = 4 + 2 * b + ch // 4
                pT = pb[bank][:, :].bitcast(BF16)
                tr(pT[:, (ch % 4) * 128:(ch % 4 + 1) * 128], xnb[:, ch * 128:(ch + 1) * 128], ident_bf[:, :],
                   [xnb, ident_bf], [pb[bank]])

        def p1_s4(t):
            b = t % 2
            r = 1 if t < NCTX_T else 0
            for ch in range(8):
                bank = 4 + 2 * b + ch // 4
                pT = pb[bank][:, :].bitcast(BF16)
                o = hT[:, ch, t * 128:(t + 1) * 128]
                i_ = pT[:, (ch % 4) * 128:(ch % 4 + 1) * 128]
                if ch < 4:
                    A(lambda e, o=o, i_=i_, ch=ch, r=r: e.activation(o, i_, AF.Identity, bias=modcol[:, ch, r:r + 1],
                                                                     scale=modcol[:, 8 + ch, r:r + 1]),
                      [pb[bank], modcol], [("hT", t)])
                else:
                    V(lambda e, o=o, i_=i_, ch=ch, r=r: e.tensor_scalar(o, i_, modcol[:, 8 + ch, r:r + 1],
                                                                        modcol[:, ch, r:r + 1], ALU.mult, ALU.add),
                      [pb[bank], modcol], [("hT", t)])

        for i_ in range(NT + 3):
            if i_ < NT:
                p1_s1(i_)
            if 0 <= i_ - 1 < NT:
                p1_s2(i_ - 1)
            if 0 <= i_ - 2 < NT:
                p1_s3(i_ - 2)
            if 0 <= i_ - 3 < NT:
                p1_s4(i_ - 3)
        hT_all = [("hT", t) for t in range(NT)]
        if debug:
            for ch in range(8):
                P.dma("sync", S["dbg_hT"][ch], hT[:, ch, :], reads=hT_all)
        if stop_after == "P1":
            return None

        P.label = f"L{L}_P2"
        P.barrier()
        P.release(m1)
        win = P.sb("win", [128, 8, IN_W], BF16)
        for (c0, c1) in ((0, 844), (844, 1688), (1688, 2532), (2532, IN_W)):
            P.dma("gpsimd", win[:, :, c0:c1], I["w_in"][L, :, c0:c1].rearrange("(k p) n -> p k n", p=128))
        wrot = P.sb("wrot", [128, 8, 1024], BF16)
        wv = win[:, :, 0:1024].rearrange("p k (g h s) -> p k g h s", h=2, s=16)
        rv = wrot[:, :, :].rearrange("p k (g h s) -> p k g h s", h=2, s=16)
        for k in range(8):
            V(lambda e, k=k: e.tensor_copy(rv[:, k, :, 0, :], wv[:, k, :, 1, :]), [win], [wrot])
            G(lambda e, k=k: e.tensor_copy(rv[:, k, :, 1, :], wv[:, k, :, 0, :]), [win], [wrot])
        cosT = P.sb("cosT", [128, NTOK], F32)
        sinT = P.sb("sinT", [128, NTOK], F32)
        P.dma("sync", cosT[:, :], I["cosT"][:, :])
        P.dma("sync", sinT[:, :], I["sinT"][:, :])
        m2 = P.mark()

        def fm_proj(bank, lhs_fn, M, tb, extra_r=()):
            t0, tn = TB[tb]
            for k in range(8):
                mm(pb[bank][0:M, 0:tn], lhs_fn(k), hT[:, k, t0:t0 + tn], k == 0, k == 7,
                   r=[win, wrot] + hT_all + list(extra_r), w=[pb[bank]])

        P.label = f"L{L}_P2a_daqk"
        stg = [P.sb(f"stg{i}", [128, NTOK], BF16) for i in range(2)]
        t1 = [P.sb(f"t1_{i}", [128, 512], F32) for i in range(2)]
        t2 = [P.sb(f"t2_{i}", [128, 512], F32) for i in range(2)]
        for ch in range(8):
            sg = stg[ch % 2]
            for tb in range(5):
                t0, tn = TB[tb]
                fm_proj(0, lambda k, ch=ch: win[:, k, ch * 128:(ch + 1) * 128], 128, tb)
                fm_proj(1, lambda k, ch=ch: wrot[:, k, ch * 128:(ch + 1) * 128], 128, tb)
                a1, a2 = t1[tb % 2], t2[tb % 2]
                V(lambda e, a1=a1, t0=t0, tn=tn: e.tensor_tensor(a1[:, 0:tn], pb[0][:, 0:tn], cosT[:, t0:t0 + tn], ALU.mult),
                  [pb[0], cosT], [a1])
                V(lambda e, a2=a2, t0=t0, tn=tn: e.tensor_tensor(a2[:, 0:tn], pb[1][:, 0:tn], sinT[:, t0:t0 + tn], ALU.mult),
                  [pb[1], sinT], [a2])
                G(lambda e, a1=a1, a2=a2, sg=sg, t0=t0, tn=tn: e.tensor_tensor(sg[:, t0:t0 + tn], a1[:, 0:tn], a2[:, 0:tn], ALU.add),
                  [a1, a2], [sg])
            P.dma("sync", S["da_qk"][ch], sg[:, :])
        P.barrier()
        P.release(m2)

        P.label = f"L{L}_P2b_mlqk"
        cw = P.sb("cw", [128, 4, 3], F32)
        cbias = P.sb("cbias", [128, 4], F32)
        for j_ in range(3):
            P.dma("sync", cw[:, :, j_], I["ml_conv_w"][L, j_].rearrange("(c p) -> p c", p=128), allow_slow_non_contiguous=True)
        P.dma("sync", cbias[:, :], I["ml_conv_b"][L].rearrange("(c p) -> p c", p=128), allow_slow_non_contiguous=True)
        pre = [P.sb(f"pre{i}", [128, NTOK], F32) for i in range(2)]
        acc = [P.sb(f"acc{i}", [128, NTOK], F32) for i in range(2)]
        stg = [P.sb(f"stgm{i}", [128, NTOK], BF16) for i in range(2)]
        for ch in range(4):
            pr, ac, sg = pre[ch % 2], acc[ch % 2], stg[ch % 2]
            for tb in range(5):
                t0, tn = TB[tb]
                bank = tb % 2
                fm_proj(bank, lambda k, ch=ch: win[:, k, 1536 + ch * 128:1536 + (ch + 1) * 128], 128, tb)
                A(lambda e, pr=pr, bank=bank, t0=t0, tn=tn: e.copy(pr[:, t0:t0 + tn], pb[bank][:, 0:tn]), [pb[bank]], [pr])
            V(lambda e, pr=pr, ac=ac, ch=ch: e.tensor_scalar(ac[:, :], pr[:, :], cw[:, ch, 1:2], cbias[:, ch:ch + 1],
                                                             ALU.mult, ALU.add), [pr, cw, cbias], [ac])
            for (s0, s1) in ((0, 256), (256, NTOK)):
                V(lambda e, pr=pr, ac=ac, ch=ch, s0=s0, s1=s1: e.scalar_tensor_tensor(
                    ac[:, s0 + 1:s1], pr[:, s0:s1 - 1], cw[:, ch, 0:1], ac[:, s0 + 1:s1], ALU.mult, ALU.add),
                  [pr, cw, ac], [ac])
                V(lambda e, pr=pr, ac=ac, ch=ch, s0=s0, s1=s1: e.scalar_tensor_tensor(
                    ac[:, s0:s1 - 1], pr[:, s0 + 1:s1], cw[:, ch, 2:3], ac[:, s0:s1 - 1], ALU.mult, ALU.add),
                  [pr, cw, ac], [ac])
            A(lambda e, ac=ac, sg=sg: e.activation(sg[:, :], ac[:, :], AF.Silu), [ac], [sg])
            P.dma("sync", S["ml_qk"][ch], sg[:, :])
        P.barrier()
        P.release(m2)

        P.label = f"L{L}_P2c_gates"
        wrep = P.sb("wrep", [128, 8, 128], BF16)
        gcol = P.sb("gcol", [128, 2, 2, 2], F32)
        for ty, nm in ((0, "ml_ib"), (1, "ml_fb")):
            for d in range(2):
                for h in range(4):
                    P.dma("sync", gcol[64 * (h % 2):64 * (h % 2) + 64, ty, d, h // 2:h // 2 + 1],
                          I[nm][L, d:d + 1, h:h + 1].broadcast_to([64, 1]))
        ngfb = P.sb("ngfb", [128, 2, 2], F32)
        V(lambda e: e.tensor_scalar_mul(ngfb[:, :, :], gcol[:, 1, :, :], -1.0), [gcol], [ngfb])
        gst = [P.sb(f"gst{i}", [128, NTOK], F32) for i in range(2)]
        gi = 0
        for ty in range(2):
            for d in range(2):
                for cc in range(2):
                    for hh in range(2):
                        colx = 2560 + 8 * ty + 4 * d + 2 * cc + hh
                        V(lambda e, hh=hh, colx=colx: e.tensor_copy(
                            wrep[:, :, 64 * hh:64 * hh + 64], win[:, :, colx:colx + 1].broadcast_to([128, 8, 64])),
                          [win], [wrep])
                    sg = gst[gi % 2]
                    gi += 1
                    for tb in range(5):
                        t0, tn = TB[tb]
                        bank = tb % 2
                        fm_proj(bank, lambda k: wrep[:, k, :], 128, tb, extra_r=[wrep])
                        if ty == 0:
                            A(lambda e, sg=sg, bank=bank, t0=t0, tn=tn, d=d, cc=cc: e.activation(
                                sg[:, t0:t0 + tn], pb[bank][:, 0:tn], AF.Identity, bias=gcol[:, 0, d, cc:cc + 1], scale=1.0),
                              [pb[bank], gcol], [sg])
                        else:
                            A(lambda e, sg=sg, bank=bank, t0=t0, tn=tn, d=d, cc=cc: e.activation(
                                sg[:, t0:t0 + tn], pb[bank][:, 0:tn], AF.Exp, bias=ngfb[:, d, cc:cc + 1], scale=-1.0),
                              [pb[bank], ngfb], [sg])
                    if ty == 1:
                        A(lambda e, sg=sg: e.activation(sg[:, :], sg[:, :], AF.Ln, bias=one_col[:, :], scale=1.0), [sg, one_col], [sg])
                        V(lambda e, sg=sg: e.tensor_scalar_mul(sg[:, :], sg[:, :], -1.0), [sg], [sg])
                    P.dma("sync", S["ml_ig" if ty == 0 else "ml_la"][d, cc], sg[:, :])
        P.barrier()
        P.release(m2)

        P.label = f"L{L}_P2d_gl"
        stg = [P.sb(f"stgg{i}", [128, NTOK], BF16) for i in range(2)]
        wpad = P.sb("wpad", [128, 8, 128], BF16)
        gi = 0
        for qk_ in range(2):
            for cc in range(2):
                sg = stg[gi % 2]
                gi += 1
                G(lambda e: e.memset(wpad[:, :, :], 0.0), [], [wpad])
                for hh in range(2):
                    c0 = 2576 + 128 * qk_ + (2 * cc + hh) * 32
                    G(lambda e, hh=hh, c0=c0: e.tensor_copy(wpad[:, :, 64 * hh:64 * hh + 32], win[:, :, c0:c0 + 32]), [win], [wpad])
                for tb in range(5):
                    t0, tn = TB[tb]
                    bank = tb % 2
                    fm_proj(bank, lambda k: wpad[:, k, :], 128, tb, extra_r=[wpad])
                    A(lambda e, sg=sg, bank=bank, t0=t0, tn=tn: e.copy(sg[:, t0:t0 + tn], pb[bank][:, 0:tn]), [pb[bank]], [sg])
                P.dma("sync", S["gl_qk"][2 * qk_ + cc], sg[:, :])
        aT = P.sb("aT", [32, NTOK], BF16)
        for tb in range(5):
            t0, tn = TB[tb]
            bank = tb % 2
            fm_proj(bank, lambda k: win[:, k, 3344:3376], 32, tb)
            A(lambda e, bank=bank, t0=t0, tn=tn: e.copy(aT[:, t0:t0 + tn], pb[bank][0:32, 0:tn]), [pb[bank]], [aT])
        wap = P.sb("wap", [32, 2, 2, 128], BF16)
        nba = P.sb("nba", [128, 2, 2], F32)
        G(lambda e: e.memset(wap[:, :, :, :], 0.0), [], [wap])
        G(lambda e: e.memset(nba[:, :, :], 0.0), [], [nba])
        for d in range(2):
            for cc in range(2):
                for hh in range(2):
                    h0 = (2 * cc + hh) * 32
                    P.dma("gpsimd", wap[16 * d:16 * d + 16, d, cc, 64 * hh:64 * hh + 32], I["gl_wa"][L, d, :, h0:h0 + 32])
                    P.dma("sync", nba[64 * hh:64 * hh + 32, d, cc:cc + 1], I["gl_ba"][L, d, h0:h0 + 32].rearrange("(p o) -> p o", o=1),
                          allow_slow_non_contiguous=True)
        V(lambda e: e.tensor_scalar_mul(nba[:, :, :], nba[:, :, :], -1.0), [nba], [nba])
        gls = [P.sb(f"gls{i}", [128, NTOK], F32) for i in range(2)]
        gi = 0
        for d in range(2):
            for cc in range(2):
                sg = gls[gi % 2]
                gi += 1
                for tb in range(5):
                    t0, tn = TB[tb]
                    bank = 2 + tb % 2
                    mm(pb[bank][:, 0:tn], wap[:, d, cc, :], aT[:, t0:t0 + tn], True, True)
                    A(lambda e, sg=sg, bank=bank, t0=t0, tn=tn, d=d, cc=cc: e.activation(
                        sg[:, t0:t0 + tn], pb[bank][:, 0:tn], AF.Exp, bias=nba[:, d, cc:cc + 1], scale=-1.0), [pb[bank], nba], [sg])
                A(lambda e, sg=sg: e.activation(sg[:, :], sg[:, :], AF.Ln, bias=one_col[:, :], scale=1.0), [sg, one_col], [sg])
                V(lambda e, sg=sg: e.tensor_scalar_mul(sg[:, :], sg[:, :], -1.0 / 16.0), [sg], [sg])
                P.dma("sync", S["gl_la"][d, cc], sg[:, :])
        P.barrier()
        P.release(m2)

        P.label = f"L{L}_P2e_tm"
        vst = [P.sb(f"vst{i}", [128, 4, 130], BF16) for i in range(4)]
        mvst = [P.sb(f"mvst{i}", [128, 4, 66], BF16) for i in range(4)]
        ost = [P.sb(f"ost{i}", [128, 256], F32) for i in range(4)]
        gvst = [P.sb(f"gvst{i}", [128, 256], BF16) for i in range(4)]
        rst = [P.sb(f"rst{i}", [128, 256], F32) for i in range(4)]
        for i in range(4):
            G(lambda e, i=i: e.memset(vst[i][:, :, :], 0.0), [], [vst[i]])
            G(lambda e, i=i: e.memset(vst[i][:, :, 128:129], 1.0), [], [vst[i]])
            G(lambda e, i=i: e.memset(mvst[i][:, :, :], 0.0), [], [mvst[i]])
            G(lambda e, i=i: e.memset(mvst[i][:, :, 64:65], 1.0), [], [mvst[i]])

        def tm_proj(bank, t, c0, ncol):
            for k in range(8):
                mm(pb[bank][:, 0:ncol], hT[:, k, t * 128:(t + 1) * 128], win[:, k, c0:c0 + ncol], k == 0, k == 7,
                   r=[win, ("hT", t)], w=[pb[bank]])

        for t in range(NT):
            b = t % 4
            ts_ = slice(t * 128, (t + 1) * 128)
            B0, B1, B2 = 3 * (t % 2), 3 * (t % 2) + 1, 3 * (t % 2) + 2
            tm_proj(B0, t, 1024, 512)
            V(lambda e, b=b, B0=B0: e.tensor_copy(vst[b][:, :, 0:128], pb[B0][:, :].rearrange("p (h d) -> p h d", h=4)), [pb[B0]], [vst[b]])
            P.dma("sync", S["da_v"][ts_, :], vst[b][:, :, :].rearrange("p h d -> p (h d)"))
            tm_proj(B1, t, 2048, 512)
            V(lambda e, b=b, B1=B1: e.tensor_copy(mvst[b][:, :, 0:64], pb[B1][:, 0:256].rearrange("p (h d) -> p h d", h=4)), [pb[B1]], [mvst[b]])
            A(lambda e, b=b, B1=B1: e.activation(ost[b][:, :], pb[B1][:, 256:512], AF.Sigmoid), [pb[B1]], [ost[b]])
            P.dma("sync", S["ml_v"][ts_, :], mvst[b][:, :, :].rearrange("p h d -> p (h d)"))
            P.dma("sync", S["ml_o"][ts_, :], ost[b][:, :])
            tm_proj(B2, t, 2832, 512)
            V(lambda e, b=b, B2=B2: e.tensor_copy(gvst[b][:, :], pb[B2][:, 0:256]), [pb[B2]], [gvst[b]])
            A(lambda e, b=b, B2=B2: e.activation(rst[b][:, :], pb[B2][:, 256:512], AF.Silu), [pb[B2]], [rst[b]])
            P.dma("sync", S["gl_v"][ts_, :], gvst[b][:, :])
            P.dma("sync", S["gl_r"][ts_, :], rst[b][:, :])
        if stop_after == "P2":
            return None

        P.label = f"L{L}_P3"
        P.barrier()
        P.release(persist_mark)
        qk = P.sb("qk", [128, 4, NTOK], BF16)
        kz = P.sb("kz", [128, 2, 4, NTOK], BF16)
        vv = P.sb("vv", [128, NT, 520], BF16)
        for m in range(2):
            G(lambda e, m=m: e.memset(kz[64 * (1 - m):64 * (1 - m) + 64, m, :, :], 0.0), [], [kz])
        for ch in range(4):
            P.dma("sync", qk[:, ch, :], S["da_qk"][ch])
            for m in range(2):
                P.dma("sync", kz[64 * m:64 * m + 64, m, ch, :], S["da_qk"][4 + ch, 64 * m:64 * m + 64, :])
        for t in range(NT):
            P.dma("sync", vv[:, t, :], S["da_v"][t * 128:(t + 1) * 128, :])
        for ex_ in (range(NE) if L == 0 else []):
            P.dma("sync", S["xslots"][ex_ * CAP:(ex_ + 1) * CAP, :].rearrange("(s p) d -> p s d", p=128), zt[:, :, :],
                  writes=[("xslots", t_, k__) for t_ in range(NT) for k__ in range(2)])
        gda = P.sb("gda", [128, 128], F32)
        P.dma("sync", gda[:, :], I["da_norm"][L:L + 1, :].broadcast_to([128, 128]))
        V(lambda e: e.tensor_scalar_mul(gda[:, :], gda[:, :], 1.0 - lam_init), [gda], [gda])
        Eb = [P.sb(f"Eb{i}", [128, 512], BF16) for i in range(3)]
        osb = [P.sb(f"osb{i}", [128, 128], F32) for i in range(2)]
        o2 = [P.sb(f"o2{i}", [128, 128], F32) for i in range(2)]
        sq = [P.sb(f"sq{i}", [128, 128], F32) for i in range(2)]
        rc = [P.sb(f"rc{i}", [128, 4], F32) for i in range(2)]
        oall = P.sb("oall", [128, NT, 4, 128], BF16)
        vvh = vv[:, :, :].rearrange("p t (h d) -> p t h d", h=4)
        qblocks = [(0, 256, [0, 1])] + [(256 + 512 * i, 512, list(range(NT))) for i in range(4)]
        nonlocal_ei = [0]
        oi = 0
        rnd = 0
        for h in range(4):
            for (q0, qn, kts) in qblocks:
                nqs = qn // 128
                ob = 2 + 3 * (rnd % 2)
                rnd += 1
                touched = set()
                seq = [(m, kt, qs) for m in range(2) for kt in kts for qs in range(nqs)]
                lastt = {}
                for (m, kt, qs) in seq:
                    lastt[(qs * 2 + m) // 3] = (m, kt, qs)
                steps = [(m, kt) for m in range(2) for kt in kts]

                def issue_scores(i):
                    m, kt = steps[i]
                    sbank = i % 2
                    mm(pb[sbank][:, 0:qn], kz[:, m, h, kt * 128:(kt + 1) * 128], qk[:, h, q0:q0 + qn], True, True,
                       r=[kz, qk], w=[pb[sbank]])
                    nonlocal_ei[0] += 1
                    E = Eb[nonlocal_ei[0] % 3]
                    A(lambda e, E=E, sbank=sbank, qn=qn: e.activation(E[:, 0:qn], pb[sbank][:, 0:qn], AF.Exp, scale=0.125),
                      [pb[sbank]], [E])
                    return E

                Es = {0: issue_scores(0)}
                for i, (m, kt) in enumerate(steps):
                    if i + 1 < len(steps):
                        Es[i + 1] = issue_scores(i + 1)
                    E = Es.pop(i)
                    for qs in range(nqs):
                        a = qs * 2 + m
                        bank = ob + a // 3
                        c0 = 130 * (a % 3)
                        st_ = bank not in touched
                        touched.add(bank)
                        sp_ = lastt[a // 3] == (m, kt, qs)
                        mm(pb[bank][:, c0:c0 + 129], E[:, qs * 128:(qs + 1) * 128], vvh[:, kt, h, 0:129], st_, sp_,
                           r=[E, vv], w=[pb[bank]])
                for qs in range(nqs):
                    j = oi % 2
                    oi += 1
                    a0, a1 = qs * 2, qs * 2 + 1
                    b0, c0 = ob + a0 // 3, 130 * (a0 % 3)
                    b1, c1 = ob + a1 // 3, 130 * (a1 % 3)
                    tq = (q0 + qs * 128) // 128
                    V(lambda e, j=j, b0=b0, c0=c0: e.reciprocal(rc[j][:, 0:1], pb[b0][:, c0 + 128:c0 + 129]), [pb[b0]], [rc[j]])
                    V(lambda e, j=j, b1=b1, c1=c1: e.reciprocal(rc[j][:, 1:2], pb[b1][:, c1 + 128:c1 + 129]), [pb[b1]], [rc[j]])
                    V(lambda e, j=j: e.tensor_tensor(rc[j][:, 1:2], rc[j][:, 1:2], nlam[:, :], ALU.mult), [rc[j], nlam], [rc[j]])
                    V(lambda e, j=j, b0=b0, c0=c0: e.tensor_scalar(osb[j][:, :], pb[b0][:, c0:c0 + 128], rc[j][:, 0:1], None, ALU.mult),
                      [pb[b0], rc[j]], [osb[j]])
                    V(lambda e, j=j, b1=b1, c1=c1: e.scalar_tensor_tensor(o2[j][:, :], pb[b1][:, c1:c1 + 128], rc[j][:, 1:2],
                                                                         osb[j][:, :], ALU.mult, ALU.add),
                      [pb[b1], rc[j], osb[j]], [o2[j]])
                    G(lambda e, j=j: e.tensor_tensor(sq[j][:, :], o2[j][:, :], o2[j][:, :], ALU.mult), [o2[j]], [sq[j]])
                    V(lambda e, j=j: e.tensor_reduce(rc[j][:, 2:3], sq[j][:, :], AX.X, ALU.add), [sq[j]], [rc[j]])
                    A(lambda e, j=j: e.activation(rc[j][:, 2:3], rc[j][:, 2:3], AF.Sqrt, bias=eps_col[:, :], scale=1.0 / 128.0),
                      [rc[j], eps_col], [rc[j]])
                    V(lambda e, j=j: e.reciprocal(rc[j][:, 3:4], rc[j][:, 2:3]), [rc[j]], [rc[j]])
                    V(lambda e, j=j, tq=tq, h=h: e.scalar_tensor_tensor(oall[:, tq, h, :], o2[j][:, :], rc[j][:, 3:4], gda[:, :], ALU.mult, ALU.mult),
                      [o2[j], rc[j], gda], [("oall", tq, h)])
        mixst = [P.sb(f"mixst{i}", [128, NTOK], BF16) for i in range(2)]
        ti = 0
        for h in range(4):
            mst = mixst[h % 2]
            for t0_ in range(0, NT, 4):
                nt_ = min(4, NT - t0_)
                bank = ti % 2
                ti += 1
                pT = pb[bank][:, :].bitcast(BF16)
                for k_ in range(nt_):
                    tr(pT[:, k_ * 128:(k_ + 1) * 128], oall[:, t0_ + k_, h, :], ident_bf[:, :], [("oall", t0_ + k_, h), ident_bf], [pb[bank]])
                if bank == 0:
                    A(lambda e, pT=pT, mst=mst, t0_=t0_, nt_=nt_: e.copy(mst[:, t0_ * 128:(t0_ + nt_) * 128], pT[:, 0:nt_ * 128]), [pb[bank]], [mst])
                else:
                    V(lambda e, pT=pT, mst=mst, t0_=t0_, nt_=nt_: e.tensor_copy(mst[:, t0_ * 128:(t0_ + nt_) * 128], pT[:, 0:nt_ * 128]), [pb[bank]], [mst])
            P.dma("sync", S["mixT"][h], mst[:, :])
        if stop_after == "P3":
            return None

        P.label = f"L{L}_P4/P5"
        def decay_attn(kind):
            P.barrier()
            P.release(persist_mark)
            ml = (kind == "ml")
            P.label = f"L{L}_P45_{kind}"
            ncc = 2
            Hc = 2
            dk = 64
            dva = 65 if ml else 64
            vstride = 66 if ml else 64
            qscale = (64 if ml else 32) ** -0.5
            qT = P.sb("qT", [128, ncc, NTOK], BF16)
            kT = P.sb("kT", [128, ncc, NTOK], BF16)
            for cc in range(ncc):
                P.dma("sync", qT[:, cc, :], S["ml_qk" if ml else "gl_qk"][cc])
                P.dma("sync", kT[:, cc, :], S["ml_qk" if ml else "gl_qk"][2 + cc])
            Vt = P.sb("Vt", [128, NT, 4 * vstride], BF16)
            for t in range(NT):
                P.dma("sync", Vt[:, t, :], S["ml_v" if ml else "gl_v"][t * 128:(t + 1) * 128, :])
            Vh = Vt[:, :, :].rearrange("p t (h d) -> p t h d", h=4)
            Hsum = P.sb("Hsum", [128, NT, 256], F32)
            Hs4 = Hsum[:, :, :].rearrange("p t (h d) -> p t h d", h=4)
            Hsum2 = P.sb("Hsum2", [128, NT, 256], F32)
            Hb4 = Hsum2[:, :, :].rearrange("p t (h d) -> p t h d", h=4)
            chains = [(d, cc) for d in range(2) for cc in range(ncc)]
            laC = {c: P.sb(f"la{c[0]}{c[1]}", [128, NTOK], F32) for c in chains}
            igC = {c: (P.sb(f"ig{c[0]}{c[1]}", [128, NTOK], F32) if ml else None) for c in chains}
            SstC = {c: P.sb(f"Sst{c[0]}{c[1]}", [128, dva], F32) for c in chains}
            SbfC = {c: P.sb(f"Sbf{c[0]}{c[1]}", [128, dva], BF16) for c in chains}
            NB = 4
            bT = [P.sb(f"bT{i}", [128, 128], F32) for i in range(NB)]
            pfx = [P.sb(f"pfx{i}", [128, 128], F32) for i in range(NB)]
            arg = [P.sb(f"arg{i}", [128, 128], F32) for i in range(NB)]
            eq = [P.sb(f"eq{i}", [128, 128], F32) for i in range(NB)]
            ek = [P.sb(f"ek{i}", [128, 128], F32) for i in range(NB)]
            ekh = [P.sb(f"ekh{i}", [128, 128], F32) for i in range(NB)]
            gam = [P.sb(f"gam{i}", [128, 1], F32) for i in range(NB)]
            qt_ = [P.sb(f"qt_{i}", [128, 128], BF16) for i in range(NB)]
            kt_ = [P.sb(f"kt_{i}", [128, 128], BF16) for i in range(NB)]
            kh_ = [P.sb(f"kh_{i}", [128, 128], BF16) for i in range(NB)]
            khT = [P.sb(f"khT{i}", [128, 128], BF16) for i in range(NB)]
            PT = [P.sb(f"PT{i}", [128, Hc, 128], BF16) for i in range(NB)]
            den = [P.sb(f"den{i}", [128, Hc, 1], F32) for i in range(NB)]
            for c in chains:
                P.dma("sync", laC[c][:, :], S["ml_la" if ml else "gl_la"][c[0], c[1]])
                if ml:
                    P.dma("sync", igC[c][:, :], S["ml_ig"][c[0], c[1]])
                V(lambda e, c=c: e.memset(SstC[c][:, :], 0.0), [], [SstC[c]])
                V(lambda e, c=c: e.memset(SbfC[c][:, :], 0.0), [], [SbfC[c]])
            orders = {0: list(range(NT)), 1: [1, 0] + list(range(NT - 1, 1, -1))}
            it = 0
            qz = [[P.sb(f"qz{i}_{hh}", [128, 128], BF16) for hh in range(Hc)] for i in range(NB)]
            for i in range(NB):
                for hh in range(Hc):
                    G(lambda e, i=i, hh=hh: e.memset(qz[i][hh][:, :], 0.0), [], [qz[i][hh]])
            for step in range(NT):
                ctxs = []
                for ci, (d, cc) in enumerate(chains):
                    la, ig = laC[(d, cc)], igC[(d, cc)]
                    t = orders[d][step]
                    j = ci
                    tsl = slice(t * 128, (t + 1) * 128)
                    if d == 0:
                        V(lambda e, j=j, tsl=tsl, la=la: e.tensor_tensor_scan(bT[j][:, :], ones_f[:, :], la[:, tsl], 0.0, ALU.mult, ALU.add),
                          [ones_f, la], [bT[j]])
                        tot = bT[j][:, 127:128]
                    else:
                        V(lambda e, j=j, tsl=tsl, la=la: e.tensor_tensor_scan(pfx[j][:, :], ones_f[:, :], la[:, tsl], 0.0, ALU.mult, ALU.add),
                          [ones_f, la], [pfx[j]])
                        V(lambda e, j=j, tsl=tsl, la=la: e.tensor_tensor(bT[j][:, :], la[:, tsl], pfx[j][:, :], ALU.subtract),
                          [la, pfx[j]], [bT[j]])
                        V(lambda e, j=j: e.tensor_scalar(bT[j][:, :], bT[j][:, :], pfx[j][:, 127:128], None, ALU.add),
                          [bT[j], pfx[j]], [bT[j]])
                        tot = bT[j][:, 0:1]
                    A(lambda e, j=j: e.activation(eq[j][:, :], bT[j][:, :], AF.Exp), [bT[j]], [eq[j]])
                    for hh in range(Hc):
                        V(lambda e, j=j, cc=cc, tsl=tsl, hh=hh: e.scalar_tensor_tensor(
                            qz[j][hh][64 * hh:64 * hh + 64, :], qT[64 * hh:64 * hh + 64, cc, tsl], qscale, eq[j][64 * hh:64 * hh + 64, :],
                            ALU.mult, ALU.mult), [qT, eq[j]], [qz[j][hh]])
                    if ml:
                        G(lambda e, j=j, tsl=tsl, ig=ig: e.tensor_tensor(arg[j][:, :], ig[:, tsl], bT[j][:, :], ALU.subtract), [ig, bT[j]], [arg[j]])
                        A(lambda e, j=j: e.activation(ek[j][:, :], arg[j][:, :], AF.Exp), [arg[j]], [ek[j]])
                        A(lambda e, j=j, tot=tot: e.activation(ekh[j][:, :], arg[j][:, :], AF.Exp, bias=tot, scale=1.0), [arg[j], bT[j]], [ekh[j]])
                    else:
                        A(lambda e, j=j: e.activation(ek[j][:, :], bT[j][:, :], AF.Exp, scale=-1.0), [bT[j]], [ek[j]])
                        A(lambda e, j=j, tot=tot: e.activation(ekh[j][:, :], bT[j][:, :], AF.Exp, bias=tot, scale=-1.0), [bT[j]], [ekh[j]])
                    A(lambda e, j=j, tot=tot: e.activation(gam[j][:, :], tot, AF.Exp), [bT[j]], [gam[j]])
                    G(lambda e, j=j, cc=cc, tsl=tsl: e.tensor_tensor(kt_[j][:, :], kT[:, cc, tsl], ek[j][:, :], ALU.mult), [kT, ek[j]], [kt_[j]])
                    G(lambda e, j=j, cc=cc, tsl=tsl: e.tensor_tensor(kh_[j][:, :], kT[:, cc, tsl], ekh[j][:, :], ALU.mult), [kT, ekh[j]], [kh_[j]])
                    pTk = pb[7][:, :].bitcast(BF16)
                    tr(pTk[:, 0:128], kh_[j][:, :], ident_bf[:, :], [kh_[j], ident_bf], [pb[7]])
                    A(lambda e, j=j, pTk=pTk: e.copy(khT[j][:, :], pTk[:, 0:128]), [pb[7]], [khT[j]])
                    for hh in range(Hc):
                        mm(pb[ci][:, hh * 128:(hh + 1) * 128], kt_[j][:, :], qz[j][hh][:, :], hh == 0, hh == Hc - 1,
                           r=[kt_[j], qz[j][hh]], w=[pb[ci]])
                    ctxs.append((d, cc, t, j, ci))
                for (d, cc, t, j, ci) in ctxs:
                    tri = tri_f if d == 0 else tri_b
                    Sbf = SbfC[(d, cc)]
                    Sst = SstC[(d, cc)]
                    hb = 4 + ci % 2
                    V(lambda e, j=j, tri=tri, ci=ci: e.tensor_tensor(
                        PT[j][:, :, :], pb[ci][:, 0:Hc * 128].rearrange("p (h l) -> p h l", h=Hc),
                        tri[:, :].unsqueeze(1).broadcast_to([128, Hc, 128]), ALU.mult), [pb[ci], tri], [PT[j]])
                    for hh in range(Hc):
                        head = Hc * cc + hh
                        hc0 = hh * 128
                        mm(pb[hb][:, hc0:hc0 + dva], PT[j][:, hh, :], Vh[:, t, head, 0:dva], hh == 0, False, r=[PT[j], Vt], w=[pb[hb]])
                        mm(pb[hb][:, hc0:hc0 + dva], qz[j][hh][:, :], Sbf[:, :], False, hh == Hc - 1, r=[qz[j][hh], Sbf], w=[pb[hb]])
                    Hx4 = Hs4 if d == 0 else Hb4
                    hkey = ("Hsum", t) if d == 0 else ("Hsum2", t)
                    if ml:
                        A(lambda e, j=j, hb=hb: e.activation(den[j][:, :, :], pb[hb][:, 0:Hc * 128].rearrange("p (h c) -> p h c", h=Hc)[:, :, 64:65],
                                                             AF.Abs), [pb[hb]], [den[j]])
                        V(lambda e, j=j: e.tensor_scalar_max(den[j][:, :, :], den[j][:, :, :], 1.0), [den[j]], [den[j]])
                        V(lambda e, j=j: e.reciprocal(den[j][:, :, :], den[j][:, :, :]), [den[j]], [den[j]])
                    for hh in range(Hc):
                        head = Hc * cc + hh
                        hc0 = hh * 128
                        if ml:
                            A(lambda e, j=j, hh=hh, hb=hb, hc0=hc0, head=head, t=t, Hx4=Hx4: e.activation(
                                Hx4[:, t, head, :], pb[hb][:, hc0:hc0 + 64], AF.Identity, scale=den[j][:, hh, :]), [pb[hb], den[j]], [hkey])
                        else:
                            A(lambda e, t=t, head=head, hb=hb, hc0=hc0, Hx4=Hx4: e.copy(Hx4[:, t, head, :], pb[hb][:, hc0:hc0 + 64]), [pb[hb]], [hkey])
                    for hh in range(Hc):
                        head = Hc * cc + hh
                        mm(pb[6][dk * hh:dk * (hh + 1), 0:dva], khT[j][:, dk * hh:dk * (hh + 1)], Vh[:, t, head, 0:dva], True, True,
                           r=[khT[j], Vt], w=[pb[6]])
                    V(lambda e, j=j, Sst=Sst: e.scalar_tensor_tensor(Sst[:, :], Sst[:, :], gam[j][:, :], pb[6][:, 0:dva], ALU.mult, ALU.add),
                      [Sst, gam[j], pb[6]], [Sst])
                    A(lambda e, Sst=Sst, Sbf=Sbf: e.copy(Sbf[:, :], Sst[:, :]), [Sst], [Sbf])
            P.label = f"L{L}_P45_{kind}_fin"
            gnm = P.sb("gnm", [128, 64], F32)
            P.dma("sync", gnm[:, :], I["ml_norm" if ml else "gl_norm"][L:L + 1, :].broadcast_to([128, 64]))
            gate = [P.sb(f"gate{i}", [128, 256], F32) for i in range(2)]
            sqh = [P.sb(f"sqh{i}", [128, 256], F32) for i in range(2)]
            ssq = [P.sb(f"ssq{i}", [128, 4], F32) for i in range(2)]
            obm = [P.sb(f"obm{i}", [128, 256], BF16) for i in range(2)]
            mst = [P.sb(f"mstm{i}", [128, NTOK], BF16) for i in range(2)]
            for t in range(NT):
                j = t % 2
                P.dma("sync", gate[j][:, :], S["ml_o" if ml else "gl_r"][t * 128:(t + 1) * 128, :])
                G(lambda e, j=j: e.tensor_tensor(gate[j][:, :].rearrange("p (h d) -> p h d", h=4),
                                                 gate[j][:, :].rearrange("p (h d) -> p h d", h=4),
                                                 gnm[:, :].unsqueeze(1).broadcast_to([128, 4, 64]), ALU.mult), [gate[j], gnm], [gate[j]])
                G(lambda e, t=t: e.tensor_tensor(Hsum[:, t, :], Hsum[:, t, :], Hsum2[:, t, :], ALU.add), [("Hsum", t), ("Hsum2", t)], [("Hsum", t)])
                V(lambda e, j=j, t=t: e.tensor_tensor(sqh[j][:, :], Hsum[:, t, :], Hsum[:, t, :], ALU.mult), [("Hsum", t)], [sqh[j]])
                V(lambda e, j=j: e.tensor_reduce(ssq[j][:, :], sqh[j][:, :].rearrange("p (h d) -> p h d", h=4), AX.X, ALU.add), [sqh[j]], [ssq[j]])
                A(lambda e, j=j: e.activation(ssq[j][:, :], ssq[j][:, :], AF.Sqrt, bias=eps_col[:, :], scale=1.0 / 64.0), [ssq[j], eps_col], [ssq[j]])
                V(lambda e, j=j: e.reciprocal(ssq[j][:, :], ssq[j][:, :]), [ssq[j]], [ssq[j]])
                for hh in range(4):
                    V(lambda e, j=j, t=t, hh=hh: e.scalar_tensor_tensor(obm[j][:, hh * 64:(hh + 1) * 64], Hs4[:, t, hh, :], ssq[j][:, hh:hh + 1],
                                                                        gate[j][:, hh * 64:(hh + 1) * 64], ALU.mult, ALU.mult),
                      [("Hsum", t), ssq[j], gate[j]], [obm[j]])
                pT = pb[5][:, :].bitcast(BF16)
                for c2_ in range(2):
                    tr(pT[:, c2_ * 128:(c2_ + 1) * 128], obm[j][:, c2_ * 128:(c2_ + 1) * 128], ident_bf[:, :], [obm[j], ident_bf], [pb[5]])
                    A(lambda e, c2_=c2_, t=t, pT=pT: e.copy(mst[c2_][:, t * 128:(t + 1) * 128], pT[:, c2_ * 128:(c2_ + 1) * 128]), [pb[5]], [mst[c2_]])
            base = 4 if ml else 6
            for c2_ in range(2):
                P.dma("sync", S["mixT"][base + c2_], mst[c2_][:, :])

        decay_attn("ml")
        if stop_after == "P4":
            return None
        decay_attn("gl")
        if stop_after == "P5":
            return None

        P.label = f"L{L}_P6"
        P.barrier()
        P.release(persist_mark)
        m6 = P.mark()
        mixT = P.sb("mixTs", [128, 8, NTOK], BF16)
        for ch in range(8):
            P.dma("sync", mixT[:, ch, :], S["mixT"][ch])
        wo = P.sb("wo", [128, 8, D], BF16)
        P.dma("gpsimd", wo[:, :, :], I["w_out"][L].rearrange("(k p) n -> p k n", p=128))
        g1bc = P.sb("g1bc", [128, 2, D], F32)
        for r in range(2):
            P.dma("sync", g1bc[:, r, :], S["modrow"][r:r + 1, 2 * D:3 * D].broadcast_to([128, D]))
        lng6 = P.sb("lng", [128, D], F32)
        lnb6 = P.sb("lnb", [128, D], F32)
        P.dma("sync", lng6[:, :], I["ln_mix_g"][L:L + 1, :].broadcast_to([128, D]))
        P.dma("sync", lnb6[:, :], I["ln_mix_b"][L:L + 1, :].broadcast_to([128, D]))
        s2bc = P.sb("s2bc", [128, 2, 2, D], F32)
        for r in range(2):
            P.dma("sync", s2bc[:, 0, r, :], S["modrow"][r:r + 1, 3 * D:4 * D].broadcast_to([128, D]))
            P.dma("sync", s2bc[:, 1, r, :], S["modrow"][r:r + 1, 4 * D:5 * D].broadcast_to([128, D]))
        V(lambda e: e.tensor_scalar_add(s2bc[:, 1, :, :], s2bc[:, 1, :, :], 1.0), [s2bc], [s2bc])
        V(lambda e: e.memset(cntE[:, :], 0.0), [], [cntE])
        fTM = [P.sb(f"fTM{i}", [128, D], BF16) for i in range(3)]
        ftmp = [P.sb(f"ftmp{i}", [128, D], F32) for i in range(3)]
        wr = P.sb("wr", [128, 8, 36], F32)
        P.dma("sync", wr[:, :, 0:4], I["moe_wg"][L].rearrange("(k p) n -> p k n", p=128))
        P.dma("sync", wr[:, :, 4:36], I["moe_we"][L].rearrange("(k p) n -> p k n", p=128))
        xt6v = [P.sb(f"xt6{i}", [128, D], F32) for i in range(3)]
        u6v = [P.sb(f"u6{i}", [128, D], F32) for i in range(3)]
        xn6v = [P.sb(f"xn6{i}", [128, D], F32) for i in range(3)]
        fTf = [P.sb(f"fTf{i}", [128, 8, 128], F32) for i in range(3)]
        stb6 = [(P.sb(f"st6{i}", [128, 2, 6], F32), P.sb(f"mv6{i}", [128, 2], F32), P.sb(f"rs6{i}", [128, 1], F32)) for i in range(3)]
        stc = [(P.sb(f"st7{i}", [128, 2, 6], F32), P.sb(f"mv7{i}", [128, 2], F32), P.sb(f"rs7{i}", [128, 1], F32)) for i in range(3)]
        rt2 = [dict(oh1=P.sb(f"oh1{i}", [128, 32], F32), oh2=P.sb(f"oh2{i}", [128, 32], F32), slot=P.sb(f"slot{i}", [128, 32], F32),
                    dst=P.sb(f"dst{i}", [128, 32], F32), ovm=P.sb(f"ovm{i}", [128, 32], F32), tm=P.sb(f"tm{i}", [128, 32], F32),
                    wvv=P.sb(f"wv{i}", [128, 32], F32), c4=P.sb(f"c4{i}", [128, 4], F32)) for i in range(3)]
        rt = [dict(lg=P.sb(f"lg{i}", [128, 36], F32), s1=P.sb(f"s1{i}", [128, 8], F32), oh=P.sb(f"oh{i}", [128, 4], F32),
                   ml_=P.sb(f"mlg{i}", [128, 32], F32), t32=P.sb(f"t32{i}", [128, 32], F32), ex=P.sb(f"ex{i}", [128, 32], F32),
                   e4=P.sb(f"e4{i}", [128, 4], F32)) for i in range(3)]
        def stA1(t):
            b = t % 3
            r = 1 if t < NCTX_T else 0
            tsl = slice(t * 128, (t + 1) * 128)
            P.dma("sync", xt6v[b][:, :], x_cur[tsl, :])
            for half in range(2):
                for k in range(8):
                    mm(pb[half][:, :], mixT[:, k, tsl], wo[:, k, half * 512:(half + 1) * 512], k == 0, k == 7, r=[mixT, wo], w=[pb[half]])
                V(lambda e, b=b, half=half, r=r: e.tensor_tensor(u6v[b][:, half * 512:(half + 1) * 512], pb[half][:, :],
                                                                  g1bc[:, r, half * 512:(half + 1) * 512], ALU.mult), [pb[half], g1bc], [u6v[b]])
                if debug:
                    pass
            V(lambda e, b=b: e.scalar_tensor_tensor(u6v[b][:, :], xt6v[b][:, :], DN_ALPHA, u6v[b][:, :], ALU.mult, ALU.add), [xt6v[b], u6v[b]], [u6v[b]])
            mean, rstd = ln_stats(u6v[b], stb6[b])
            V(lambda e, b=b, mean=mean, rstd=rstd: e.tensor_scalar(u6v[b][:, :], u6v[b][:, :], mean, rstd, ALU.subtract, ALU.mult),
              [u6v[b], stb6[b][1], stb6[b][2]], [u6v[b]])
            G(lambda e, b=b: e.tensor_tensor(u6v[b][:, :], u6v[b][:, :], lng6[:, :], ALU.mult), [u6v[b], lng6], [u6v[b]])
            G(lambda e, b=b: e.tensor_tensor(u6v[b][:, :], u6v[b][:, :], lnb6[:, :], ALU.add), [u6v[b], lnb6], [u6v[b]])
            P.dma("sync", S["x1"][tsl, :], u6v[b][:, :])

        def stA2(t):
            b = t % 3
            r = 1 if t < NCTX_T else 0
            tsl = slice(t * 128, (t + 1) * 128)
            mean2, rstd2 = ln_stats(u6v[b], stc[b])
            V(lambda e, b=b, mean2=mean2, rstd2=rstd2: e.tensor_scalar(xn6v[b][:, :], u6v[b][:, :], mean2, rstd2, ALU.subtract, ALU.mult),
              [u6v[b], stc[b][1], stc[b][2]], [xn6v[b]])
            for ch in range(8):
                bank = 2 + ch // 4
                tr(pb[bank][:, (ch % 4) * 128:(ch % 4 + 1) * 128], xn6v[b][:, ch * 128:(ch + 1) * 128], ident_f[:, :], [xn6v[b], ident_f], [pb[bank]])
            for ch in range(8):
                bank = 2 + ch // 4
                i_ = pb[bank][:, (ch % 4) * 128:(ch % 4 + 1) * 128]
                A(lambda e, b=b, ch=ch, i_=i_, r=r: e.activation(fTf[b][:, ch, :], i_, AF.Identity, bias=modcol[:, 24 + ch, r:r + 1],
                                                                 scale=modcol[:, 32 + ch, r:r + 1]), [pb[bank], modcol], [fTf[b]])
            G(lambda e, b=b, r=r: e.tensor_tensor(ftmp[b][:, :], xn6v[b][:, :], s2bc[:, 1, r, :], ALU.mult), [xn6v[b], s2bc], [ftmp[b]])
            G(lambda e, b=b, r=r: e.tensor_tensor(fTM[b][:, :], ftmp[b][:, :], s2bc[:, 0, r, :], ALU.add), [ftmp[b], s2bc], [fTM[b]])

        def stB(t):
            b = t % 3
            r = 1 if t < NCTX_T else 0
            tsl = slice(t * 128, (t + 1) * 128)
            for k in range(8):
                mm(pb[4][:, 0:36], fTf[b][:, k, :], wr[:, k, :], k == 0, k == 7, r=[fTf[b], wr], w=[pb[4]])
            R_ = rt[b]
            lg, s1, oh, mlg, t32, ex, e4 = R_["lg"], R_["s1"], R_["oh"], R_["ml_"], R_["t32"], R_["ex"], R_["e4"]
            V(lambda e, lg=lg: e.tensor_copy(lg[:, :], pb[4][:, 0:36]), [pb[4]], [lg])
            V(lambda e, lg=lg, s1=s1: e.tensor_reduce(s1[:, 0:1], lg[:, 0:4], AX.X, ALU.max), [lg], [s1])
            V(lambda e, s1=s1: e.tensor_scalar_mul(s1[:, 1:2], s1[:, 0:1], -1.0), [s1], [s1])
            A(lambda e, lg=lg, s1=s1, e4=e4: e.activation(e4[:, :], lg[:, 0:4], AF.Exp, bias=s1[:, 1:2], scale=1.0), [lg, s1], [e4])
            V(lambda e, s1=s1, e4=e4: e.tensor_reduce(s1[:, 2:3], e4[:, :], AX.X, ALU.add), [e4], [s1])
            V(lambda e, s1=s1: e.reciprocal(s1[:, 2:3], s1[:, 2:3]), [s1], [s1])
            V(lambda e, lg=lg, s1=s1, oh=oh: e.tensor_scalar(oh[:, :], lg[:, 0:4], s1[:, 0:1], None, ALU.is_ge), [lg, s1], [oh])
            V(lambda e, oh=oh: e.tensor_scalar(oh[:, :], oh[:, :], -1.0, 1e30, ALU.add, ALU.mult), [oh], [oh])
            for g in range(4):
                V(lambda e, g=g, lg=lg, oh=oh, mlg=mlg: e.tensor_scalar(mlg[:, g * 8:(g + 1) * 8], lg[:, 4 + g * 8:4 + (g + 1) * 8],
                                                                       oh[:, g:g + 1], None, ALU.add), [lg, oh], [mlg])
            V(lambda e, mlg=mlg, s1=s1: e.tensor_reduce(s1[:, 3:4], mlg[:, :], AX.X, ALU.max), [mlg], [s1])
            V(lambda e, mlg=mlg, s1=s1, t32=t32: e.tensor_scalar(t32[:, :], mlg[:, :], s1[:, 3:4], -1e30, ALU.is_ge, ALU.mult), [mlg, s1], [t32])
            V(lambda e, mlg=mlg, t32=t32: e.tensor_tensor(t32[:, :], t32[:, :], mlg[:, :], ALU.add), [mlg, t32], [t32])
            V(lambda e, t32=t32, s1=s1: e.tensor_reduce(s1[:, 4:5], t32[:, :], AX.X, ALU.max), [t32], [s1])
            V(lambda e, mlg=mlg, s1=s1, t32=t32: e.tensor_scalar(t32[:, :], mlg[:, :], s1[:, 4:5], None, ALU.is_ge), [mlg, s1], [t32])
            V(lambda e, s1=s1: e.tensor_scalar_mul(s1[:, 5:6], s1[:, 3:4], -1.0), [s1], [s1])
            A(lambda e, mlg=mlg, s1=s1, ex=ex: e.activation(ex[:, :], mlg[:, :], AF.Exp, bias=s1[:, 5:6], scale=1.0), [mlg, s1], [ex])
            V(lambda e, ex=ex, t32=t32: e.tensor_tensor(ex[:, :], ex[:, :], t32[:, :], ALU.mult), [ex, t32], [ex])
            V(lambda e, ex=ex, s1=s1: e.tensor_reduce(s1[:, 6:7], ex[:, :], AX.X, ALU.add), [ex], [s1])
            V(lambda e, s1=s1: e.reciprocal(s1[:, 6:7], s1[:, 6:7]), [s1], [s1])
            V(lambda e, s1=s1: e.tensor_tensor(s1[:, 6:7], s1[:, 6:7], s1[:, 2:3], ALU.mult), [s1], [s1])
            V(lambda e, ex=ex, s1=s1, t=t: e.tensor_scalar(Wt[:, t, :], ex[:, :], s1[:, 6:7], None, ALU.mult), [ex, s1], [Wt])
            Q_ = rt2[b]
            oh1, oh2, slot, dst, ovm, tm, wvv, c4 = Q_["oh1"], Q_["oh2"], Q_["slot"], Q_["dst"], Q_["ovm"], Q_["tm"], Q_["wvv"], Q_["c4"]
            V(lambda e, mlg=mlg, s1=s1, oh1=oh1: e.tensor_scalar(oh1[:, :], mlg[:, :], s1[:, 3:4], None, ALU.is_ge), [mlg, s1], [oh1])
            V(lambda e, t32=t32, oh1=oh1, oh2=oh2: e.tensor_tensor(oh2[:, :], t32[:, :], oh1[:, :], ALU.subtract), [t32, oh1], [oh2])
            mm(pb[5][:, 0:32], tri_f[:, :], t32[:, :], True, True, r=[tri_f, t32], w=[pb[5]])
            mm(pb[6][:, 0:32], ones_f[:, :], t32[:, :], True, True, r=[ones_f, t32], w=[pb[6]])
            V(lambda e, slot=slot, t32=t32: e.tensor_tensor(slot[:, :], pb[5][:, 0:32], t32[:, :], ALU.subtract), [pb[5], t32], [slot])
            V(lambda e, slot=slot: e.tensor_tensor(slot[:, :], slot[:, :], cntE[:, :], ALU.add), [slot, cntE], [slot])
            V(lambda e: e.tensor_tensor(cntE[:, :], cntE[:, :], pb[6][:, 0:32], ALU.add), [cntE, pb[6]], [cntE])
            V(lambda e, slot=slot, ovm=ovm: e.tensor_scalar(ovm[:, :], slot[:, :], float(CAP), None, ALU.is_ge), [slot], [ovm])
            V(lambda e, slot=slot, dst=dst: e.tensor_tensor(dst[:, :], slot[:, :], ecolC[:, :], ALU.add), [slot, ecolC], [dst])
            V(lambda e, dst=dst, ovm=ovm: e.scalar_tensor_tensor(dst[:, :], ovm[:, :], 1.0e6, dst[:, :], ALU.mult, ALU.add), [ovm, dst], [dst])
            V(lambda e, t=t, ovm=ovm, wvv=wvv: e.tensor_tensor(wvv[:, :], Wt[:, t, :], ovm[:, :], ALU.mult), [Wt, ovm], [wvv])
            V(lambda e, t=t, wvv=wvv: e.tensor_tensor(wvv[:, :], Wt[:, t, :], wvv[:, :], ALU.subtract), [Wt, wvv], [wvv])
            for k_, oh in ((0, oh1), (1, oh2)):
                V(lambda e, oh=oh, dst=dst, tm=tm: e.tensor_tensor(tm[:, :], oh[:, :], dst[:, :], ALU.mult), [oh, dst], [tm])
                V(lambda e, tm=tm, c4=c4, k_=k_: e.tensor_reduce(c4[:, k_:k_ + 1], tm[:, :], AX.X, ALU.add), [tm], [c4])
                V(lambda e, oh=oh, wvv=wvv, tm=tm: e.tensor_tensor(tm[:, :], oh[:, :], wvv[:, :], ALU.mult), [oh, wvv], [tm])
                V(lambda e, tm=tm, t=t, k_=k_: e.tensor_reduce(wsel[:, t, k_:k_ + 1], tm[:, :], AX.X, ALU.add), [tm], [wsel])
            V(lambda e, c4=c4, t=t: e.tensor_copy(idxs[:, t, :], c4[:, 0:2]), [c4], [idxs])
            for k_ in range(2):
                P.ops.append(dict(eng="gpsimd", dma=True, bar=None, r=P._keys([fTM[b], idxs]), w=[("xslots", t, k_)],
                                  fn=lambda e, b=b, t=t, k_=k_: e.indirect_dma_start(
                                      out=S["xslots"][:, :], out_offset=bass.IndirectOffsetOnAxis(idxs[:, t, k_:k_ + 1], 0),
                                      in_=fTM[b][:, :], in_offset=None, bounds_check=get_bc(e), oob_is_err=False)))
        for i_ in range(NT + 2):
            if i_ < NT:
                stA1(i_)
            if 0 <= i_ - 1 < NT:
                stA2(i_ - 1)
            if 0 <= i_ - 2 < NT:
                stB(i_ - 2)
        if debug:
            for t in range(NT):
                P.dma("sync", S["dbg_W"][t * 128:(t + 1) * 128, :], Wt[:, t, :])
        if stop_after == "P6":
            return None

        P.label = f"L{L}_P7"
        P.barrier()
        P.release(m6)
        yacc = P.sb("yacc", [128, NT, D], F32)
        after_yacc = P.mark()
        w1b = [P.sb(f"w1b{i}", [128, 8, 512], BF16) for i in range(2)]
        w3b = [P.sb(f"w3b{i}", [128, 8, 512], BF16) for i in range(2)]
        w2b = [P.sb(f"w2b{i}", [128, 4, D], BF16) for i in range(2)]
        xs = [P.sb(f"xs{i}", [128, NST, D], BF16) for i in range(2)]
        xT = [P.sb(f"xTs{i}", [128, 8, CAP], BF16) for i in range(2)]
        gTb = [P.sb(f"gTb{i}", [128, 4, CAP], BF16) for i in range(2)]
        s1b = [P.sb(f"s1b{i}", [128, 512], F32) for i in range(2)]
        ysb = [P.sb(f"ysb{i}", [128, D], BF16) for i in range(2)]
        SB = [(0, CAP // 2), (CAP // 2, CAP // 2)] if CAP > 512 else [(0, CAP)]
        yi = 0

        def moe_load(ex_):
            eb = ex_ % 2
            P.dma("gpsimd", w1b[eb][:, :, :], I["moe_w1"][L, ex_].rearrange("(k p) n -> p k n", p=128))
            P.dma("gpsimd", w3b[eb][:, :, :], I["moe_w3"][L, ex_].rearrange("(k p) n -> p k n", p=128))
            P.dma("gpsimd", w2b[eb][:, :, :], I["moe_w2"][L, ex_].rearrange("(k p) n -> p k n", p=128))
            P.dma("sync", xs[eb][:, :, :], S["xslots"][ex_ * CAP:(ex_ + 1) * CAP, :].rearrange("(s p) d -> p s d", p=128),
                  reads=[("xslots", t_, k__) for t_ in range(NT) for k__ in range(2)])

        def moe_transposes(ex_):
            eb = ex_ % 2
            for st in range(NST):
                bank = 6 + st % 2
                pT = pb[bank][:, :].bitcast(BF16)
                for k in range(8):
                    tr(pT[:, k * 128:(k + 1) * 128], xs[eb][:, st, k * 128:(k + 1) * 128], ident_bf[:, :], [xs[eb], ident_bf], [pb[bank]])
                if st % 2 == 0:
                    A(lambda e, eb=eb, st=st, pT=pT: e.copy(xT[eb][:, :, st * 128:(st + 1) * 128], pT[:, :].rearrange("p (k s) -> p k s", k=8)),
                      [pb[bank]], [xT[eb]])
                else:
                    V(lambda e, eb=eb, st=st, pT=pT: e.tensor_copy(xT[eb][:, :, st * 128:(st + 1) * 128], pT[:, :].rearrange("p (k s) -> p k s", k=8)),
                      [pb[bank]], [xT[eb]])

        moe_load(0)
        moe_transposes(0)
        for ex_ in range(NE):
            eb = ex_ % 2
            if ex_ + 1 < NE:
                moe_load(ex_ + 1)
            gT = gTb[eb]
            for (t0, tn) in SB:
                for hc in range(4):
                    b1, b3 = (hc % 2) * 2, (hc % 2) * 2 + 1
                    for k in range(8):
                        mm(pb[b1][:, 0:tn], w1b[eb][:, k, hc * 128:(hc + 1) * 128], xT[eb][:, k, t0:t0 + tn], k == 0, k == 7,
                           r=[w1b[eb], xT[eb]], w=[pb[b1]])
                    for k in range(8):
                        mm(pb[b3][:, 0:tn], w3b[eb][:, k, hc * 128:(hc + 1) * 128], xT[eb][:, k, t0:t0 + tn], k == 0, k == 7,
                           r=[w3b[eb], xT[eb]], w=[pb[b3]])
                    sb_ = s1b[hc % 2]
                    A(lambda e, sb_=sb_, b1=b1, tn=tn: e.activation(sb_[:, 0:tn], pb[b1][:, 0:tn], AF.Silu), [pb[b1]], [sb_])
                    V(lambda e, sb_=sb_, b3=b3, tn=tn, gT=gT, hc=hc, t0=t0: e.tensor_tensor(gT[:, hc, t0:t0 + tn], sb_[:, 0:tn], pb[b3][:, 0:tn], ALU.mult),
                      [sb_, pb[b3]], [gT])
            if ex_ + 1 < NE:
                moe_transposes(ex_ + 1)
            for st in range(NST):
                yb = ysb[yi % 2]
                yi += 1
                for half in range(2):
                    bank = 4 + half
                    for hc in range(4):
                        mm(pb[bank][:, :], gT[:, hc, st * 128:(st + 1) * 128], w2b[eb][:, hc, half * 512:(half + 1) * 512], hc == 0, hc == 3,
                           r=[gT, w2b[eb]], w=[pb[bank]])
                    if half == 0:
                        A(lambda e, yb=yb, bank=bank: e.copy(yb[:, 0:512], pb[bank][:, :]), [pb[bank]], [yb])
                    else:
                        V(lambda e, yb=yb, bank=bank: e.tensor_copy(yb[:, 512:1024], pb[bank][:, :]), [pb[bank]], [yb])
                r0 = ex_ * CAP + st * 128
                P.dma("sync", S["yslots"][r0:r0 + 128, :], yb[:, :], writes=[("yslots", ex_, st)])
        P.label = f"L{L}_P7_combine"
        yg = [P.sb(f"yg{i}", [128, D], BF16) for i in range(4)]
        for i in range(4):
            V(lambda e, i=i: e.memset(yg[i][:, :], 0.0), [], [yg[i]])
        for t in range(NT):
            g0, g1_ = yg[(2 * t) % 4], yg[(2 * t + 1) % 4]
            for k_, gt in ((0, g0), (1, g1_)):
                P.ops.append(dict(eng="gpsimd", dma=True, bar=None, r=[("yslots", e_, s_) for e_ in range(NE) for s_ in range(NST)] + P._keys([idxs]), w=P._keys([gt]),
                                  fn=lambda e, t=t, k_=k_, gt=gt: e.indirect_dma_start(
                                      out=gt[:, :], out_offset=None, in_=S["yslots"][:, :],
                                      in_offset=bass.IndirectOffsetOnAxis(idxs[:, t, k_:k_ + 1], 0),
                                      bounds_check=get_bc(e), oob_is_err=False)))
            V(lambda e, t=t, g0=g0: e.tensor_scalar(yacc[:, t, :], g0[:, :], wsel[:, t, 0:1], None, ALU.mult), [g0, wsel], [("yacc", t)])
            V(lambda e, t=t, g1_=g1_: e.scalar_tensor_tensor(yacc[:, t, :], g1_[:, :], wsel[:, t, 1:2], yacc[:, t, :], ALU.mult, ALU.add),
              [g1_, wsel, ("yacc", t)], [("yacc", t)])
        if debug:
            for t in range(NT):
                P.dma("sync", S["dbg_moe"][t * 128:(t + 1) * 128, :], yacc[:, t, :], reads=[("yacc", t)])
        if stop_after == "P7":
            return None

        P.label = f"L{L}_P8"
        P.barrier()
        P.release(after_yacc)
        g2bc = P.sb("g2bc", [128, 2, D], F32)
        for r in range(2):
            P.dma("sync", g2bc[:, r, :], S["modrow"][r:r + 1, 5 * D:6 * D].broadcast_to([128, D]))
        lng8v = P.sb("lng8", [128, D], F32)
        lnb8v = P.sb("lnb8", [128, D], F32)
        P.dma("sync", lng8v[:, :], I["ln_ffn_g"][L:L + 1, :].broadcast_to([128, D]))
        P.dma("sync", lnb8v[:, :], I["ln_ffn_b"][L:L + 1, :].broadcast_to([128, D]))
        xt8v = [P.sb(f"xt8{i}", [128, D], F32) for i in range(4)]
        u8v = [P.sb(f"u8{i}", [128, D], F32) for i in range(4)]
        stb8 = [(P.sb(f"st8{i}", [128, 2, 6], F32), P.sb(f"mv8{i}", [128, 2], F32), P.sb(f"rs8{i}", [128, 1], F32)) for i in range(4)]
        tiles8 = [t for t in range(NT) if not (last and t < NCTX_T)]

        def p8_s1(t):
            b = t % 4
            r = 1 if t < NCTX_T else 0
            st, mv, rstd = stb8[b]
            xb, ub = xt8v[b], u8v[b]
            P.dma("sync", xb[:, :], S["x1"][t * 128:(t + 1) * 128, :])
            V(lambda e: e.tensor_tensor(ub[:, :], yacc[:, t, :], g2bc[:, r, :], ALU.mult), [("yacc", t), g2bc], [ub])
            V(lambda e: e.scalar_tensor_tensor(ub[:, :], xb[:, :], DN_ALPHA, ub[:, :], ALU.mult, ALU.add), [xb, ub], [ub])
            V(lambda e: e.bn_stats(st[:, 0, :], ub[:, 0:512]), [ub], [st])
            V(lambda e: e.bn_stats(st[:, 1, :], ub[:, 512:1024]), [ub], [st])
            V(lambda e: e.bn_aggr(mv[:, :], st[:, :, :].rearrange("p a b -> p (a b)")), [st], [mv])

        def p8_s2(t):
            b = t % 4
            st, mv, rstd = stb8[b]
            ub = u8v[b]
            A(lambda e: e.activation(rstd[:, :], mv[:, 1:2], AF.Sqrt, bias=eps_col[:, :], scale=1.0), [mv, eps_col], [rstd])
            V(lambda e: e.reciprocal(rstd[:, :], rstd[:, :]), [rstd], [rstd])
            V(lambda e: e.tensor_scalar(ub[:, :], ub[:, :], mv[:, 0:1], rstd[:, 0:1], ALU.subtract, ALU.mult), [ub, mv, rstd], [ub])

        def p8_s3(t):
            b = t % 4
            ub = u8v[b]
            G(lambda e: e.tensor_tensor(ub[:, :], ub[:, :], lng8v[:, :], ALU.mult), [ub, lng8v], [ub])
            G(lambda e: e.tensor_tensor(ub[:, :], ub[:, :], lnb8v[:, :], ALU.add), [ub, lnb8v], [ub])
            if last:
                P.dma("sync", out_d[(t - NCTX_T) * 128:(t - NCTX_T + 1) * 128, :], ub[:, :])
            else:
                P.dma("sync", S["xnext"][t * 128:(t + 1) * 128, :], ub[:, :])

        n8 = len(tiles8)
        for i_ in range(n8 + 2):
            if i_ < n8:
                p8_s1(tiles8[i_])
            if 0 <= i_ - 1 < n8:
                p8_s2(tiles8[i_ - 1])
            if 0 <= i_ - 2 < n8:
                p8_s3(tiles8[i_ - 2])
        return S["xnext"]

    x_cur = I["xin"]
    for L_ in range(n_layers):
        x_cur = layer(L_, x_cur)
        if x_cur is None:
            break

    stats = P.emit()
    global _LAST_VLABELS
    _LAST_VLABELS = P.vlabels
    return nc, stats, consts, list(S.keys())


_CACHE = {}


def make_in_maps(inputs, consts):
    x = np.asarray(inputs["x"], np.float32)
    ctx = np.asarray(inputs["ctx"], np.float32)
    c = np.asarray(inputs["c"], np.float32)
    c_ctx = np.asarray(inputs["c_ctx"], np.float32)
    shared = {k: np.ascontiguousarray(np.asarray(inputs[k], np.float32)) for k in IN_SHAPES if k not in ("xin", "c2")}
    shared.update(consts)
    maps = []
    for b in range(x.shape[0]):
        m = dict(shared)
        m["xin"] = np.ascontiguousarray(np.concatenate([ctx[b], x[b]], axis=0))
        m["c2"] = np.ascontiguousarray(np.stack([c[b], c_ctx], axis=1))
        maps.append(m)
    return maps


def kernel(**inputs):
    if "nc" not in _CACHE:
        nc, stats, consts, _ = build(debug=False)
        _CACHE["nc"] = nc
        _CACHE["consts"] = consts
    nc = _CACHE["nc"]
    maps = make_in_maps(inputs, _CACHE["consts"])
    res = run_bass_kernel_spmd(nc, maps, core_ids=list(range(8)))
    out = np.stack([np.asarray(r["out"], np.float32) for r in res.results], axis=0)
    return out
```

```python
import math
import contextlib
import numpy as np
import ml_dtypes
import concourse.bass as bass
import concourse.mybir as mybir
from concourse.bass_utils import run_bass_kernel_spmd

F32 = mybir.dt.float32
BF16 = mybir.dt.bfloat16
ALU = mybir.AluOpType
AF = mybir.ActivationFunctionType
AX = mybir.AxisListType

D = 1024
NTOK = 2304
NT = 18
NCTX_T = 2
DEPTH = 2
IN_W = 3376
LN_EPS = 1e-6
DN_ALPHA = (2 * DEPTH) ** 0.25
NE = 32
CAP = 640
NST = CAP // 128
U32 = mybir.dt.uint32
TB = [(0, 512), (512, 512), (1024, 512), (1536, 512), (2048, 256)]

COMPUTE = ("tensor", "vector", "scalar", "gpsimd")
ENGS = ("tensor", "vector", "scalar", "gpsimd", "sync")
NDMASEM = 8


class Prog:
    def __init__(self, nc):
        self.nc = nc
        self.ops = []
        self.sb_base = 16512
        self.sb_top = 16512
        self.sb_limit = 229376 - 64
        self.uid = 0
        self.psum_names = set()
        self.label = ""

    def mark(self):
        return self.sb_top

    def release(self, m):
        self.sb_top = m

    def sb(self, name, shape, dt):
        nbytes = int(np.prod(shape[1:])) * (2 if dt == BF16 else 4)
        nbytes = (nbytes + 63) // 64 * 64
        off = self.sb_top
        assert off + nbytes <= self.sb_limit, f"SBUF overflow {name} {off}+{nbytes}"
        self.sb_top = off + nbytes
        self.uid += 1
        return self.nc.alloc_sbuf_tensor_at(f"{name}_{self.uid}", list(shape), dt, offset=off)

    @staticmethod
    def _keys(lst):
        out = []
        for a in lst:
            if a is None:
                continue
            if isinstance(a, (str, tuple)):
                out.append(a)
            else:
                t = a.tensor if hasattr(a, "tensor") else a
                out.append(t.name)
        return out

    def op(self, eng, fn, reads=(), writes=()):
        self.ops.append(dict(eng=eng, fn=fn, r=self._keys(reads), w=self._keys(writes), dma=False, bar=None, lab=self.label))

    def dma(self, eng, out, in_, reads=None, writes=None, **kw):
        r = self._keys(reads if reads is not None else [in_])
        w = self._keys(writes if writes is not None else [out])
        self.ops.append(dict(eng=eng, fn=lambda e: e.dma_start(out=out, in_=in_, **kw), r=r, w=w, dma=True, bar=None))

    def barrier(self):
        for e in ENGS:
            self.ops.append(dict(eng=e, fn=None, r=[], w=[], dma=False, bar=True))

    def emit(self):
        nc = self.nc
        ops = self.ops
        n = len(ops)
        last_w = {}
        readers = {}
        deps = [None] * n
        pending_dma = []
        last_real = {}
        for i, o in enumerate(ops):
            dd = {}
            if o["bar"]:
                for j in pending_dma:
                    dd[j] = True
                for e2 in ENGS:
                    if e2 != o["eng"] and last_real.get(e2) is not None:
                        dd[last_real[e2]] = True
                if o["eng"] == ENGS[-1]:
                    pending_dma = []
                deps[i] = sorted(dd)
                continue
            d = set()
            for k in o["r"]:
                if k in last_w:
                    d.add((last_w[k], "raw"))
                if k in self.psum_names:
                    for j in readers.get(k, ()):
                        if ops[j]["eng"] != o["eng"]:
                            d.add((j, "rar"))
            for k in o["w"]:
                if k in last_w:
                    d.add((last_w[k], "waw"))
                for j in readers.get(k, ()):
                    d.add((j, "war"))
            for k in o["r"]:
                readers.setdefault(k, []).append(i)
            for k in o["w"]:
                last_w[k] = i
                readers[k] = []
            for j, kind in d:
                if j == i:
                    continue
                oj = ops[j]
                if (not oj["dma"]) and (not o["dma"]) and oj["eng"] == o["eng"]:
                    if o["eng"] == "tensor":
                        continue
                dd[j] = True
            deps[i] = sorted(dd)
            if o["dma"]:
                pending_dma.append(i)
            else:
                last_real[o["eng"]] = i
        signal = [False] * n
        for i in range(n):
            for j in deps[i]:
                signal[j] = True
        cnt = {e: 0 for e in ENGS}
        dcnt = {e: 0 for e in ENGS}
        ev = [None] * n
        for i, o in enumerate(ops):
            e = o["eng"]
            if o["dma"]:
                k = dcnt[e] % NDMASEM
                m = dcnt[e] // NDMASEM + 1
                dcnt[e] += 1
                ev[i] = (("d", e, k), 16 * m)
            elif signal[i]:
                cnt[e] += 1
                ev[i] = (("c", e), cnt[e])
        self.stats = dict(n=n, sig=dict(cnt), dma=dict(dcnt))
        self.vlabels = [o.get("lab", "") for o in ops if o["eng"] == "vector" and o["fn"] is not None and not o["dma"]]
        semkeys = sorted(set(v[0] for v in ev if v is not None), key=str)
        with contextlib.ExitStack() as st:
            sems = {}
            for sk in semkeys:
                sems[sk] = st.enter_context(nc.semaphore("s_" + "_".join(str(x) for x in sk)))
            block = st.enter_context(nc.Block())
            per = {e: [] for e in ENGS}
            for i, o in enumerate(ops):
                per[o["eng"]].append(i)
            final_waits = {}
            for i, o in enumerate(ops):
                if o["dma"]:
                    final_waits[ev[i][0]] = max(final_waits.get(ev[i][0], 0), ev[i][1])

            def run_engine(ename, eobj):
                waited = {}
                for i in per[ename]:
                    o = ops[i]
                    need = {}
                    for j in deps[i]:
                        sk, val = ev[j]
                        need[sk] = max(need.get(sk, 0), val)
                    if o["dma"]:
                        sk, val = ev[i]
                        if val > 16:
                            need[sk] = max(need.get(sk, 0), val - 16)
                    for sk, val in need.items():
                        if waited.get(sk, 0) >= val:
                            continue
                        eobj.wait_ge(sems[sk], val)
                        waited[sk] = val
                    if o["fn"] is None:
                        continue
                    ins = o["fn"](eobj)
                    if ev[i] is not None:
                        sk, val = ev[i]
                        ins.then_inc(sems[sk], 16 if o["dma"] else 1)
                if ename == "sync":
                    for sk, val in final_waits.items():
                        if waited.get(sk, 0) < val:
                            eobj.wait_ge(sems[sk], val)
                    for e2 in COMPUTE:
                        if cnt[e2] > 0:
                            eobj.wait_ge(sems[("c", e2)], cnt[e2])

            block.tensor(lambda e: run_engine("tensor", e))
            block.vector(lambda e: run_engine("vector", e))
            block.scalar(lambda e: run_engine("scalar", e))
            block.gpsimd(lambda e: run_engine("gpsimd", e))
            block.sync(lambda e: run_engine("sync", e))
        return self.stats


def make_consts():
    c = {}
    c["ident_bf"] = np.eye(128, dtype=np.float32).astype(ml_dtypes.bfloat16)
    c["ident_f"] = np.eye(128, dtype=np.float32)
    s = np.arange(128)[:, None]
    l = np.arange(128)[None, :]
    c["tri_f"] = (s <= l).astype(np.float32)
    c["tri_b"] = (s >= l).astype(np.float32)
    n_freq = 16
    inv_freq = (10000.0 ** (-np.arange(n_freq, dtype=np.float32) / n_freq)).astype(np.float32)
    t = np.arange(2048)
    row = (t // 64).astype(np.float32)
    col = (t % 64).astype(np.float32)
    ang_r = row[:, None] * inv_freq
    ang_c = col[:, None] * inv_freq
    ang = np.concatenate([ang_r, ang_r, ang_c, ang_c], axis=-1).astype(np.float32)
    cos = np.cos(ang).astype(np.float32)
    sin = np.sin(ang).astype(np.float32)
    sgn = np.concatenate([-np.ones(16), np.ones(16), -np.ones(16), np.ones(16)]).astype(np.float32)
    cosT = np.ones((128, NTOK), np.float32)
    sinT = np.zeros((128, NTOK), np.float32)
    for m in range(2):
        cosT[64 * m:64 * m + 64, 256:] = cos.T
        sinT[64 * m:64 * m + 64, 256:] = (sin * sgn[None, :]).T
    c["cosT"] = cosT
    c["sinT"] = sinT
    c["ecolC"] = np.tile((np.arange(NE, dtype=np.float32) * CAP)[None, :], (128, 1)).astype(np.float32)
    return c


CONST_DT = {"ident_bf": BF16, "ident_f": F32, "tri_f": F32, "tri_b": F32, "cosT": F32, "sinT": F32, "ecolC": F32}

IN_SHAPES = {
    "xin": ([NTOK, D], F32), "c2": ([D, 2], F32),
    "w_ada": ([DEPTH, D, 6 * D], F32), "b_ada": ([DEPTH, 6 * D], F32), "w_in": ([DEPTH, D, IN_W], F32),
    "da_lambda": ([DEPTH, 4, 64], F32), "da_norm": ([DEPTH, 128], F32),
    "ml_conv_w": ([DEPTH, 3, 512], F32), "ml_conv_b": ([DEPTH, 512], F32),
    "ml_ib": ([DEPTH, 2, 4], F32), "ml_fb": ([DEPTH, 2, 4], F32), "ml_norm": ([DEPTH, 64], F32),
    "gl_wa": ([DEPTH, 2, 16, 128], F32), "gl_ba": ([DEPTH, 2, 128], F32), "gl_norm": ([DEPTH, 64], F32),
    "w_out": ([DEPTH, D, D], F32),
    "ln_mix_g": ([DEPTH, D], F32), "ln_mix_b": ([DEPTH, D], F32),
    "ln_ffn_g": ([DEPTH, D], F32), "ln_ffn_b": ([DEPTH, D], F32),
    "moe_wg": ([DEPTH, D, 4], F32), "moe_we": ([DEPTH, D, 32], F32),
    "moe_w1": ([DEPTH, NE, D, 512], F32), "moe_w3": ([DEPTH, NE, D, 512], F32), "moe_w2": ([DEPTH, NE, 512, D], F32),
}


def build(debug=False, n_layers=DEPTH, stop_after=None):
    nc = bass.Bass("TRN2", target_bir_lowering=False)
    P = Prog(nc)
    I = {}
    for k, (shp, dt) in IN_SHAPES.items():
        I[k] = nc.dram_tensor(k, shp, dt, kind="ExternalInput")
    consts = make_consts()
    for k, v in consts.items():
        I[k] = nc.dram_tensor(k, list(v.shape), CONST_DT[k], kind="ExternalInput")
    out_d = nc.dram_tensor("out", [2048, D], F32, kind="ExternalOutput")
    skind = "ExternalOutput" if debug else "Internal"
    S = {}

    def scratch(name, shape, dt):
        S[name] = nc.dram_tensor(name, list(shape), dt, kind=skind)
        return S[name]

    scratch("modrow", [2, 6 * D], F32)
    scratch("da_qk", [8, 128, NTOK], BF16)
    scratch("da_v", [NTOK, 4 * 130], BF16)
    scratch("ml_qk", [4, 128, NTOK], BF16)
    scratch("ml_v", [NTOK, 4 * 66], BF16)
    scratch("ml_o", [NTOK, 256], F32)
    scratch("ml_la", [2, 2, 128, NTOK], F32)
    scratch("ml_ig", [2, 2, 128, NTOK], F32)
    scratch("gl_qk", [4, 128, NTOK], BF16)
    scratch("gl_v", [NTOK, 256], BF16)
    scratch("gl_r", [NTOK, 256], F32)
    scratch("gl_la", [2, 2, 128, NTOK], F32)
    scratch("mixT", [8, 128, NTOK], BF16)
    scratch("x1", [NTOK, D], F32)
    scratch("xnext", [NTOK, D], F32)
    scratch("xslots", [NE * CAP, D], BF16)
    scratch("yslots", [NE * CAP, D], BF16)
    if debug:
        scratch("dbg_hT", [8, 128, NTOK], BF16)
        scratch("dbg_y", [NTOK, D], F32)
        scratch("dbg_fT", [8, 128, NTOK], BF16)
        scratch("dbg_W", [NTOK, 32], F32)
        scratch("dbg_moe", [NTOK, D], F32)

    pb = [nc.alloc_psum_tensor(f"pb{i}", [128, 512], F32) for i in range(8)]
    P.psum_names = set(t.name for t in pb)

    ident_bf = P.sb("ident_bf", [128, 128], BF16)
    ident_f = P.sb("ident_f", [128, 128], F32)
    tri_f = P.sb("tri_f", [128, 128], F32)
    tri_b = P.sb("tri_b", [128, 128], F32)
    ones_f = P.sb("ones_f", [128, 128], F32)
    modcol = P.sb("modcol", [128, 48, 2], F32)
    nlam = P.sb("nlam", [128, 1], F32)
    Wt = P.sb("Wt", [128, NT, 32], F32)
    idxs = P.sb("idxs", [128, NT, 2], U32)
    wsel = P.sb("wsel", [128, NT, 2], F32)
    ecolC = P.sb("ecolC", [128, 32], F32)
    cntE = P.sb("cntE", [128, 32], F32)
    zt = P.sb("zt", [128, NST, D], BF16)
    persist_mark = P.mark()

    P.dma("sync", ident_bf[:, :], I["ident_bf"][:, :])
    P.dma("sync", ident_f[:, :], I["ident_f"][:, :])
    P.dma("sync", tri_f[:, :], I["tri_f"][:, :])
    P.dma("sync", tri_b[:, :], I["tri_b"][:, :])
    P.dma("sync", ecolC[:, :], I["ecolC"][:, :])
    P.op("vector", lambda e: e.memset(ones_f[:, :], 1.0), [], [ones_f])
    P.op("gpsimd", lambda e: e.memset(zt[:, :, :], 0.0), [], [zt])

    regs = {}

    def get_bc(e):
        if "bc" not in regs:
            regs["bc"] = e.alloc_register("bcreg")
            e.reg_mov(regs["bc"], NE * CAP - 1)
        return regs["bc"]

    def V(fn, r, w):
        P.op("vector", fn, r, w)

    def A(fn, r, w):
        P.op("scalar", fn, r, w)

    def G(fn, r, w):
        P.op("gpsimd", fn, r, w)

    def T(fn, r, w):
        P.op("tensor", fn, r, w)

    def mm(out, lhsT, rhs, start, stop, r=None, w=None):
        T(lambda e: e.matmul(out, lhsT, rhs, start=start, stop=stop), r if r is not None else [lhsT, rhs],
          w if w is not None else [out])

    def tr(out, in_, ident, r=None, w=None):
        T(lambda e: e.transpose(out, in_, ident), r if r is not None else [in_, ident], w if w is not None else [out])

    def ln_stats(xt, tagbuf):
        st, mv, rstd = tagbuf
        V(lambda e: e.bn_stats(st[:, 0, :], xt[:, 0:512]), [xt], [st])
        V(lambda e: e.bn_stats(st[:, 1, :], xt[:, 512:1024]), [xt], [st])
        V(lambda e: e.bn_aggr(mv[:, :], st[:, :, :].rearrange("p a b -> p (a b)")), [st], [mv])
        A(lambda e: e.activation(rstd[:, :], mv[:, 1:2], AF.Sqrt, bias=eps_col[:, :], scale=1.0), [mv, eps_col], [rstd])
        V(lambda e: e.reciprocal(rstd[:, :], rstd[:, :]), [rstd], [rstd])
        return mv[:, 0:1], rstd[:, 0:1]

    eps_col = P.sb("eps_col", [128, 1], F32)
    V(lambda e: e.memset(eps_col[:, :], LN_EPS), [], [eps_col])
    one_col = P.sb("one_col", [128, 1], F32)
    V(lambda e: e.memset(one_col[:, :], 1.0), [], [one_col])
    persist_mark = P.mark()

    def layer(L, x_cur):
        lam_init = 0.8 - 0.6 * math.exp(-0.3 * L)
        last = (L == DEPTH - 1)

        P.label = f"L{L}_P0"
        P.barrier()
        P.release(persist_mark)
        c2 = P.sb("c2", [128, 8, 2], F32)
        c2b = P.sb("c2b", [128, 8, 2], BF16)
        brow = P.sb("brow", [2, 6 * D], F32)
        mrow = P.sb("mrow", [2, 6 * D], F32)
        wa = [P.sb(f"wa{i}", [128, 8, 512], BF16) for i in range(2)]
        P.dma("sync", c2[:, :, :], I["c2"].ap().rearrange("(k p) r -> p k r", p=128))
        A(lambda e: e.activation(c2b[:, :, :], c2[:, :, :], AF.Silu), [c2], [c2b])
        for r in range(2):
            P.dma("sync", brow[r:r + 1, :], I["b_ada"][L:L + 1, :])
        for cb in range(12):
            w = wa[cb % 2]
            P.dma("gpsimd", w[:, :, :], I["w_ada"][L, :, cb * 512:(cb + 1) * 512].rearrange("(k p) n -> p k n", p=128))
            for k in range(8):
                mm(pb[cb % 2][0:2, :], c2b[:, k, :], w[:, k, :], k == 0, k == 7)
            V(lambda e, cb=cb: e.tensor_tensor(mrow[:, cb * 512:(cb + 1) * 512], pb[cb % 2][0:2, :],
                                               brow[:, cb * 512:(cb + 1) * 512], ALU.add),
              [pb[cb % 2], brow], [mrow])
        P.dma("sync", S["modrow"][:, :], mrow[:, :])
        for r in range(2):
            P.dma("sync", modcol[:, :, r], S["modrow"][r].rearrange("(c p) -> p c", p=128),
                  allow_slow_non_contiguous=True)
        V(lambda e: e.tensor_scalar_add(modcol[:, 8:16, :], modcol[:, 8:16, :], 1.0), [modcol], [modcol])
        V(lambda e: e.tensor_scalar_add(modcol[:, 32:40, :], modcol[:, 32:40, :], 1.0), [modcol], [modcol])
        lamt = P.sb("lamt", [128, 4, 64], F32)
        lamp = P.sb("lamp", [128, 2, 64], F32)
        lamd = P.sb("lamd", [128, 2], F32)
        P.dma("sync", lamt[:, :, :], I["da_lambda"][L:L + 1, :, :].broadcast_to([128, 4, 64]))
        V(lambda e: e.tensor_tensor(lamp[:, 0, :], lamt[:, 0, :], lamt[:, 1, :], ALU.mult), [lamt], [lamp])
        V(lambda e: e.tensor_tensor(lamp[:, 1, :], lamt[:, 2, :], lamt[:, 3, :], ALU.mult), [lamt], [lamp])
        V(lambda e: e.tensor_reduce(lamd[:, :], lamp[:, :, :], AX.X, ALU.add), [lamp], [lamd])
        A(lambda e: e.activation(lamd[:, :], lamd[:, :], AF.Exp), [lamd], [lamd])
        V(lambda e: e.scalar_tensor_tensor(nlam[:, :], lamd[:, 1:2], -lam_init, lamd[:, 0:1], ALU.add, ALU.subtract),
          [lamd], [nlam])

        P.label = f"L{L}_P1"
        P.barrier()
        P.release(persist_mark)
        hT = P.sb("hT", [128, 8, NTOK], BF16)
        win = P.sb("win", [128, 8, IN_W], BF16)
        for (c0, c1) in ((0, 844), (844, 1688), (1688, 2532), (2532, IN_W)):
            P.dma("gpsimd", win[:, :, c0:c1], I["w_in"][L, :, c0:c1].rearrange("(k p) n -> p k n", p=128))
        m1 = P.mark()
        xt = [P.sb(f"xt{i}", [128, D], F32) for i in range(4)]
        xn = [P.sb(f"xn{i}", [128, D], BF16) for i in range(4)]
        stb = [(P.sb(f"st{i}", [128, 2, 6], F32), P.sb(f"mv{i}", [128, 2], F32), P.sb(f"rs{i}", [128, 1], F32))
               for i in range(4)]

        def p1_s1(t):
            xb = xt[t % 4]
            st, mv, rstd = stb[t % 4]
            P.dma("sync", xb[:, :], x_cur[t * 128:(t + 1) * 128, :])
            V(lambda e: e.bn_stats(st[:, 0, :], xb[:, 0:512]), [xb], [st])
            V(lambda e: e.bn_stats(st[:, 1, :], xb[:, 512:1024]), [xb], [st])
            V(lambda e: e.bn_aggr(mv[:, :], st[:, :, :].rearrange("p a b -> p (a b)")), [st], [mv])

        def p1_s2(t):
            xb = xt[t % 4]
            st, mv, rstd = stb[t % 4]
            xnb = xn[t % 4]
            A(lambda e: e.activation(rstd[:, :], mv[:, 1:2], AF.Sqrt, bias=eps_col[:, :], scale=1.0), [mv, eps_col], [rstd])
            V(lambda e: e.reciprocal(rstd[:, :], rstd[:, :]), [rstd], [rstd])
            V(lambda e: e.tensor_scalar(xnb[:, :], xb[:, :], mv[:, 0:1], rstd[:, 0:1], ALU.subtract, ALU.mult), [xb, mv, rstd], [xnb])

        def p1_s3(t):
            b = t % 2
            xnb = xn[t % 4]
            for ch in range(8):
                bank = 4 + 2 * b + ch // 4
                pT = pb[bank][:, :].bitcast(BF16)
                tr(pT[:, (ch % 4) * 128:(ch % 4 + 1) * 128], xnb[:, ch * 128:(ch + 1) * 128], ident_bf[:, :],
                   [xnb, ident_bf], [pb[bank]])

        def p1_s4(t):
            b = t % 2
            r = 1 if t < NCTX_T else 0
            for ch in range(8):
                bank = 4 + 2 * b + ch // 4
                pT = pb[bank][:, :].bitcast(BF16)
                o = hT[:, ch, t * 128:(t + 1) * 128]
                i_ = pT[:, (ch % 4) * 128:(ch % 4 + 1) * 128]
                if ch < 4:
                    A(lambda e, o=o, i_=i_, ch=ch, r=r: e.activation(o, i_, AF.Identity, bias=modcol[:, ch, r:r + 1],
                                                                     scale=modcol[:, 8 + ch, r:r + 1]),
                      [pb[bank], modcol], [("hT", t)])
                else:
                    V(lambda e, o=o, i_=i_, ch=ch, r=r: e.tensor_scalar(o, i_, modcol[:, 8 + ch, r:r + 1],
                                                                        modcol[:, ch, r:r + 1], ALU.mult, ALU.add),
                      [pb[bank], modcol], [("hT", t)])

        for i_ in range(NT + 3):
            if i_ < NT:
                p1_s1(i_)
            if 0 <= i_ - 1 < NT:
                p1_s2(i_ - 1)
            if 0 <= i_ - 2 < NT:
                p1_s3(i_ - 2)
            if 0 <= i_ - 3 < NT:
                p1_s4(i_ - 3)
        hT_all = [("hT", t) for t in range(NT)]
        if debug:
            for ch in range(8):
                P.dma("sync", S["dbg_hT"][ch], hT[:, ch, :], reads=hT_all)
        if stop_after == "P1":
            return None

        P.label = f"L{L}_P2"
        P.barrier()
        P.release(m1)
        wrot = P.sb("wrot", [128, 8, 1024], BF16)
        wv = win[:, :, 0:1024].rearrange("p k (g h s) -> p k g h s", h=2, s=16)
        rv = wrot[:, :, :].rearrange("p k (g h s) -> p k g h s", h=2, s=16)
        for k in range(8):
            V(lambda e, k=k: e.tensor_copy(rv[:, k, :, 0, :], wv[:, k, :, 1, :]), [win], [wrot])
            G(lambda e, k=k: e.tensor_copy(rv[:, k, :, 1, :], wv[:, k, :, 0, :]), [win], [wrot])
        cosT = P.sb("cosT", [128, NTOK], F32)
        sinT = P.sb("sinT", [128, NTOK], F32)
        P.dma("sync", cosT[:, :], I["cosT"][:, :])
        P.dma("sync", sinT[:, :], I["sinT"][:, :])
        m2 = P.mark()

        def fm_proj(bank, lhs_fn, M, tb, extra_r=()):
            t0, tn = TB[tb]
            for k in range(8):
                mm(pb[bank][0:M, 0:tn], lhs_fn(k), hT[:, k, t0:t0 + tn], k == 0, k == 7,
                   r=[win, wrot] + hT_all + list(extra_r), w=[pb[bank]])

        P.label = f"L{L}_P2a_daqk"
        stg = [P.sb(f"stg{i}", [128, NTOK], BF16) for i in range(2)]
        t1 = [P.sb(f"t1_{i}", [128, 512], F32) for i in range(2)]
        t2 = [P.sb(f"t2_{i}", [128, 512], F32) for i in range(2)]
        for ch in range(8):
            sg = stg[ch % 2]
            for tb in range(5):
                t0, tn = TB[tb]
                fm_proj(0, lambda k, ch=ch: win[:, k, ch * 128:(ch + 1) * 128], 128, tb)
                fm_proj(1, lambda k, ch=ch: wrot[:, k, ch * 128:(ch + 1) * 128], 128, tb)
                a1, a2 = t1[tb % 2], t2[tb % 2]
                V(lambda e, a1=a1, t0=t0, tn=tn: e.tensor_tensor(a1[:, 0:tn], pb[0][:, 0:tn], cosT[:, t0:t0 + tn], ALU.mult),
                  [pb[0], cosT], [a1])
                V(lambda e, a2=a2, t0=t0, tn=tn: e.tensor_tensor(a2[:, 0:tn], pb[1][:, 0:tn], sinT[:, t0:t0 + tn], ALU.mult),
                  [pb[1], sinT], [a2])
                G(lambda e, a1=a1, a2=a2, sg=sg, t0=t0, tn=tn: e.tensor_tensor(sg[:, t0:t0 + tn], a1[:, 0:tn], a2[:, 0:tn], ALU.add),
                  [a1, a2], [sg])
            P.dma("sync", S["da_qk"][ch], sg[:, :])
        P.barrier()
        P.release(m2)

        P.label = f"L{L}_P2b_mlqk"
        cw = P.sb("cw", [128, 4, 3], F32)
        cbias = P.sb("cbias", [128, 4], F32)
        for j_ in range(3):
            P.dma("sync", cw[:, :, j_], I["ml_conv_w"][L, j_].rearrange("(c p) -> p c", p=128), allow_slow_non_contiguous=True)
        P.dma("sync", cbias[:, :], I["ml_conv_b"][L].rearrange("(c p) -> p c", p=128), allow_slow_non_contiguous=True)
        pre = [P.sb(f"pre{i}", [128, NTOK], F32) for i in range(2)]
        acc = [P.sb(f"acc{i}", [128, NTOK], F32) for i in range(2)]
        stg = [P.sb(f"stgm{i}", [128, NTOK], BF16) for i in range(2)]
        for ch in range(4):
            pr, ac, sg = pre[ch % 2], acc[ch % 2], stg[ch % 2]
            for tb in range(5):
                t0, tn = TB[tb]
                bank = tb % 2
                fm_proj(bank, lambda k, ch=ch: win[:, k, 1536 + ch * 128:1536 + (ch + 1) * 128], 128, tb)
                A(lambda e, pr=pr, bank=bank, t0=t0, tn=tn: e.copy(pr[:, t0:t0 + tn], pb[bank][:, 0:tn]), [pb[bank]], [pr])
            V(lambda e, pr=pr, ac=ac, ch=ch: e.tensor_scalar(ac[:, :], pr[:, :], cw[:, ch, 1:2], cbias[:, ch:ch + 1],
                                                             ALU.mult, ALU.add), [pr, cw, cbias], [ac])
            for (s0, s1) in ((0, 256), (256, NTOK)):
                V(lambda e, pr=pr, ac=ac, ch=ch, s0=s0, s1=s1: e.scalar_tensor_tensor(
                    ac[:, s0 + 1:s1], pr[:, s0:s1 - 1], cw[:, ch, 0:1], ac[:, s0 + 1:s1], ALU.mult, ALU.add),
                  [pr, cw, ac], [ac])
                V(lambda e, pr=pr, ac=ac, ch=ch, s0=s0, s1=s1: e.scalar_tensor_tensor(
                    ac[:, s0:s1 - 1], pr[:, s0 + 1:s1], cw[:, ch, 2:3], ac[:, s0:s1 - 1], ALU.mult, ALU.add),
                  [pr, cw, ac], [ac])
            A(lambda e, ac=ac, sg=sg: e.activation(sg[:, :], ac[:, :], AF.Silu), [ac], [sg])
            P.dma("sync", S["ml_qk"][ch], sg[:, :])
        P.barrier()
        P.release(m2)

        P.label = f"L{L}_P2c_gates"
        wrep = P.sb("wrep", [128, 8, 128], BF16)
        gcol = P.sb("gcol", [128, 2, 2, 2], F32)
        for ty, nm in ((0, "ml_ib"), (1, "ml_fb")):
            for d in range(2):
                for h in range(4):
                    P.dma("sync", gcol[64 * (h % 2):64 * (h % 2) + 64, ty, d, h // 2:h // 2 + 1],
                          I[nm][L, d:d + 1, h:h + 1].broadcast_to([64, 1]))
        ngfb = P.sb("ngfb", [128, 2, 2], F32)
        V(lambda e: e.tensor_scalar_mul(ngfb[:, :, :], gcol[:, 1, :, :], -1.0), [gcol], [ngfb])
        gst = [P.sb(f"gst{i}", [128, NTOK], F32) for i in range(2)]
        gi = 0
        for ty in range(2):
            for d in range(2):
                for cc in range(2):
                    for hh in range(2):
                        colx = 2560 + 8 * ty + 4 * d + 2 * cc + hh
                        V(lambda e, hh=hh, colx=colx: e.tensor_copy(
                            wrep[:, :, 64 * hh:64 * hh + 64], win[:, :, colx:colx + 1].broadcast_to([128, 8, 64])),
                          [win], [wrep])
                    sg = gst[gi % 2]
                    gi += 1
                    for tb in range(5):
                        t0, tn = TB[tb]
                        bank = tb % 2
                        fm_proj(bank, lambda k: wrep[:, k, :], 128, tb, extra_r=[wrep])
                        if ty == 0:
                            A(lambda e, sg=sg, bank=bank, t0=t0, tn=tn, d=d, cc=cc: e.activation(
                                sg[:, t0:t0 + tn], pb[bank][:, 0:tn], AF.Identity, bias=gcol[:, 0, d, cc:cc + 1], scale=1.0),
                              [pb[bank], gcol], [sg])
                        else:
                            A(lambda e, sg=sg, bank=bank, t0=t0, tn=tn, d=d, cc=cc: e.activation(
                                sg[:, t0:t0 + tn], pb[bank][:, 0:tn], AF.Exp, bias=ngfb[:, d, cc:cc + 1], scale=-1.0),
                              [pb[bank], ngfb], [sg])
                    if ty == 1:
                        A(lambda e, sg=sg: e.activation(sg[:, :], sg[:, :], AF.Ln, bias=one_col[:, :], scale=1.0), [sg, one_col], [sg])
                        V(lambda e, sg=sg: e.tensor_scalar_mul(sg[:, :], sg[:, :], -1.0), [sg], [sg])
                    P.dma("sync", S["ml_ig" if ty == 0 else "ml_la"][d, cc], sg[:, :])
        P.barrier()
        P.release(m2)

        P.label = f"L{L}_P2d_gl"
        stg = [P.sb(f"stgg{i}", [128, NTOK], BF16) for i in range(2)]
        wpad = P.sb("wpad", [128, 8, 128], BF16)
        gi = 0
        for qk_ in range(2):
            for cc in range(2):
                sg = stg[gi % 2]
                gi += 1
                G(lambda e: e.memset(wpad[:, :, :], 0.0), [], [wpad])
                for hh in range(2):
                    c0 = 2576 + 128 * qk_ + (2 * cc + hh) * 32
                    G(lambda e, hh=hh, c0=c0: e.tensor_copy(wpad[:, :, 64 * hh:64 * hh + 32], win[:, :, c0:c0 + 32]), [win], [wpad])
                for tb in range(5):
                    t0, tn = TB[tb]
                    bank = tb % 2
                    fm_proj(bank, lambda k: wpad[:, k, :], 128, tb, extra_r=[wpad])
                    A(lambda e, sg=sg, bank=bank, t0=t0, tn=tn: e.copy(sg[:, t0:t0 + tn], pb[bank][:, 0:tn]), [pb[bank]], [sg])
                P.dma("sync", S["gl_qk"][2 * qk_ + cc], sg[:, :])
        aT = P.sb("aT", [32, NTOK], BF16)
        for tb in range(5):
            t0, tn = TB[tb]
            bank = tb % 2
            fm_proj(bank, lambda k: win[:, k, 3344:3376], 32, tb)
            A(lambda e, bank=bank, t0=t0, tn=tn: e.copy(aT[:, t0:t0 + tn], pb[bank][0:32, 0:tn]), [pb[bank]], [aT])
        wap = P.sb("wap", [32, 2, 2, 128], BF16)
        nba = P.sb("nba", [128, 2, 2], F32)
        G(lambda e: e.memset(wap[:, :, :, :], 0.0), [], [wap])
        G(lambda e: e.memset(nba[:, :, :], 0.0), [], [nba])
        for d in range(2):
            for cc in range(2):
                for hh in range(2):
                    h0 = (2 * cc + hh) * 32
                    P.dma("gpsimd", wap[16 * d:16 * d + 16, d, cc, 64 * hh:64 * hh + 32], I["gl_wa"][L, d, :, h0:h0 + 32])
                    P.dma("sync", nba[64 * hh:64 * hh + 32, d, cc:cc + 1], I["gl_ba"][L, d, h0:h0 + 32].rearrange("(p o) -> p o", o=1),
                          allow_slow_non_contiguous=True)
        V(lambda e: e.tensor_scalar_mul(nba[:, :, :], nba[:, :, :], -1.0), [nba], [nba])
        gls = [P.sb(f"gls{i}", [128, NTOK], F32) for i in range(2)]
        gi = 0
        for d in range(2):
            for cc in range(2):
                sg = gls[gi % 2]
                gi += 1
                for tb in range(5):
                    t0, tn = TB[tb]
                    bank = 2 + tb % 2
                    mm(pb[bank][:, 0:tn], wap[:, d, cc, :], aT[:, t0:t0 + tn], True, True)
                    A(lambda e, sg=sg, bank=bank, t0=t0, tn=tn, d=d, cc=cc: e.activation(
                        sg[:, t0:t0 + tn], pb[bank][:, 0:tn], AF.Exp, bias=nba[:, d, cc:cc + 1], scale=-1.0), [pb[bank], nba], [sg])
                A(lambda e, sg=sg: e.activation(sg[:, :], sg[:, :], AF.Ln, bias=one_col[:, :], scale=1.0), [sg, one_col], [sg])
                V(lambda e, sg=sg: e.tensor_scalar_mul(sg[:, :], sg[:, :], -1.0 / 16.0), [sg], [sg])
                P.dma("sync", S["gl_la"][d, cc], sg[:, :])
        P.barrier()
        P.release(m2)

        P.label = f"L{L}_P2e_tm"
        vst = [P.sb(f"vst{i}", [128, 4, 130], BF16) for i in range(2)]
        mvst = [P.sb(f"mvst{i}", [128, 4, 66], BF16) for i in range(2)]
        ost = [P.sb(f"ost{i}", [128, 256], F32) for i in range(2)]
        gvst = [P.sb(f"gvst{i}", [128, 256], BF16) for i in range(2)]
        rst = [P.sb(f"rst{i}", [128, 256], F32) for i in range(2)]
        for i in range(2):
            G(lambda e, i=i: e.memset(vst[i][:, :, :], 0.0), [], [vst[i]])
            G(lambda e, i=i: e.memset(vst[i][:, :, 128:129], 1.0), [], [vst[i]])
            G(lambda e, i=i: e.memset(mvst[i][:, :, :], 0.0), [], [mvst[i]])
            G(lambda e, i=i: e.memset(mvst[i][:, :, 64:65], 1.0), [], [mvst[i]])

        def tm_proj(bank, t, c0, ncol):
            for k in range(8):
                mm(pb[bank][:, 0:ncol], hT[:, k, t * 128:(t + 1) * 128], win[:, k, c0:c0 + ncol], k == 0, k == 7,
                   r=[win, ("hT", t)], w=[pb[bank]])

        for t in range(NT):
            b = t % 2
            ts_ = slice(t * 128, (t + 1) * 128)
            B0, B1, B2 = 3 * b, 3 * b + 1, 3 * b + 2
            tm_proj(B0, t, 1024, 512)
            V(lambda e, b=b, B0=B0: e.tensor_copy(vst[b][:, :, 0:128], pb[B0][:, :].rearrange("p (h d) -> p h d", h=4)), [pb[B0]], [vst[b]])
            P.dma("sync", S["da_v"][ts_, :], vst[b][:, :, :].rearrange("p h d -> p (h d)"))
            tm_proj(B1, t, 2048, 512)
            V(lambda e, b=b, B1=B1: e.tensor_copy(mvst[b][:, :, 0:64], pb[B1][:, 0:256].rearrange("p (h d) -> p h d", h=4)), [pb[B1]], [mvst[b]])
            A(lambda e, b=b, B1=B1: e.activation(ost[b][:, :], pb[B1][:, 256:512], AF.Sigmoid), [pb[B1]], [ost[b]])
            P.dma("sync", S["ml_v"][ts_, :], mvst[b][:, :, :].rearrange("p h d -> p (h d)"))
            P.dma("sync", S["ml_o"][ts_, :], ost[b][:, :])
            tm_proj(B2, t, 2832, 512)
            V(lambda e, b=b, B2=B2: e.tensor_copy(gvst[b][:, :], pb[B2][:, 0:256]), [pb[B2]], [gvst[b]])
            A(lambda e, b=b, B2=B2: e.activation(rst[b][:, :], pb[B2][:, 256:512], AF.Silu), [pb[B2]], [rst[b]])
            P.dma("sync", S["gl_v"][ts_, :], gvst[b][:, :])
            P.dma("sync", S["gl_r"][ts_, :], rst[b][:, :])
        if stop_after == "P2":
            return None

        P.label = f"L{L}_P3"
        P.barrier()
        P.release(persist_mark)
        qk = P.sb("qk", [128, 4, NTOK], BF16)
        kz = P.sb("kz", [128, 2, 4, NTOK], BF16)
        vv = P.sb("vv", [128, NT, 520], BF16)
        for m in range(2):
            G(lambda e, m=m: e.memset(kz[64 * (1 - m):64 * (1 - m) + 64, m, :, :], 0.0), [], [kz])
        for ch in range(4):
            P.dma("sync", qk[:, ch, :], S["da_qk"][ch])
            for m in range(2):
                P.dma("sync", kz[64 * m:64 * m + 64, m, ch, :], S["da_qk"][4 + ch, 64 * m:64 * m + 64, :])
        for t in range(NT):
            P.dma("sync", vv[:, t, :], S["da_v"][t * 128:(t + 1) * 128, :])
        for ex_ in (range(NE) if L == 0 else []):
            P.dma("sync", S["xslots"][ex_ * CAP:(ex_ + 1) * CAP, :].rearrange("(s p) d -> p s d", p=128), zt[:, :, :],
                  writes=[("xslots", t_, k__) for t_ in range(NT) for k__ in range(2)])
        gda = P.sb("gda", [128, 128], F32)
        P.dma("sync", gda[:, :], I["da_norm"][L:L + 1, :].broadcast_to([128, 128]))
        V(lambda e: e.tensor_scalar_mul(gda[:, :], gda[:, :], 1.0 - lam_init), [gda], [gda])
        Eb = [P.sb(f"Eb{i}", [128, 512], BF16) for i in range(3)]
        osb = [P.sb(f"osb{i}", [128, 128], F32) for i in range(2)]
        o2 = [P.sb(f"o2{i}", [128, 128], F32) for i in range(2)]
        sq = [P.sb(f"sq{i}", [128, 128], F32) for i in range(2)]
        rc = [P.sb(f"rc{i}", [128, 4], F32) for i in range(2)]
        oall = P.sb("oall", [128, NT, 4, 128], BF16)
        vvh = vv[:, :, :].rearrange("p t (h d) -> p t h d", h=4)
        qblocks = [(0, 256, [0, 1])] + [(256 + 512 * i, 512, list(range(NT))) for i in range(4)]
        nonlocal_ei = [0]
        oi = 0
        rnd = 0
        for h in range(4):
            for (q0, qn, kts) in qblocks:
                nqs = qn // 128
                ob = 2 + 3 * (rnd % 2)
                rnd += 1
                touched = set()
                seq = [(m, kt, qs) for m in range(2) for kt in kts for qs in range(nqs)]
                lastt = {}
                for (m, kt, qs) in seq:
                    lastt[(qs * 2 + m) // 3] = (m, kt, qs)
                steps = [(m, kt) for m in range(2) for kt in kts]

                def issue_scores(i):
                    m, kt = steps[i]
                    sbank = i % 2
                    mm(pb[sbank][:, 0:qn], kz[:, m, h, kt * 128:(kt + 1) * 128], qk[:, h, q0:q0 + qn], True, True,
                       r=[kz, qk], w=[pb[sbank]])
                    nonlocal_ei[0] += 1
                    E = Eb[nonlocal_ei[0] % 3]
                    A(lambda e, E=E, sbank=sbank, qn=qn: e.activation(E[:, 0:qn], pb[sbank][:, 0:qn], AF.Exp, scale=0.125),
                      [pb[sbank]], [E])
                    return E

                Es = {0: issue_scores(0)}
                for i, (m, kt) in enumerate(steps):
                    if i + 1 < len(steps):
                        Es[i + 1] = issue_scores(i + 1)
                    E = Es.pop(i)
                    for qs in range(nqs):
                        a = qs * 2 + m
                        bank = ob + a // 3
                        c0 = 130 * (a % 3)
                        st_ = bank not in touched
                        touched.add(bank)
                        sp_ = lastt[a // 3] == (m, kt, qs)
                        mm(pb[bank][:, c0:c0 + 129], E[:, qs * 128:(qs + 1) * 128], vvh[:, kt, h, 0:129], st_, sp_,
                           r=[E, vv], w=[pb[bank]])
                for qs in range(nqs):
                    j = oi % 2
                    oi += 1
                    a0, a1 = qs * 2, qs * 2 + 1
                    b0, c0 = ob + a0 // 3, 130 * (a0 % 3)
                    b1, c1 = ob + a1 // 3, 130 * (a1 % 3)
                    tq = (q0 + qs * 128) // 128
                    V(lambda e, j=j, b0=b0, c0=c0: e.reciprocal(rc[j][:, 0:1], pb[b0][:, c0 + 128:c0 + 129]), [pb[b0]], [rc[j]])
                    V(lambda e, j=j, b1=b1, c1=c1: e.reciprocal(rc[j][:, 1:2], pb[b1][:, c1 + 128:c1 + 129]), [pb[b1]], [rc[j]])
                    V(lambda e, j=j: e.tensor_tensor(rc[j][:, 1:2], rc[j][:, 1:2], nlam[:, :], ALU.mult), [rc[j], nlam], [rc[j]])
                    V(lambda e, j=j, b0=b0, c0=c0: e.tensor_scalar(osb[j][:, :], pb[b0][:, c0:c0 + 128], rc[j][:, 0:1], None, ALU.mult),
                      [pb[b0], rc[j]], [osb[j]])
                    V(lambda e, j=j, b1=b1, c1=c1: e.scalar_tensor_tensor(o2[j][:, :], pb[b1][:, c1:c1 + 128], rc[j][:, 1:2],
                                                                         osb[j][:, :], ALU.mult, ALU.add),
                      [pb[b1], rc[j], osb[j]], [o2[j]])
                    G(lambda e, j=j: e.tensor_tensor(sq[j][:, :], o2[j][:, :], o2[j][:, :], ALU.mult), [o2[j]], [sq[j]])
                    V(lambda e, j=j: e.tensor_reduce(rc[j][:, 2:3], sq[j][:, :], AX.X, ALU.add), [sq[j]], [rc[j]])
                    A(lambda e, j=j: e.activation(rc[j][:, 2:3], rc[j][:, 2:3], AF.Sqrt, bias=eps_col[:, :], scale=1.0 / 128.0),
                      [rc[j], eps_col], [rc[j]])
                    V(lambda e, j=j: e.reciprocal(rc[j][:, 3:4], rc[j][:, 2:3]), [rc[j]], [rc[j]])
                    V(lambda e, j=j, tq=tq, h=h: e.scalar_tensor_tensor(oall[:, tq, h, :], o2[j][:, :], rc[j][:, 3:4], gda[:, :], ALU.mult, ALU.mult),
                      [o2[j], rc[j], gda], [("oall", tq, h)])
        mixst = [P.sb(f"mixst{i}", [128, NTOK], BF16) for i in range(2)]
        ti = 0
        for h in range(4):
            mst = mixst[h % 2]
            for t0_ in range(0, NT, 4):
                nt_ = min(4, NT - t0_)
                bank = ti % 2
                ti += 1
                pT = pb[bank][:, :].bitcast(BF16)
                for k_ in range(nt_):
                    tr(pT[:, k_ * 128:(k_ + 1) * 128], oall[:, t0_ + k_, h, :], ident_bf[:, :], [("oall", t0_ + k_, h), ident_bf], [pb[bank]])
                if bank == 0:
                    A(lambda e, pT=pT, mst=mst, t0_=t0_, nt_=nt_: e.copy(mst[:, t0_ * 128:(t0_ + nt_) * 128], pT[:, 0:nt_ * 128]), [pb[bank]], [mst])
                else:
                    V(lambda e, pT=pT, mst=mst, t0_=t0_, nt_=nt_: e.tensor_copy(mst[:, t0_ * 128:(t0_ + nt_) * 128], pT[:, 0:nt_ * 128]), [pb[bank]], [mst])
            P.dma("sync", S["mixT"][h], mst[:, :])
        if stop_after == "P3":
            return None

        P.label = f"L{L}_P4/P5"
        def decay_attn(kind):
            P.barrier()
            P.release(persist_mark)
            ml = (kind == "ml")
            P.label = f"L{L}_P45_{kind}"
            ncc = 2
            Hc = 2
            dk = 64
            dva = 65 if ml else 64
            vstride = 66 if ml else 64
            qscale = (64 if ml else 32) ** -0.5
            qT = P.sb("qT", [128, ncc, NTOK], BF16)
            kT = P.sb("kT", [128, ncc, NTOK], BF16)
            for cc in range(ncc):
                P.dma("sync", qT[:, cc, :], S["ml_qk" if ml else "gl_qk"][cc])
                P.dma("sync", kT[:, cc, :], S["ml_qk" if ml else "gl_qk"][2 + cc])
            Vt = P.sb("Vt", [128, NT, 4 * vstride], BF16)
            for t in range(NT):
                P.dma("sync", Vt[:, t, :], S["ml_v" if ml else "gl_v"][t * 128:(t + 1) * 128, :])
            Vh = Vt[:, :, :].rearrange("p t (h d) -> p t h d", h=4)
            Hsum = P.sb("Hsum", [128, NT, 256], F32)
            Hs4 = Hsum[:, :, :].rearrange("p t (h d) -> p t h d", h=4)
            Hsum2 = P.sb("Hsum2", [128, NT, 256], F32)
            Hb4 = Hsum2[:, :, :].rearrange("p t (h d) -> p t h d", h=4)
            chains = [(d, cc) for d in range(2) for cc in range(ncc)]
            laC = {c: P.sb(f"la{c[0]}{c[1]}", [128, NTOK], F32) for c in chains}
            igC = {c: (P.sb(f"ig{c[0]}{c[1]}", [128, NTOK], F32) if ml else None) for c in chains}
            SstC = {c: P.sb(f"Sst{c[0]}{c[1]}", [128, dva], F32) for c in chains}
            SbfC = {c: P.sb(f"Sbf{c[0]}{c[1]}", [128, dva], BF16) for c in chains}
            NB = 4
            bT = [P.sb(f"bT{i}", [128, 128], F32) for i in range(NB)]
            pfx = [P.sb(f"pfx{i}", [128, 128], F32) for i in range(NB)]
            arg = [P.sb(f"arg{i}", [128, 128], F32) for i in range(NB)]
            eq = [P.sb(f"eq{i}", [128, 128], F32) for i in range(NB)]
            ek = [P.sb(f"ek{i}", [128, 128], F32) for i in range(NB)]
            ekh = [P.sb(f"ekh{i}", [128, 128], F32) for i in range(NB)]
            gam = [P.sb(f"gam{i}", [128, 1], F32) for i in range(NB)]
            qt_ = [P.sb(f"qt_{i}", [128, 128], BF16) for i in range(NB)]
            kt_ = [P.sb(f"kt_{i}", [128, 128], BF16) for i in range(NB)]
            kh_ = [P.sb(f"kh_{i}", [128, 128], BF16) for i in range(NB)]
            khT = [P.sb(f"khT{i}", [128, 128], BF16) for i in range(NB)]
            PT = [P.sb(f"PT{i}", [128, Hc, 128], BF16) for i in range(NB)]
            den = [P.sb(f"den{i}", [128, Hc, 1], F32) for i in range(NB)]
            for c in chains:
                P.dma("sync", laC[c][:, :], S["ml_la" if ml else "gl_la"][c[0], c[1]])
                if ml:
                    P.dma("sync", igC[c][:, :], S["ml_ig"][c[0], c[1]])
                V(lambda e, c=c: e.memset(SstC[c][:, :], 0.0), [], [SstC[c]])
                V(lambda e, c=c: e.memset(SbfC[c][:, :], 0.0), [], [SbfC[c]])
            orders = {0: list(range(NT)), 1: [1, 0] + list(range(NT - 1, 1, -1))}
            it = 0
            qz = [[P.sb(f"qz{i}_{hh}", [128, 128], BF16) for hh in range(Hc)] for i in range(NB)]
            for i in range(NB):
                for hh in range(Hc):
                    G(lambda e, i=i, hh=hh: e.memset(qz[i][hh][:, :], 0.0), [], [qz[i][hh]])
            for step in range(NT):
                ctxs = []
                for ci, (d, cc) in enumerate(chains):
                    la, ig = laC[(d, cc)], igC[(d, cc)]
                    t = orders[d][step]
                    j = ci
                    tsl = slice(t * 128, (t + 1) * 128)
                    if d == 0:
                        V(lambda e, j=j, tsl=tsl, la=la: e.tensor_tensor_scan(bT[j][:, :], ones_f[:, :], la[:, tsl], 0.0, ALU.mult, ALU.add),
                          [ones_f, la], [bT[j]])
                        tot = bT[j][:, 127:128]
                    else:
                        V(lambda e, j=j, tsl=tsl, la=la: e.tensor_tensor_scan(pfx[j][:, :], ones_f[:, :], la[:, tsl], 0.0, ALU.mult, ALU.add),
                          [ones_f, la], [pfx[j]])
                        V(lambda e, j=j, tsl=tsl, la=la: e.tensor_tensor(bT[j][:, :], la[:, tsl], pfx[j][:, :], ALU.subtract),
                          [la, pfx[j]], [bT[j]])
                        V(lambda e, j=j: e.tensor_scalar(bT[j][:, :], bT[j][:, :], pfx[j][:, 127:128], None, ALU.add),
                          [bT[j], pfx[j]], [bT[j]])
                        tot = bT[j][:, 0:1]
                    A(lambda e, j=j: e.activation(eq[j][:, :], bT[j][:, :], AF.Exp), [bT[j]], [eq[j]])
                    for hh in range(Hc):
                        V(lambda e, j=j, cc=cc, tsl=tsl, hh=hh: e.scalar_tensor_tensor(
                            qz[j][hh][64 * hh:64 * hh + 64, :], qT[64 * hh:64 * hh + 64, cc, tsl], qscale, eq[j][64 * hh:64 * hh + 64, :],
                            ALU.mult, ALU.mult), [qT, eq[j]], [qz[j][hh]])
                    if ml:
                        G(lambda e, j=j, tsl=tsl, ig=ig: e.tensor_tensor(arg[j][:, :], ig[:, tsl], bT[j][:, :], ALU.subtract), [ig, bT[j]], [arg[j]])
                        A(lambda e, j=j: e.activation(ek[j][:, :], arg[j][:, :], AF.Exp), [arg[j]], [ek[j]])
                        A(lambda e, j=j, tot=tot: e.activation(ekh[j][:, :], arg[j][:, :], AF.Exp, bias=tot, scale=1.0), [arg[j], bT[j]], [ekh[j]])
                    else:
                        A(lambda e, j=j: e.activation(ek[j][:, :], bT[j][:, :], AF.Exp, scale=-1.0), [bT[j]], [ek[j]])
                        A(lambda e, j=j, tot=tot: e.activation(ekh[j][:, :], bT[j][:, :], AF.Exp, bias=tot, scale=-1.0), [bT[j]], [ekh[j]])
                    A(lambda e, j=j, tot=tot: e.activation(gam[j][:, :], tot, AF.Exp), [bT[j]], [gam[j]])
                    G(lambda e, j=j, cc=cc, tsl=tsl: e.tensor_tensor(kt_[j][:, :], kT[:, cc, tsl], ek[j][:, :], ALU.mult), [kT, ek[j]], [kt_[j]])
                    G(lambda e, j=j, cc=cc, tsl=tsl: e.tensor_tensor(kh_[j][:, :], kT[:, cc, tsl], ekh[j][:, :], ALU.mult), [kT, ekh[j]], [kh_[j]])
                    pTk = pb[7][:, :].bitcast(BF16)
                    tr(pTk[:, 0:128], kh_[j][:, :], ident_bf[:, :], [kh_[j], ident_bf], [pb[7]])
                    A(lambda e, j=j, pTk=pTk: e.copy(khT[j][:, :], pTk[:, 0:128]), [pb[7]], [khT[j]])
                    for hh in range(Hc):
                        mm(pb[ci][:, hh * 128:(hh + 1) * 128], kt_[j][:, :], qz[j][hh][:, :], hh == 0, hh == Hc - 1,
                           r=[kt_[j], qz[j][hh]], w=[pb[ci]])
                    ctxs.append((d, cc, t, j, ci))
                for (d, cc, t, j, ci) in ctxs:
                    tri = tri_f if d == 0 else tri_b
                    Sbf = SbfC[(d, cc)]
                    Sst = SstC[(d, cc)]
                    hb = 4 + ci % 2
                    V(lambda e, j=j, tri=tri, ci=ci: e.tensor_tensor(
                        PT[j][:, :, :], pb[ci][:, 0:Hc * 128].rearrange("p (h l) -> p h l", h=Hc),
                        tri[:, :].unsqueeze(1).broadcast_to([128, Hc, 128]), ALU.mult), [pb[ci], tri], [PT[j]])
                    for hh in range(Hc):
                        head = Hc * cc + hh
                        hc0 = hh * 128
                        mm(pb[hb][:, hc0:hc0 + dva], PT[j][:, hh, :], Vh[:, t, head, 0:dva], hh == 0, False, r=[PT[j], Vt], w=[pb[hb]])
                        mm(pb[hb][:, hc0:hc0 + dva], qz[j][hh][:, :], Sbf[:, :], False, hh == Hc - 1, r=[qz[j][hh], Sbf], w=[pb[hb]])
                    Hx4 = Hs4 if d == 0 else Hb4
                    hkey = ("Hsum", t) if d == 0 else ("Hsum2", t)
                    if ml:
                        A(lambda e, j=j, hb=hb: e.activation(den[j][:, :, :], pb[hb][:, 0:Hc * 128].rearrange("p (h c) -> p h c", h=Hc)[:, :, 64:65],
                                                             AF.Abs), [pb[hb]], [den[j]])
                        V(lambda e, j=j: e.tensor_scalar_max(den[j][:, :, :], den[j][:, :, :], 1.0), [den[j]], [den[j]])
                        V(lambda e, j=j: e.reciprocal(den[j][:, :, :], den[j][:, :, :]), [den[j]], [den[j]])
                    for hh in range(Hc):
                        head = Hc * cc + hh
                        hc0 = hh * 128
                        if ml:
                            A(lambda e, j=j, hh=hh, hb=hb, hc0=hc0, head=head, t=t, Hx4=Hx4: e.activation(
                                Hx4[:, t, head, :], pb[hb][:, hc0:hc0 + 64], AF.Identity, scale=den[j][:, hh, :]), [pb[hb], den[j]], [hkey])
                        else:
                            A(lambda e, t=t, head=head, hb=hb, hc0=hc0, Hx4=Hx4: e.copy(Hx4[:, t, head, :], pb[hb][:, hc0:hc0 + 64]), [pb[hb]], [hkey])
                    for hh in range(Hc):
                        head = Hc * cc + hh
                        mm(pb[6][dk * hh:dk * (hh + 1), 0:dva], khT[j][:, dk * hh:dk * (hh + 1)], Vh[:, t, head, 0:dva], True, True,
                           r=[khT[j], Vt], w=[pb[6]])
                    V(lambda e, j=j, Sst=Sst: e.scalar_tensor_tensor(Sst[:, :], Sst[:, :], gam[j][:, :], pb[6][:, 0:dva], ALU.mult, ALU.add),
                      [Sst, gam[j], pb[6]], [Sst])
                    A(lambda e, Sst=Sst, Sbf=Sbf: e.copy(Sbf[:, :], Sst[:, :]), [Sst], [Sbf])
            P.label = f"L{L}_P45_{kind}_fin"
            gnm = P.sb("gnm", [128, 64], F32)
            P.dma("sync", gnm[:, :], I["ml_norm" if ml else "gl_norm"][L:L + 1, :].broadcast_to([128, 64]))
            gate = [P.sb(f"gate{i}", [128, 256], F32) for i in range(2)]
            sqh = [P.sb(f"sqh{i}", [128, 256], F32) for i in range(2)]
            ssq = [P.sb(f"ssq{i}", [128, 4], F32) for i in range(2)]
            obm = [P.sb(f"obm{i}", [128, 256], BF16) for i in range(2)]
            mst = [P.sb(f"mstm{i}", [128, NTOK], BF16) for i in range(2)]
            for t in range(NT):
                j = t % 2
                P.dma("sync", gate[j][:, :], S["ml_o" if ml else "gl_r"][t * 128:(t + 1) * 128, :])
                G(lambda e, j=j: e.tensor_tensor(gate[j][:, :].rearrange("p (h d) -> p h d", h=4),
                                                 gate[j][:, :].rearrange("p (h d) -> p h d", h=4),
                                                 gnm[:, :].unsqueeze(1).broadcast_to([128, 4, 64]), ALU.mult), [gate[j], gnm], [gate[j]])
                G(lambda e, t=t: e.tensor_tensor(Hsum[:, t, :], Hsum[:, t, :], Hsum2[:, t, :], ALU.add), [("Hsum", t), ("Hsum2", t)], [("Hsum", t)])
                V(lambda e, j=j, t=t: e.tensor_tensor(sqh[j][:, :], Hsum[:, t, :], Hsum[:, t, :], ALU.mult), [("Hsum", t)], [sqh[j]])
                V(lambda e, j=j: e.tensor_reduce(ssq[j][:, :], sqh[j][:, :].rearrange("p (h d) -> p h d", h=4), AX.X, ALU.add), [sqh[j]], [ssq[j]])
                A(lambda e, j=j: e.activation(ssq[j][:, :], ssq[j][:, :], AF.Sqrt, bias=eps_col[:, :], scale=1.0 / 64.0), [ssq[j], eps_col], [ssq[j]])
                V(lambda e, j=j: e.reciprocal(ssq[j][:, :], ssq[j][:, :]), [ssq[j]], [ssq[j]])
                for hh in range(4):
                    V(lambda e, j=j, t=t, hh=hh: e.scalar_tensor_tensor(obm[j][:, hh * 64:(hh + 1) * 64], Hs4[:, t, hh, :], ssq[j][:, hh:hh + 1],
                                                                        gate[j][:, hh * 64:(hh + 1) * 64], ALU.mult, ALU.mult),
                      [("Hsum", t), ssq[j], gate[j]], [obm[j]])
                pT = pb[5][:, :].bitcast(BF16)
                for c2_ in range(2):
                    tr(pT[:, c2_ * 128:(c2_ + 1) * 128], obm[j][:, c2_ * 128:(c2_ + 1) * 128], ident_bf[:, :], [obm[j], ident_bf], [pb[5]])
                    A(lambda e, c2_=c2_, t=t, pT=pT: e.copy(mst[c2_][:, t * 128:(t + 1) * 128], pT[:, c2_ * 128:(c2_ + 1) * 128]), [pb[5]], [mst[c2_]])
            base = 4 if ml else 6
            for c2_ in range(2):
                P.dma("sync", S["mixT"][base + c2_], mst[c2_][:, :])

        decay_attn("ml")
        if stop_after == "P4":
            return None
        decay_attn("gl")
        if stop_after == "P5":
            return None

        P.label = f"L{L}_P6"
        P.barrier()
        P.release(persist_mark)
        m6 = P.mark()
        mixT = P.sb("mixTs", [128, 8, NTOK], BF16)
        for ch in range(8):
            P.dma("sync", mixT[:, ch, :], S["mixT"][ch])
        wo = P.sb("wo", [128, 8, D], BF16)
        P.dma("gpsimd", wo[:, :, :], I["w_out"][L].rearrange("(k p) n -> p k n", p=128))
        g1bc = P.sb("g1bc", [128, 2, D], F32)
        for r in range(2):
            P.dma("sync", g1bc[:, r, :], S["modrow"][r:r + 1, 2 * D:3 * D].broadcast_to([128, D]))
        lng6 = P.sb("lng", [128, D], F32)
        lnb6 = P.sb("lnb", [128, D], F32)
        P.dma("sync", lng6[:, :], I["ln_mix_g"][L:L + 1, :].broadcast_to([128, D]))
        P.dma("sync", lnb6[:, :], I["ln_mix_b"][L:L + 1, :].broadcast_to([128, D]))
        s2bc = P.sb("s2bc", [128, 2, 2, D], F32)
        for r in range(2):
            P.dma("sync", s2bc[:, 0, r, :], S["modrow"][r:r + 1, 3 * D:4 * D].broadcast_to([128, D]))
            P.dma("sync", s2bc[:, 1, r, :], S["modrow"][r:r + 1, 4 * D:5 * D].broadcast_to([128, D]))
        V(lambda e: e.tensor_scalar_add(s2bc[:, 1, :, :], s2bc[:, 1, :, :], 1.0), [s2bc], [s2bc])
        V(lambda e: e.memset(cntE[:, :], 0.0), [], [cntE])
        fTM = [P.sb(f"fTM{i}", [128, D], BF16) for i in range(3)]
        ftmp = [P.sb(f"ftmp{i}", [128, D], F32) for i in range(3)]
        wr = P.sb("wr", [128, 8, 36], F32)
        P.dma("sync", wr[:, :, 0:4], I["moe_wg"][L].rearrange("(k p) n -> p k n", p=128))
        P.dma("sync", wr[:, :, 4:36], I["moe_we"][L].rearrange("(k p) n -> p k n", p=128))
        xt6v = [P.sb(f"xt6{i}", [128, D], F32) for i in range(3)]
        u6v = [P.sb(f"u6{i}", [128, D], F32) for i in range(3)]
        xn6v = [P.sb(f"xn6{i}", [128, D], F32) for i in range(3)]
        fTf = [P.sb(f"fTf{i}", [128, 8, 128], F32) for i in range(3)]
        stb6 = [(P.sb(f"st6{i}", [128, 2, 6], F32), P.sb(f"mv6{i}", [128, 2], F32), P.sb(f"rs6{i}", [128, 1], F32)) for i in range(3)]
        stc = [(P.sb(f"st7{i}", [128, 2, 6], F32), P.sb(f"mv7{i}", [128, 2], F32), P.sb(f"rs7{i}", [128, 1], F32)) for i in range(3)]
        rt2 = [dict(oh1=P.sb(f"oh1{i}", [128, 32], F32), oh2=P.sb(f"oh2{i}", [128, 32], F32), slot=P.sb(f"slot{i}", [128, 32], F32),
                    dst=P.sb(f"dst{i}", [128, 32], F32), ovm=P.sb(f"ovm{i}", [128, 32], F32), tm=P.sb(f"tm{i}", [128, 32], F32),
                    wvv=P.sb(f"wv{i}", [128, 32], F32), c4=P.sb(f"c4{i}", [128, 4], F32)) for i in range(3)]
        rt = [dict(lg=P.sb(f"lg{i}", [128, 36], F32), s1=P.sb(f"s1{i}", [128, 8], F32), oh=P.sb(f"oh{i}", [128, 4], F32),
                   ml_=P.sb(f"mlg{i}", [128, 32], F32), t32=P.sb(f"t32{i}", [128, 32], F32), ex=P.sb(f"ex{i}", [128, 32], F32),
                   e4=P.sb(f"e4{i}", [128, 4], F32)) for i in range(3)]
        def stA1(t):
            b = t % 3
            r = 1 if t < NCTX_T else 0
            tsl = slice(t * 128, (t + 1) * 128)
            P.dma("sync", xt6v[b][:, :], x_cur[tsl, :])
            for half in range(2):
                for k in range(8):
                    mm(pb[half][:, :], mixT[:, k, tsl], wo[:, k, half * 512:(half + 1) * 512], k == 0, k == 7, r=[mixT, wo], w=[pb[half]])
                V(lambda e, b=b, half=half, r=r: e.tensor_tensor(u6v[b][:, half * 512:(half + 1) * 512], pb[half][:, :],
                                                                  g1bc[:, r, half * 512:(half + 1) * 512], ALU.mult), [pb[half], g1bc], [u6v[b]])
                if debug:
                    pass
            V(lambda e, b=b: e.scalar_tensor_tensor(u6v[b][:, :], xt6v[b][:, :], DN_ALPHA, u6v[b][:, :], ALU.mult, ALU.add), [xt6v[b], u6v[b]], [u6v[b]])
            mean, rstd = ln_stats(u6v[b], stb6[b])
            V(lambda e, b=b, mean=mean, rstd=rstd: e.tensor_scalar(u6v[b][:, :], u6v[b][:, :], mean, rstd, ALU.subtract, ALU.mult),
              [u6v[b], stb6[b][1], stb6[b][2]], [u6v[b]])
            G(lambda e, b=b: e.tensor_tensor(u6v[b][:, :], u6v[b][:, :], lng6[:, :], ALU.mult), [u6v[b], lng6], [u6v[b]])
            G(lambda e, b=b: e.tensor_tensor(u6v[b][:, :], u6v[b][:, :], lnb6[:, :], ALU.add), [u6v[b], lnb6], [u6v[b]])
            P.dma("sync", S["x1"][tsl, :], u6v[b][:, :])

        def stA2(t):
            b = t % 3
            r = 1 if t < NCTX_T else 0
            tsl = slice(t * 128, (t + 1) * 128)
            mean2, rstd2 = ln_stats(u6v[b], stc[b])
            V(lambda e, b=b, mean2=mean2, rstd2=rstd2: e.tensor_scalar(xn6v[b][:, :], u6v[b][:, :], mean2, rstd2, ALU.subtract, ALU.mult),
              [u6v[b], stc[b][1], stc[b][2]], [xn6v[b]])
            for ch in range(8):
                bank = 2 + ch // 4
                tr(pb[bank][:, (ch % 4) * 128:(ch % 4 + 1) * 128], xn6v[b][:, ch * 128:(ch + 1) * 128], ident_f[:, :], [xn6v[b], ident_f], [pb[bank]])
            for ch in range(8):
                bank = 2 + ch // 4
                i_ = pb[bank][:, (ch % 4) * 128:(ch % 4 + 1) * 128]
                A(lambda e, b=b, ch=ch, i_=i_, r=r: e.activation(fTf[b][:, ch, :], i_, AF.Identity, bias=modcol[:, 24 + ch, r:r + 1],
                                                                 scale=modcol[:, 32 + ch, r:r + 1]), [pb[bank], modcol], [fTf[b]])
            G(lambda e, b=b, r=r: e.tensor_tensor(ftmp[b][:, :], xn6v[b][:, :], s2bc[:, 1, r, :], ALU.mult), [xn6v[b], s2bc], [ftmp[b]])
            G(lambda e, b=b, r=r: e.tensor_tensor(fTM[b][:, :], ftmp[b][:, :], s2bc[:, 0, r, :], ALU.add), [ftmp[b], s2bc], [fTM[b]])

        def stB(t):
            b = t % 3
            r = 1 if t < NCTX_T else 0
            tsl = slice(t * 128, (t + 1) * 128)
            for k in range(8):
                mm(pb[4][:, 0:36], fTf[b][:, k, :], wr[:, k, :], k == 0, k == 7, r=[fTf[b], wr], w=[pb[4]])
            R_ = rt[b]
            lg, s1, oh, mlg, t32, ex, e4 = R_["lg"], R_["s1"], R_["oh"], R_["ml_"], R_["t32"], R_["ex"], R_["e4"]
            V(lambda e, lg=lg: e.tensor_copy(lg[:, :], pb[4][:, 0:36]), [pb[4]], [lg])
            V(lambda e, lg=lg, s1=s1: e.tensor_reduce(s1[:, 0:1], lg[:, 0:4], AX.X, ALU.max), [lg], [s1])
            V(lambda e, s1=s1: e.tensor_scalar_mul(s1[:, 1:2], s1[:, 0:1], -1.0), [s1], [s1])
            A(lambda e, lg=lg, s1=s1, e4=e4: e.activation(e4[:, :], lg[:, 0:4], AF.Exp, bias=s1[:, 1:2], scale=1.0), [lg, s1], [e4])
            V(lambda e, s1=s1, e4=e4: e.tensor_reduce(s1[:, 2:3], e4[:, :], AX.X, ALU.add), [e4], [s1])
            V(lambda e, s1=s1: e.reciprocal(s1[:, 2:3], s1[:, 2:3]), [s1], [s1])
            V(lambda e, lg=lg, s1=s1, oh=oh: e.tensor_scalar(oh[:, :], lg[:, 0:4], s1[:, 0:1], None, ALU.is_ge), [lg, s1], [oh])
            V(lambda e, oh=oh: e.tensor_scalar(oh[:, :], oh[:, :], -1.0, 1e30, ALU.add, ALU.mult), [oh], [oh])
            for g in range(4):
                V(lambda e, g=g, lg=lg, oh=oh, mlg=mlg: e.tensor_scalar(mlg[:, g * 8:(g + 1) * 8], lg[:, 4 + g * 8:4 + (g + 1) * 8],
                                                                       oh[:, g:g + 1], None, ALU.add), [lg, oh], [mlg])
            V(lambda e, mlg=mlg, s1=s1: e.tensor_reduce(s1[:, 3:4], mlg[:, :], AX.X, ALU.max), [mlg], [s1])
            V(lambda e, mlg=mlg, s1=s1, t32=t32: e.tensor_scalar(t32[:, :], mlg[:, :], s1[:, 3:4], -1e30, ALU.is_ge, ALU.mult), [mlg, s1], [t32])
            V(lambda e, mlg=mlg, t32=t32: e.tensor_tensor(t32[:, :], t32[:, :], mlg[:, :], ALU.add), [mlg, t32], [t32])
            V(lambda e, t32=t32, s1=s1: e.tensor_reduce(s1[:, 4:5], t32[:, :], AX.X, ALU.max), [t32], [s1])
            V(lambda e, mlg=mlg, s1=s1, t32=t32: e.tensor_scalar(t32[:, :], mlg[:, :], s1[:, 4:5], None, ALU.is_ge), [mlg, s1], [t32])
            V(lambda e, s1=s1: e.tensor_scalar_mul(s1[:, 5:6], s1[:, 3:4], -1.0), [s1], [s1])
            A(lambda e, mlg=mlg, s1=s1, ex=ex: e.activation(ex[:, :], mlg[:, :], AF.Exp, bias=s1[:, 5:6], scale=1.0), [mlg, s1], [ex])
            V(lambda e, ex=ex, t32=t32: e.tensor_tensor(ex[:, :], ex[:, :], t32[:, :], ALU.mult), [ex, t32], [ex])
            V(lambda e, ex=ex, s1=s1: e.tensor_reduce(s1[:, 6:7], ex[:, :], AX.X, ALU.add), [ex], [s1])
            V(lambda e, s1=s1: e.reciprocal(s1[:, 6:7], s1[:, 6:7]), [s1], [s1])
            V(lambda e, s1=s1: e.tensor_tensor(s1[:, 6:7], s1[:, 6:7], s1[:, 2:3], ALU.mult), [s1], [s1])
            V(lambda e, ex=ex, s1=s1, t=t: e.tensor_scalar(Wt[:, t, :], ex[:, :], s1[:, 6:7], None, ALU.mult), [ex, s1], [Wt])
            Q_ = rt2[b]
            oh1, oh2, slot, dst, ovm, tm, wvv, c4 = Q_["oh1"], Q_["oh2"], Q_["slot"], Q_["dst"], Q_["ovm"], Q_["tm"], Q_["wvv"], Q_["c4"]
            V(lambda e, mlg=mlg, s1=s1, oh1=oh1: e.tensor_scalar(oh1[:, :], mlg[:, :], s1[:, 3:4], None, ALU.is_ge), [mlg, s1], [oh1])
            V(lambda e, t32=t32, oh1=oh1, oh2=oh2: e.tensor_tensor(oh2[:, :], t32[:, :], oh1[:, :], ALU.subtract), [t32, oh1], [oh2])
            mm(pb[5][:, 0:32], tri_f[:, :], t32[:, :], True, True, r=[tri_f, t32], w=[pb[5]])
            mm(pb[6][:, 0:32], ones_f[:, :], t32[:, :], True, True, r=[ones_f, t32], w=[pb[6]])
            V(lambda e, slot=slot, t32=t32: e.tensor_tensor(slot[:, :], pb[5][:, 0:32], t32[:, :], ALU.subtract), [pb[5], t32], [slot])
            V(lambda e, slot=slot: e.tensor_tensor(slot[:, :], slot[:, :], cntE[:, :], ALU.add), [slot, cntE], [slot])
            V(lambda e: e.tensor_tensor(cntE[:, :], cntE[:, :], pb[6][:, 0:32], ALU.add), [cntE, pb[6]], [cntE])
            V(lambda e, slot=slot, ovm=ovm: e.tensor_scalar(ovm[:, :], slot[:, :], float(CAP), None, ALU.is_ge), [slot], [ovm])
            V(lambda e, slot=slot, dst=dst: e.tensor_tensor(dst[:, :], slot[:, :], ecolC[:, :], ALU.add), [slot, ecolC], [dst])
            V(lambda e, dst=dst, ovm=ovm: e.scalar_tensor_tensor(dst[:, :], ovm[:, :], 1.0e6, dst[:, :], ALU.mult, ALU.add), [ovm, dst], [dst])
            V(lambda e, t=t, ovm=ovm, wvv=wvv: e.tensor_tensor(wvv[:, :], Wt[:, t, :], ovm[:, :], ALU.mult), [Wt, ovm], [wvv])
            V(lambda e, t=t, wvv=wvv: e.tensor_tensor(wvv[:, :], Wt[:, t, :], wvv[:, :], ALU.subtract), [Wt, wvv], [wvv])
            for k_, oh in ((0, oh1), (1, oh2)):
                V(lambda e, oh=oh, dst=dst, tm=tm: e.tensor_tensor(tm[:, :], oh[:, :], dst[:, :], ALU.mult), [oh, dst], [tm])
                V(lambda e, tm=tm, c4=c4, k_=k_: e.tensor_reduce(c4[:, k_:k_ + 1], tm[:, :], AX.X, ALU.add), [tm], [c4])
                V(lambda e, oh=oh, wvv=wvv, tm=tm: e.tensor_tensor(tm[:, :], oh[:, :], wvv[:, :], ALU.mult), [oh, wvv], [tm])
                V(lambda e, tm=tm, t=t, k_=k_: e.tensor_reduce(wsel[:, t, k_:k_ + 1], tm[:, :], AX.X, ALU.add), [tm], [wsel])
            V(lambda e, c4=c4, t=t: e.tensor_copy(idxs[:, t, :], c4[:, 0:2]), [c4], [idxs])
            for k_ in range(2):
                P.ops.append(dict(eng="gpsimd", dma=True, bar=None, r=P._keys([fTM[b], idxs]), w=[("xslots", t, k_)],
                                  fn=lambda e, b=b, t=t, k_=k_: e.indirect_dma_start(
                                      out=S["xslots"][:, :], out_offset=bass.IndirectOffsetOnAxis(idxs[:, t, k_:k_ + 1], 0),
                                      in_=fTM[b][:, :], in_offset=None, bounds_check=get_bc(e), oob_is_err=False)))
        for i_ in range(NT + 2):
            if i_ < NT:
                stA1(i_)
            if 0 <= i_ - 1 < NT:
                stA2(i_ - 1)
            if 0 <= i_ - 2 < NT:
                stB(i_ - 2)
        if debug:
            for t in range(NT):
                P.dma("sync", S["dbg_W"][t * 128:(t + 1) * 128, :], Wt[:, t, :])
        if stop_after == "P6":
            return None

        P.label = f"L{L}_P7"
        P.barrier()
        P.release(m6)
        yacc = P.sb("yacc", [128, NT, D], F32)
        after_yacc = P.mark()
        w1b = [P.sb(f"w1b{i}", [128, 8, 512], BF16) for i in range(2)]
        w3b = [P.sb(f"w3b{i}", [128, 8, 512], BF16) for i in range(2)]
        w2b = [P.sb(f"w2b{i}", [128, 4, D], BF16) for i in range(2)]
        xs = [P.sb(f"xs{i}", [128, NST, D], BF16) for i in range(2)]
        xT = [P.sb(f"xTs{i}", [128, 8, CAP], BF16) for i in range(2)]
        gTb = [P.sb(f"gTb{i}", [128, 4, CAP], BF16) for i in range(2)]
        s1b = [P.sb(f"s1b{i}", [128, 512], F32) for i in range(2)]
        ysb = [P.sb(f"ysb{i}", [128, D], BF16) for i in range(2)]
        SB = [(0, CAP // 2), (CAP // 2, CAP // 2)] if CAP > 512 else [(0, CAP)]
        yi = 0

        def moe_load(ex_):
            eb = ex_ % 2
            P.dma("gpsimd", w1b[eb][:, :, :], I["moe_w1"][L, ex_].rearrange("(k p) n -> p k n", p=128))
            P.dma("gpsimd", w3b[eb][:, :, :], I["moe_w3"][L, ex_].rearrange("(k p) n -> p k n", p=128))
            P.dma("gpsimd", w2b[eb][:, :, :], I["moe_w2"][L, ex_].rearrange("(k p) n -> p k n", p=128))
            P.dma("sync", xs[eb][:, :, :], S["xslots"][ex_ * CAP:(ex_ + 1) * CAP, :].rearrange("(s p) d -> p s d", p=128),
                  reads=[("xslots", t_, k__) for t_ in range(NT) for k__ in range(2)])

        def moe_transposes(ex_):
            eb = ex_ % 2
            for st in range(NST):
                bank = 6 + st % 2
                pT = pb[bank][:, :].bitcast(BF16)
                for k in range(8):
                    tr(pT[:, k * 128:(k + 1) * 128], xs[eb][:, st, k * 128:(k + 1) * 128], ident_bf[:, :], [xs[eb], ident_bf], [pb[bank]])
                if st % 2 == 0:
                    A(lambda e, eb=eb, st=st, pT=pT: e.copy(xT[eb][:, :, st * 128:(st + 1) * 128], pT[:, :].rearrange("p (k s) -> p k s", k=8)),
                      [pb[bank]], [xT[eb]])
                else:
                    V(lambda e, eb=eb, st=st, pT=pT: e.tensor_copy(xT[eb][:, :, st * 128:(st + 1) * 128], pT[:, :].rearrange("p (k s) -> p k s", k=8)),
                      [pb[bank]], [xT[eb]])

        moe_load(0)
        moe_transposes(0)
        for ex_ in range(NE):
            eb = ex_ % 2
            if ex_ + 1 < NE:
                moe_load(ex_ + 1)
            gT = gTb[eb]
            for (t0, tn) in SB:
                for hc in range(4):
                    b1, b3 = (hc % 2) * 2, (hc % 2) * 2 + 1
                    for k in range(8):
                        mm(pb[b1][:, 0:tn], w1b[eb][:, k, hc * 128:(hc + 1) * 128], xT[eb][:, k, t0:t0 + tn], k == 0, k == 7,
                           r=[w1b[eb], xT[eb]], w=[pb[b1]])
                    for k in range(8):
                        mm(pb[b3][:, 0:tn], w3b[eb][:, k, hc * 128:(hc + 1) * 128], xT[eb][:, k, t0:t0 + tn], k == 0, k == 7,
                           r=[w3b[eb], xT[eb]], w=[pb[b3]])
                    sb_ = s1b[hc % 2]
                    A(lambda e, sb_=sb_, b1=b1, tn=tn: e.activation(sb_[:, 0:tn], pb[b1][:, 0:tn], AF.Silu), [pb[b1]], [sb_])
                    V(lambda e, sb_=sb_, b3=b3, tn=tn, gT=gT, hc=hc, t0=t0: e.tensor_tensor(gT[:, hc, t0:t0 + tn], sb_[:, 0:tn], pb[b3][:, 0:tn], ALU.mult),
                      [sb_, pb[b3]], [gT])
            if ex_ + 1 < NE:
                moe_transposes(ex_ + 1)
            for st in range(NST):
                yb = ysb[yi % 2]
                yi += 1
                for half in range(2):
                    bank = 4 + half
                    for hc in range(4):
                        mm(pb[bank][:, :], gT[:, hc, st * 128:(st + 1) * 128], w2b[eb][:, hc, half * 512:(half + 1) * 512], hc == 0, hc == 3,
                           r=[gT, w2b[eb]], w=[pb[bank]])
                    if half == 0:
                        A(lambda e, yb=yb, bank=bank: e.copy(yb[:, 0:512], pb[bank][:, :]), [pb[bank]], [yb])
                    else:
                        V(lambda e, yb=yb, bank=bank: e.tensor_copy(yb[:, 512:1024], pb[bank][:, :]), [pb[bank]], [yb])
                r0 = ex_ * CAP + st * 128
                P.dma("sync", S["yslots"][r0:r0 + 128, :], yb[:, :], writes=[("yslots", ex_, st)])
        P.label = f"L{L}_P7_combine"
        yg = [P.sb(f"yg{i}", [128, D], BF16) for i in range(4)]
        for i in range(4):
            V(lambda e, i=i: e.memset(yg[i][:, :], 0.0), [], [yg[i]])
        for t in range(NT):
            g0, g1_ = yg[(2 * t) % 4], yg[(2 * t + 1) % 4]
            for k_, gt in ((0, g0), (1, g1_)):
                P.ops.append(dict(eng="gpsimd", dma=True, bar=None, r=[("yslots", e_, s_) for e_ in range(NE) for s_ in range(NST)] + P._keys([idxs]), w=P._keys([gt]),
                                  fn=lambda e, t=t, k_=k_, gt=gt: e.indirect_dma_start(
                                      out=gt[:, :], out_offset=None, in_=S["yslots"][:, :],
                                      in_offset=bass.IndirectOffsetOnAxis(idxs[:, t, k_:k_ + 1], 0),
                                      bounds_check=get_bc(e), oob_is_err=False)))
            V(lambda e, t=t, g0=g0: e.tensor_scalar(yacc[:, t, :], g0[:, :], wsel[:, t, 0:1], None, ALU.mult), [g0, wsel], [("yacc", t)])
            V(lambda e, t=t, g1_=g1_: e.scalar_tensor_tensor(yacc[:, t, :], g1_[:, :], wsel[:, t, 1:2], yacc[:, t, :], ALU.mult, ALU.add),
              [g1_, wsel, ("yacc", t)], [("yacc", t)])
        if debug:
            for t in range(NT):
                P.dma("sync", S["dbg_moe"][t * 128:(t + 1) * 128, :], yacc[:, t, :], reads=[("yacc", t)])
        if stop_after == "P7":
            return None

        P.label = f"L{L}_P8"
        P.barrier()
        P.release(after_yacc)
        g2bc = P.sb("g2bc", [128, 2, D], F32)
        for r in range(2):
            P.dma("sync", g2bc[:, r, :], S["modrow"][r:r + 1, 5 * D:6 * D].broadcast_to([128, D]))
        lng8v = P.sb("lng8", [128, D], F32)
        lnb8v = P.sb("lnb8", [128, D], F32)
        P.dma("sync", lng8v[:, :], I["ln_ffn_g"][L:L + 1, :].broadcast_to([128, D]))
        P.dma("sync", lnb8v[:, :], I["ln_ffn_b"][L:L + 1, :].broadcast_to([128, D]))
        xt8v = [P.sb(f"xt8{i}", [128, D], F32) for i in range(4)]
        u8v = [P.sb(f"u8{i}", [128, D], F32) for i in range(4)]
        stb8 = [(P.sb(f"st8{i}", [128, 2, 6], F32), P.sb(f"mv8{i}", [128, 2], F32), P.sb(f"rs8{i}", [128, 1], F32)) for i in range(4)]
        tiles8 = [t for t in range(NT) if not (last and t < NCTX_T)]

        def p8_s1(t):
            b = t % 4
            r = 1 if t < NCTX_T else 0
            st, mv, rstd = stb8[b]
            xb, ub = xt8v[b], u8v[b]
            P.dma("sync", xb[:, :], S["x1"][t * 128:(t + 1) * 128, :])
            V(lambda e: e.tensor_tensor(ub[:, :], yacc[:, t, :], g2bc[:, r, :], ALU.mult), [("yacc", t), g2bc], [ub])
            V(lambda e: e.scalar_tensor_tensor(ub[:, :], xb[:, :], DN_ALPHA, ub[:, :], ALU.mult, ALU.add), [xb, ub], [ub])
            V(lambda e: e.bn_stats(st[:, 0, :], ub[:, 0:512]), [ub], [st])
            V(lambda e: e.bn_stats(st[:, 1, :], ub[:, 512:1024]), [ub], [st])
            V(lambda e: e.bn_aggr(mv[:, :], st[:, :, :].rearrange("p a b -> p (a b)")), [st], [mv])

        def p8_s2(t):
            b = t % 4
            st, mv, rstd = stb8[b]
            ub = u8v[b]
            A(lambda e: e.activation(rstd[:, :], mv[:, 1:2], AF.Sqrt, bias=eps_col[:, :], scale=1.0), [mv, eps_col], [rstd])
            V(lambda e: e.reciprocal(rstd[:, :], rstd[:, :]), [rstd], [rstd])
            V(lambda e: e.tensor_scalar(ub[:, :], ub[:, :], mv[:, 0:1], rstd[:, 0:1], ALU.subtract, ALU.mult), [ub, mv, rstd], [ub])

        def p8_s3(t):
            b = t % 4
            ub = u8v[b]
            G(lambda e: e.tensor_tensor(ub[:, :], ub[:, :], lng8v[:, :], ALU.mult), [ub, lng8v], [ub])
            G(lambda e: e.tensor_tensor(ub[:, :], ub[:, :], lnb8v[:, :], ALU.add), [ub, lnb8v], [ub])
            if last:
                P.dma("sync", out_d[(t - NCTX_T) * 128:(t - NCTX_T + 1) * 128, :], ub[:, :])
            else:
                P.dma("sync", S["xnext"][t * 128:(t + 1) * 128, :], ub[:, :])

        n8 = len(tiles8)
        for i_ in range(n8 + 2):
            if i_ < n8:
                p8_s1(tiles8[i_])
            if 0 <= i_ - 1 < n8:
                p8_s2(tiles8[i_ - 1])
            if 0 <= i_ - 2 < n8:
                p8_s3(tiles8[i_ - 2])
        return S["xnext"]

    x_cur = I["xin"]
    for L_ in range(n_layers):
        x_cur = layer(L_, x_cur)
        if x_cur is None:
            break

    stats = P.emit()
    global _LAST_VLABELS
    _LAST_VLABELS = P.vlabels
    return nc, stats, consts, list(S.keys())


_CACHE = {}


def make_in_maps(inputs, consts):
    x = np.asarray(inputs["x"], np.float32)
    ctx = np.asarray(inputs["ctx"], np.float32)
    c = np.asarray(inputs["c"], np.float32)
    c_ctx = np.asarray(inputs["c_ctx"], np.float32)
    shared = {k: np.ascontiguousarray(np.asarray(inputs[k], np.float32)) for k in IN_SHAPES if k not in ("xin", "c2")}
    shared.update(consts)
    maps = []
    for b in range(x.shape[0]):
        m = dict(shared)
        m["xin"] = np.ascontiguousarray(np.concatenate([ctx[b], x[b]], axis=0))
        m["c2"] = np.ascontiguousarray(np.stack([c[b], c_ctx], axis=1))
        maps.append(m)
    return maps


def kernel(**inputs):
    if "nc" not in _CACHE:
        nc, stats, consts, _ = build(debug=False)
        _CACHE["nc"] = nc
        _CACHE["consts"] = consts
    nc = _CACHE["nc"]
    maps = make_in_maps(inputs, _CACHE["consts"])
    res = run_bass_kernel_spmd(nc, maps, core_ids=list(range(8)))
    out = np.stack([np.asarray(r["out"], np.float32) for r in res.results], axis=0)
    return out
```

```python
import math
import contextlib
import numpy as np
import ml_dtypes
import concourse.bass as bass
import concourse.mybir as mybir
from concourse.bass_utils import run_bass_kernel_spmd

F32 = mybir.dt.float32
BF16 = mybir.dt.bfloat16
ALU = mybir.AluOpType
AF = mybir.ActivationFunctionType
AX = mybir.AxisListType

D = 1024
NTOK = 2304
NT = 18
NCTX_T = 2
DEPTH = 2
IN_W = 3376
LN_EPS = 1e-6
DN_ALPHA = (2 * DEPTH) ** 0.25
NE = 32
CAP = 640
NST = CAP // 128
U32 = mybir.dt.uint32
TB = [(0, 512), (512, 512), (1024, 512), (1536, 512), (2048, 256)]

COMPUTE = ("tensor", "vector", "scalar", "gpsimd")
ENGS = ("tensor", "vector", "scalar", "gpsimd", "sync")
NDMASEM = 12


class Prog:
    def __init__(self, nc):
        self.nc = nc
        self.ops = []
        self.sb_base = 16512
        self.sb_top = 16512
        self.sb_limit = 229376 - 64
        self.uid = 0
        self.psum_names = set()
        self.label = ""

    def mark(self):
        return self.sb_top

    def release(self, m):
        self.sb_top = m

    def sb(self, name, shape, dt):
        nbytes = int(np.prod(shape[1:])) * (2 if dt == BF16 else 4)
        nbytes = (nbytes + 63) // 64 * 64
        off = self.sb_top
        assert off + nbytes <= self.sb_limit, f"SBUF overflow {name} {off}+{nbytes}"
        self.sb_top = off + nbytes
        self.uid += 1
        return self.nc.alloc_sbuf_tensor_at(f"{name}_{self.uid}", list(shape), dt, offset=off)

    @staticmethod
    def _keys(lst):
        out = []
        for a in lst:
            if a is None:
                continue
            if isinstance(a, (str, tuple)):
                out.append(a)
            else:
                t = a.tensor if hasattr(a, "tensor") else a
                out.append(t.name)
        return out

    def op(self, eng, fn, reads=(), writes=()):
        self.ops.append(dict(eng=eng, fn=fn, r=self._keys(reads), w=self._keys(writes), dma=False, bar=None, lab=self.label))

    def dma(self, eng, out, in_, reads=None, writes=None, **kw):
        r = self._keys(reads if reads is not None else [in_])
        w = self._keys(writes if writes is not None else [out])
        self.ops.append(dict(eng=eng, fn=lambda e: e.dma_start(out=out, in_=in_, **kw), r=r, w=w, dma=True, bar=None))

    def barrier(self):
        for e in ENGS:
            self.ops.append(dict(eng=e, fn=None, r=[], w=[], dma=False, bar=True))

    def emit(self):
        nc = self.nc
        ops = self.ops
        n = len(ops)
        last_w = {}
        readers = {}
        deps = [None] * n
        pending_dma = []
        last_real = {}
        for i, o in enumerate(ops):
            dd = {}
            if o["bar"]:
                for j in pending_dma:
                    dd[j] = True
                for e2 in ENGS:
                    if e2 != o["eng"] and last_real.get(e2) is not None:
                        dd[last_real[e2]] = True
                if o["eng"] == ENGS[-1]:
                    pending_dma = []
                deps[i] = sorted(dd)
                continue
            d = set()
            for k in o["r"]:
                if k in last_w:
                    d.add((last_w[k], "raw"))
                if k in self.psum_names:
                    for j in readers.get(k, ()):
                        if ops[j]["eng"] != o["eng"]:
                            d.add((j, "rar"))
            for k in o["w"]:
                if k in last_w:
                    d.add((last_w[k], "waw"))
                for j in readers.get(k, ()):
                    d.add((j, "war"))
            for k in o["r"]:
                readers.setdefault(k, []).append(i)
            for k in o["w"]:
                last_w[k] = i
                readers[k] = []
            for j, kind in d:
                if j == i:
                    continue
                oj = ops[j]
                if (not oj["dma"]) and (not o["dma"]) and oj["eng"] == o["eng"]:
                    if o["eng"] == "tensor":
                        continue
                dd[j] = True
            deps[i] = sorted(dd)
            if o["dma"]:
                pending_dma.append(i)
            else:
                last_real[o["eng"]] = i
        signal = [False] * n
        for i in range(n):
            for j in deps[i]:
                signal[j] = True
        cnt = {e: 0 for e in ENGS}
        dcnt = {e: 0 for e in ENGS}
        ev = [None] * n
        for i, o in enumerate(ops):
            e = o["eng"]
            if o["dma"]:
                k = dcnt[e] % NDMASEM
                m = dcnt[e] // NDMASEM + 1
                dcnt[e] += 1
                ev[i] = (("d", e, k), 16 * m)
            elif signal[i]:
                cnt[e] += 1
                ev[i] = (("c", e), cnt[e])
        self.stats = dict(n=n, sig=dict(cnt), dma=dict(dcnt))
        self.vlabels = [o.get("lab", "") for o in ops if o["eng"] == "vector" and o["fn"] is not None and not o["dma"]]
        semkeys = sorted(set(v[0] for v in ev if v is not None), key=str)
        with contextlib.ExitStack() as st:
            sems = {}
            for sk in semkeys:
                sems[sk] = st.enter_context(nc.semaphore("s_" + "_".join(str(x) for x in sk)))
            block = st.enter_context(nc.Block())
            per = {e: [] for e in ENGS}
            for i, o in enumerate(ops):
                per[o["eng"]].append(i)
            final_waits = {}
            for i, o in enumerate(ops):
                if o["dma"]:
                    final_waits[ev[i][0]] = max(final_waits.get(ev[i][0], 0), ev[i][1])

            def run_engine(ename, eobj):
                waited = {}
                for i in per[ename]:
                    o = ops[i]
                    need = {}
                    for j in deps[i]:
                        sk, val = ev[j]
                        need[sk] = max(need.get(sk, 0), val)
                    if o["dma"]:
                        sk, val = ev[i]
                        if val > 16:
                            need[sk] = max(need.get(sk, 0), val - 16)
                    for sk, val in need.items():
                        if waited.get(sk, 0) >= val:
                            continue
                        eobj.wait_ge(sems[sk], val)
                        waited[sk] = val
                    if o["fn"] is None:
                        continue
                    ins = o["fn"](eobj)
                    if ev[i] is not None:
                        sk, val = ev[i]
                        ins.then_inc(sems[sk], 16 if o["dma"] else 1)
                if ename == "sync":
                    for sk, val in final_waits.items():
                        if waited.get(sk, 0) < val:
                            eobj.wait_ge(sems[sk], val)
                    for e2 in COMPUTE:
                        if cnt[e2] > 0:
                            eobj.wait_ge(sems[("c", e2)], cnt[e2])

            block.tensor(lambda e: run_engine("tensor", e))
            block.vector(lambda e: run_engine("vector", e))
            block.scalar(lambda e: run_engine("scalar", e))
            block.gpsimd(lambda e: run_engine("gpsimd", e))
            block.sync(lambda e: run_engine("sync", e))
        return self.stats


def make_consts():
    c = {}
    c["ident_bf"] = np.eye(128, dtype=np.float32).astype(ml_dtypes.bfloat16)
    c["ident_f"] = np.eye(128, dtype=np.float32)
    s = np.arange(128)[:, None]
    l = np.arange(128)[None, :]
    c["tri_f"] = (s <= l).astype(np.float32)
    c["tri_b"] = (s >= l).astype(np.float32)
    n_freq = 16
    inv_freq = (10000.0 ** (-np.arange(n_freq, dtype=np.float32) / n_freq)).astype(np.float32)
    t = np.arange(2048)
    row = (t // 64).astype(np.float32)
    col = (t % 64).astype(np.float32)
    ang_r = row[:, None] * inv_freq
    ang_c = col[:, None] * inv_freq
    ang = np.concatenate([ang_r, ang_r, ang_c, ang_c], axis=-1).astype(np.float32)
    cos = np.cos(ang).astype(np.float32)
    sin = np.sin(ang).astype(np.float32)
    sgn = np.concatenate([-np.ones(16), np.ones(16), -np.ones(16), np.ones(16)]).astype(np.float32)
    cosT = np.ones((128, NTOK), np.float32)
    sinT = np.zeros((128, NTOK), np.float32)
    for m in range(2):
        cosT[64 * m:64 * m + 64, 256:] = cos.T
        sinT[64 * m:64 * m + 64, 256:] = (sin * sgn[None, :]).T
    c["cosT"] = cosT
    c["sinT"] = sinT
    c["ecolC"] = np.tile((np.arange(NE, dtype=np.float32) * CAP)[None, :], (128, 1)).astype(np.float32)
    return c


CONST_DT = {"ident_bf": BF16, "ident_f": F32, "tri_f": F32, "tri_b": F32, "cosT": F32, "sinT": F32, "ecolC": F32}

IN_SHAPES = {
    "xin": ([NTOK, D], F32), "c2": ([D, 2], F32),
    "w_ada": ([DEPTH, D, 6 * D], F32), "b_ada": ([DEPTH, 6 * D], F32), "w_in": ([DEPTH, D, IN_W], F32),
    "da_lambda": ([DEPTH, 4, 64], F32), "da_norm": ([DEPTH, 128], F32),
    "ml_conv_w": ([DEPTH, 3, 512], F32), "ml_conv_b": ([DEPTH, 512], F32),
    "ml_ib": ([DEPTH, 2, 4], F32), "ml_fb": ([DEPTH, 2, 4], F32), "ml_norm": ([DEPTH, 64], F32),
    "gl_wa": ([DEPTH, 2, 16, 128], F32), "gl_ba": ([DEPTH, 2, 128], F32), "gl_norm": ([DEPTH, 64], F32),
    "w_out": ([DEPTH, D, D], F32),
    "ln_mix_g": ([DEPTH, D], F32), "ln_mix_b": ([DEPTH, D], F32),
    "ln_ffn_g": ([DEPTH, D], F32), "ln_ffn_b": ([DEPTH, D], F32),
    "moe_wg": ([DEPTH, D, 4], F32), "moe_we": ([DEPTH, D, 32], F32),
    "moe_w1": ([DEPTH, NE, D, 512], F32), "moe_w3": ([DEPTH, NE, D, 512], F32), "moe_w2": ([DEPTH, NE, 512, D], F32),
}


def build(debug=False, n_layers=DEPTH, stop_after=None):
    nc = bass.Bass("TRN2", target_bir_lowering=False)
    P = Prog(nc)
    I = {}
    for k, (shp, dt) in IN_SHAPES.items():
        I[k] = nc.dram_tensor(k, shp, dt, kind="ExternalInput")
    consts = make_consts()
    for k, v in consts.items():
        I[k] = nc.dram_tensor(k, list(v.shape), CONST_DT[k], kind="ExternalInput")
    out_d = nc.dram_tensor("out", [2048, D], F32, kind="ExternalOutput")
    skind = "ExternalOutput" if debug else "Internal"
    S = {}

    def scratch(name, shape, dt):
        S[name] = nc.dram_tensor(name, list(shape), dt, kind=skind)
        return S[name]

    scratch("modrow", [2, 6 * D], F32)
    scratch("da_qk", [8, 128, NTOK], BF16)
    scratch("da_v", [NTOK, 4 * 130], BF16)
    scratch("ml_qk", [4, 128, NTOK], BF16)
    scratch("ml_v", [NTOK, 4 * 66], BF16)
    scratch("ml_o", [NTOK, 256], F32)
    scratch("ml_la", [2, 2, 128, NTOK], F32)
    scratch("ml_ig", [2, 2, 128, NTOK], F32)
    scratch("gl_qk", [4, 128, NTOK], BF16)
    scratch("gl_v", [NTOK, 256], BF16)
    scratch("gl_r", [NTOK, 256], F32)
    scratch("gl_la", [2, 2, 128, NTOK], F32)
    scratch("mixT", [8, 128, NTOK], BF16)
    scratch("x1", [NTOK, D], F32)
    scratch("xnext", [NTOK, D], F32)
    scratch("xslots", [NE * CAP, D], BF16)
    scratch("yslots", [NE * CAP, D], BF16)
    if debug:
        scratch("dbg_hT", [8, 128, NTOK], BF16)
        scratch("dbg_y", [NTOK, D], F32)
        scratch("dbg_fT", [8, 128, NTOK], BF16)
        scratch("dbg_W", [NTOK, 32], F32)
        scratch("dbg_moe", [NTOK, D], F32)

    pb = [nc.alloc_psum_tensor(f"pb{i}", [128, 512], F32) for i in range(8)]
    P.psum_names = set(t.name for t in pb)

    ident_bf = P.sb("ident_bf", [128, 128], BF16)
    ident_f = P.sb("ident_f", [128, 128], F32)
    tri_f = P.sb("tri_f", [128, 128], F32)
    tri_b = P.sb("tri_b", [128, 128], F32)
    ones_f = P.sb("ones_f", [128, 128], F32)
    modcol = P.sb("modcol", [128, 48, 2], F32)
    nlam = P.sb("nlam", [128, 1], F32)
    Wt = P.sb("Wt", [128, NT, 32], F32)
    idxs = P.sb("idxs", [128, NT, 2], U32)
    wsel = P.sb("wsel", [128, NT, 2], F32)
    ecolC = P.sb("ecolC", [128, 32], F32)
    cntE = P.sb("cntE", [128, 32], F32)
    zt = P.sb("zt", [128, NST, D], BF16)
    persist_mark = P.mark()

    P.dma("sync", ident_bf[:, :], I["ident_bf"][:, :])
    P.dma("sync", ident_f[:, :], I["ident_f"][:, :])
    P.dma("sync", tri_f[:, :], I["tri_f"][:, :])
    P.dma("sync", tri_b[:, :], I["tri_b"][:, :])
    P.dma("sync", ecolC[:, :], I["ecolC"][:, :])
    P.op("vector", lambda e: e.memset(ones_f[:, :], 1.0), [], [ones_f])
    P.op("gpsimd", lambda e: e.memset(zt[:, :, :], 0.0), [], [zt])

    regs = {}

    def get_bc(e):
        if "bc" not in regs:
            regs["bc"] = e.alloc_register("bcreg")
            e.reg_mov(regs["bc"], NE * CAP - 1)
        return regs["bc"]

    def V(fn, r, w):
        P.op("vector", fn, r, w)

    def A(fn, r, w):
        P.op("scalar", fn, r, w)

    def G(fn, r, w):
        P.op("gpsimd", fn, r, w)

    def T(fn, r, w):
        P.op("tensor", fn, r, w)

    def mm(out, lhsT, rhs, start, stop, r=None, w=None):
        T(lambda e: e.matmul(out, lhsT, rhs, start=start, stop=stop), r if r is not None else [lhsT, rhs],
          w if w is not None else [out])

    def tr(out, in_, ident, r=None, w=None):
        T(lambda e: e.transpose(out, in_, ident), r if r is not None else [in_, ident], w if w is not None else [out])

    def ln_stats(xt, tagbuf):
        st, mv, rstd = tagbuf
        V(lambda e: e.bn_stats(st[:, 0, :], xt[:, 0:512]), [xt], [st])
        V(lambda e: e.bn_stats(st[:, 1, :], xt[:, 512:1024]), [xt], [st])
        V(lambda e: e.bn_aggr(mv[:, :], st[:, :, :].rearrange("p a b -> p (a b)")), [st], [mv])
        A(lambda e: e.activation(rstd[:, :], mv[:, 1:2], AF.Sqrt, bias=eps_col[:, :], scale=1.0), [mv, eps_col], [rstd])
        V(lambda e: e.reciprocal(rstd[:, :], rstd[:, :]), [rstd], [rstd])
        return mv[:, 0:1], rstd[:, 0:1]

    eps_col = P.sb("eps_col", [128, 1], F32)
    V(lambda e: e.memset(eps_col[:, :], LN_EPS), [], [eps_col])
    one_col = P.sb("one_col", [128, 1], F32)
    V(lambda e: e.memset(one_col[:, :], 1.0), [], [one_col])
    persist_mark = P.mark()

    def layer(L, x_cur):
        lam_init = 0.8 - 0.6 * math.exp(-0.3 * L)
        last = (L == DEPTH - 1)

        P.label = f"L{L}_P0"
        P.barrier()
        P.release(persist_mark)
        c2 = P.sb("c2", [128, 8, 2], F32)
        c2b = P.sb("c2b", [128, 8, 2], BF16)
        brow = P.sb("brow", [2, 6 * D], F32)
        mrow = P.sb("mrow", [2, 6 * D], F32)
        wa = [P.sb(f"wa{i}", [128, 8, 512], BF16) for i in range(2)]
        P.dma("sync", c2[:, :, :], I["c2"].ap().rearrange("(k p) r -> p k r", p=128))
        A(lambda e: e.activation(c2b[:, :, :], c2[:, :, :], AF.Silu), [c2], [c2b])
        for r in range(2):
            P.dma("sync", brow[r:r + 1, :], I["b_ada"][L:L + 1, :])
        for cb in range(12):
            w = wa[cb % 2]
            P.dma("gpsimd", w[:, :, :], I["w_ada"][L, :, cb * 512:(cb + 1) * 512].rearrange("(k p) n -> p k n", p=128))
            for k in range(8):
                mm(pb[cb % 2][0:2, :], c2b[:, k, :], w[:, k, :], k == 0, k == 7)
            V(lambda e, cb=cb: e.tensor_tensor(mrow[:, cb * 512:(cb + 1) * 512], pb[cb % 2][0:2, :],
                                               brow[:, cb * 512:(cb + 1) * 512], ALU.add),
              [pb[cb % 2], brow], [mrow])
        P.dma("sync", S["modrow"][:, :], mrow[:, :])
        for r in range(2):
            P.dma("sync", modcol[:, :, r], S["modrow"][r].rearrange("(c p) -> p c", p=128),
                  allow_slow_non_contiguous=True)
        V(lambda e: e.tensor_scalar_add(modcol[:, 8:16, :], modcol[:, 8:16, :], 1.0), [modcol], [modcol])
        V(lambda e: e.tensor_scalar_add(modcol[:, 32:40, :], modcol[:, 32:40, :], 1.0), [modcol], [modcol])
        lamt = P.sb("lamt", [128, 4, 64], F32)
        lamp = P.sb("lamp", [128, 2, 64], F32)
        lamd = P.sb("lamd", [128, 2], F32)
        P.dma("sync", lamt[:, :, :], I["da_lambda"][L:L + 1, :, :].broadcast_to([128, 4, 64]))
        V(lambda e: e.tensor_tensor(lamp[:, 0, :], lamt[:, 0, :], lamt[:, 1, :], ALU.mult), [lamt], [lamp])
        V(lambda e: e.tensor_tensor(lamp[:, 1, :], lamt[:, 2, :], lamt[:, 3, :], ALU.mult), [lamt], [lamp])
        V(lambda e: e.tensor_reduce(lamd[:, :], lamp[:, :, :], AX.X, ALU.add), [lamp], [lamd])
        A(lambda e: e.activation(lamd[:, :], lamd[:, :], AF.Exp), [lamd], [lamd])
        V(lambda e: e.scalar_tensor_tensor(nlam[:, :], lamd[:, 1:2], -lam_init, lamd[:, 0:1], ALU.add, ALU.subtract),
          [lamd], [nlam])

        P.label = f"L{L}_P1"
        P.barrier()
        P.release(persist_mark)
        hT = P.sb("hT", [128, 8, NTOK], BF16)
        win = P.sb("win", [128, 8, IN_W], BF16)
        for (c0, c1) in ((0, 844), (844, 1688), (1688, 2532), (2532, IN_W)):
            P.dma("gpsimd", win[:, :, c0:c1], I["w_in"][L, :, c0:c1].rearrange("(k p) n -> p k n", p=128))
        m1 = P.mark()
        xt = [P.sb(f"xt{i}", [128, D], F32) for i in range(4)]
        xn = [P.sb(f"xn{i}", [128, D], BF16) for i in range(4)]
        stb = [(P.sb(f"st{i}", [128, 2, 6], F32), P.sb(f"mv{i}", [128, 2], F32), P.sb(f"rs{i}", [128, 1], F32))
               for i in range(4)]

        def p1_s1(t):
            xb = xt[t % 4]
            st, mv, rstd = stb[t % 4]
            P.dma("sync", xb[:, :], x_cur[t * 128:(t + 1) * 128, :])
            V(lambda e: e.bn_stats(st[:, 0, :], xb[:, 0:512]), [xb], [st])
            V(lambda e: e.bn_stats(st[:, 1, :], xb[:, 512:1024]), [xb], [st])
            V(lambda e: e.bn_aggr(mv[:, :], st[:, :, :].rearrange("p a b -> p (a b)")), [st], [mv])

        def p1_s2(t):
            xb = xt[t % 4]
            st, mv, rstd = stb[t % 4]
            xnb = xn[t % 4]
            A(lambda e: e.activation(rstd[:, :], mv[:, 1:2], AF.Sqrt, bias=eps_col[:, :], scale=1.0), [mv, eps_col], [rstd])
            V(lambda e: e.reciprocal(rstd[:, :], rstd[:, :]), [rstd], [rstd])
            V(lambda e: e.tensor_scalar(xnb[:, :], xb[:, :], mv[:, 0:1], rstd[:, 0:1], ALU.subtract, ALU.mult), [xb, mv, rstd], [xnb])

        def p1_s3(t):
            b = t % 2
            xnb = xn[t % 4]
            for ch in range(8):
                bank = 4 + 2 * b + ch // 4
                pT = pb[bank][:, :].bitcast(BF16)
                tr(pT[:, (ch % 4) * 128:(ch % 4 + 1) * 128], xnb[:, ch * 128:(ch + 1) * 128], ident_bf[:, :],
                   [xnb, ident_bf], [pb[bank]])

        def p1_s4(t):
            b = t % 2
            r = 1 if t < NCTX_T else 0
            for ch in range(8):
                bank = 4 + 2 * b + ch // 4
                pT = pb[bank][:, :].bitcast(BF16)
                o = hT[:, ch, t * 128:(t + 1) * 128]
                i_ = pT[:, (ch % 4) * 128:(ch % 4 + 1) * 128]
                if ch < 4:
                    A(lambda e, o=o, i_=i_, ch=ch, r=r: e.activation(o, i_, AF.Identity, bias=modcol[:, ch, r:r + 1],
                                                                     scale=modcol[:, 8 + ch, r:r + 1]),
                      [pb[bank], modcol], [("hT", t)])
                else:
                    V(lambda e, o=o, i_=i_, ch=ch, r=r: e.tensor_scalar(o, i_, modcol[:, 8 + ch, r:r + 1],
                                                                        modcol[:, ch, r:r + 1], ALU.mult, ALU.add),
                      [pb[bank], modcol], [("hT", t)])

        for i_ in range(NT + 3):
            if i_ < NT:
                p1_s1(i_)
            if 0 <= i_ - 1 < NT:
                p1_s2(i_ - 1)
            if 0 <= i_ - 2 < NT:
                p1_s3(i_ - 2)
            if 0 <= i_ - 3 < NT:
                p1_s4(i_ - 3)
        hT_all = [("hT", t) for t in range(NT)]
        if debug:
            for ch in range(8):
                P.dma("sync", S["dbg_hT"][ch], hT[:, ch, :], reads=hT_all)
        if stop_after == "P1":
            return None

        P.label = f"L{L}_P2"
        P.barrier()
        P.release(m1)
        wrot = P.sb("wrot", [128, 8, 1024], BF16)
        wv = win[:, :, 0:1024].rearrange("p k (g h s) -> p k g h s", h=2, s=16)
        rv = wrot[:, :, :].rearrange("p k (g h s) -> p k g h s", h=2, s=16)
        for k in range(8):
            V(lambda e, k=k: e.tensor_copy(rv[:, k, :, 0, :], wv[:, k, :, 1, :]), [win], [wrot])
            G(lambda e, k=k: e.tensor_copy(rv[:, k, :, 1, :], wv[:, k, :, 0, :]), [win], [wrot])
        cosT = P.sb("cosT", [128, NTOK], F32)
        sinT = P.sb("sinT", [128, NTOK], F32)
        P.dma("sync", cosT[:, :], I["cosT"][:, :])
        P.dma("sync", sinT[:, :], I["sinT"][:, :])
        m2 = P.mark()

        def fm_proj(bank, lhs_fn, M, tb, extra_r=()):
            t0, tn = TB[tb]
            for k in range(8):
                mm(pb[bank][0:M, 0:tn], lhs_fn(k), hT[:, k, t0:t0 + tn], k == 0, k == 7,
                   r=[win, wrot] + hT_all + list(extra_r), w=[pb[bank]])

        P.label = f"L{L}_P2a_daqk"
        stg = [P.sb(f"stg{i}", [128, NTOK], BF16) for i in range(2)]
        t1 = [P.sb(f"t1_{i}", [128, 512], F32) for i in range(2)]
        t2 = [P.sb(f"t2_{i}", [128, 512], F32) for i in range(2)]
        for ch in range(8):
            sg = stg[ch % 2]
            for tb in range(5):
                t0, tn = TB[tb]
                fm_proj(0, lambda k, ch=ch: win[:, k, ch * 128:(ch + 1) * 128], 128, tb)
                fm_proj(1, lambda k, ch=ch: wrot[:, k, ch * 128:(ch + 1) * 128], 128, tb)
                a1, a2 = t1[tb % 2], t2[tb % 2]
                V(lambda e, a1=a1, t0=t0, tn=tn: e.tensor_tensor(a1[:, 0:tn], pb[0][:, 0:tn], cosT[:, t0:t0 + tn], ALU.mult),
                  [pb[0], cosT], [a1])
                V(lambda e, a2=a2, t0=t0, tn=tn: e.tensor_tensor(a2[:, 0:tn], pb[1][:, 0:tn], sinT[:, t0:t0 + tn], ALU.mult),
                  [pb[1], sinT], [a2])
                G(lambda e, a1=a1, a2=a2, sg=sg, t0=t0, tn=tn: e.tensor_tensor(sg[:, t0:t0 + tn], a1[:, 0:tn], a2[:, 0:tn], ALU.add),
                  [a1, a2], [sg])
            P.dma("sync", S["da_qk"][ch], sg[:, :])
        P.barrier()
        P.release(m2)

        P.label = f"L{L}_P2b_mlqk"
        cw = P.sb("cw", [128, 4, 3], F32)
        cbias = P.sb("cbias", [128, 4], F32)
        for j_ in range(3):
            P.dma("sync", cw[:, :, j_], I["ml_conv_w"][L, j_].rearrange("(c p) -> p c", p=128), allow_slow_non_contiguous=True)
        P.dma("sync", cbias[:, :], I["ml_conv_b"][L].rearrange("(c p) -> p c", p=128), allow_slow_non_contiguous=True)
        pre = [P.sb(f"pre{i}", [128, NTOK], F32) for i in range(2)]
        acc = [P.sb(f"acc{i}", [128, NTOK], F32) for i in range(2)]
        stg = [P.sb(f"stgm{i}", [128, NTOK], BF16) for i in range(2)]
        for ch in range(4):
            pr, ac, sg = pre[ch % 2], acc[ch % 2], stg[ch % 2]
            for tb in range(5):
                t0, tn = TB[tb]
                bank = tb % 2
                fm_proj(bank, lambda k, ch=ch: win[:, k, 1536 + ch * 128:1536 + (ch + 1) * 128], 128, tb)
                A(lambda e, pr=pr, bank=bank, t0=t0, tn=tn: e.copy(pr[:, t0:t0 + tn], pb[bank][:, 0:tn]), [pb[bank]], [pr])
            V(lambda e, pr=pr, ac=ac, ch=ch: e.tensor_scalar(ac[:, :], pr[:, :], cw[:, ch, 1:2], cbias[:, ch:ch + 1],
                                                             ALU.mult, ALU.add), [pr, cw, cbias], [ac])
            for (s0, s1) in ((0, 256), (256, NTOK)):
                V(lambda e, pr=pr, ac=ac, ch=ch, s0=s0, s1=s1: e.scalar_tensor_tensor(
                    ac[:, s0 + 1:s1], pr[:, s0:s1 - 1], cw[:, ch, 0:1], ac[:, s0 + 1:s1], ALU.mult, ALU.add),
                  [pr, cw, ac], [ac])
                V(lambda e, pr=pr, ac=ac, ch=ch, s0=s0, s1=s1: e.scalar_tensor_tensor(
                    ac[:, s0:s1 - 1], pr[:, s0 + 1:s1], cw[:, ch, 2:3], ac[:, s0:s1 - 1], ALU.mult, ALU.add),
                  [pr, cw, ac], [ac])
            A(lambda e, ac=ac, sg=sg: e.activation(sg[:, :], ac[:, :], AF.Silu), [ac], [sg])
            P.dma("sync", S["ml_qk"][ch], sg[:, :])
        P.barrier()
        P.release(m2)

        P.label = f"L{L}_P2c_gates"
        wrep = P.sb("wrep", [128, 8, 128], BF16)
        gcol = P.sb("gcol", [128, 2, 2, 2], F32)
        for ty, nm in ((0, "ml_ib"), (1, "ml_fb")):
            for d in range(2):
                for h in range(4):
                    P.dma("sync", gcol[64 * (h % 2):64 * (h % 2) + 64, ty, d, h // 2:h // 2 + 1],
                          I[nm][L, d:d + 1, h:h + 1].broadcast_to([64, 1]))
        ngfb = P.sb("ngfb", [128, 2, 2], F32)
        V(lambda e: e.tensor_scalar_mul(ngfb[:, :, :], gcol[:, 1, :, :], -1.0), [gcol], [ngfb])
        gst = [P.sb(f"gst{i}", [128, NTOK], F32) for i in range(2)]
        gi = 0
        for ty in range(2):
            for d in range(2):
                for cc in range(2):
                    for hh in range(2):
                        colx = 2560 + 8 * ty + 4 * d + 2 * cc + hh
                        V(lambda e, hh=hh, colx=colx: e.tensor_copy(
                            wrep[:, :, 64 * hh:64 * hh + 64], win[:, :, colx:colx + 1].broadcast_to([128, 8, 64])),
                          [win], [wrep])
                    sg = gst[gi % 2]
                    gi += 1
                    for tb in range(5):
                        t0, tn = TB[tb]
                        bank = tb % 2
                        fm_proj(bank, lambda k: wrep[:, k, :], 128, tb, extra_r=[wrep])
                        if ty == 0:
                            A(lambda e, sg=sg, bank=bank, t0=t0, tn=tn, d=d, cc=cc: e.activation(
                                sg[:, t0:t0 + tn], pb[bank][:, 0:tn], AF.Identity, bias=gcol[:, 0, d, cc:cc + 1], scale=1.0),
                              [pb[bank], gcol], [sg])
                        else:
                            A(lambda e, sg=sg, bank=bank, t0=t0, tn=tn, d=d, cc=cc: e.activation(
                                sg[:, t0:t0 + tn], pb[bank][:, 0:tn], AF.Exp, bias=ngfb[:, d, cc:cc + 1], scale=-1.0),
                              [pb[bank], ngfb], [sg])
                    if ty == 1:
                        A(lambda e, sg=sg: e.activation(sg[:, :], sg[:, :], AF.Ln, bias=one_col[:, :], scale=1.0), [sg, one_col], [sg])
                        V(lambda e, sg=sg: e.tensor_scalar_mul(sg[:, :], sg[:, :], -1.0), [sg], [sg])
                    P.dma("sync", S["ml_ig" if ty == 0 else "ml_la"][d, cc], sg[:, :])
        P.barrier()
        P.release(m2)

        P.label = f"L{L}_P2d_gl"
        stg = [P.sb(f"stgg{i}", [128, NTOK], BF16) for i in range(2)]
        wpad = P.sb("wpad", [128, 8, 128], BF16)
        gi = 0
        for qk_ in range(2):
            for cc in range(2):
                sg = stg[gi % 2]
                gi += 1
                G(lambda e: e.memset(wpad[:, :, :], 0.0), [], [wpad])
                for hh in range(2):
                    c0 = 2576 + 128 * qk_ + (2 * cc + hh) * 32
                    G(lambda e, hh=hh, c0=c0: e.tensor_copy(wpad[:, :, 64 * hh:64 * hh + 32], win[:, :, c0:c0 + 32]), [win], [wpad])
                for tb in range(5):
                    t0, tn = TB[tb]
                    bank = tb % 2
                    fm_proj(bank, lambda k: wpad[:, k, :], 128, tb, extra_r=[wpad])
                    A(lambda e, sg=sg, bank=bank, t0=t0, tn=tn: e.copy(sg[:, t0:t0 + tn], pb[bank][:, 0:tn]), [pb[bank]], [sg])
                P.dma("sync", S["gl_qk"][2 * qk_ + cc], sg[:, :])
        aT = P.sb("aT", [32, NTOK], BF16)
        for tb in range(5):
            t0, tn = TB[tb]
            bank = tb % 2
            fm_proj(bank, lambda k: win[:, k, 3344:3376], 32, tb)
            A(lambda e, bank=bank, t0=t0, tn=tn: e.copy(aT[:, t0:t0 + tn], pb[bank][0:32, 0:tn]), [pb[bank]], [aT])
        wap = P.sb("wap", [32, 2, 2, 128], BF16)
        nba = P.sb("nba", [128, 2, 2], F32)
        G(lambda e: e.memset(wap[:, :, :, :], 0.0), [], [wap])
        G(lambda e: e.memset(nba[:, :, :], 0.0), [], [nba])
        for d in range(2):
            for cc in range(2):
                for hh in range(2):
                    h0 = (2 * cc + hh) * 32
                    P.dma("gpsimd", wap[16 * d:16 * d + 16, d, cc, 64 * hh:64 * hh + 32], I["gl_wa"][L, d, :, h0:h0 + 32])
                    P.dma("sync", nba[64 * hh:64 * hh + 32, d, cc:cc + 1], I["gl_ba"][L, d, h0:h0 + 32].rearrange("(p o) -> p o", o=1),
                          allow_slow_non_contiguous=True)
        V(lambda e: e.tensor_scalar_mul(nba[:, :, :], nba[:, :, :], -1.0), [nba], [nba])
        gls = [P.sb(f"gls{i}", [128, NTOK], F32) for i in range(2)]
        gi = 0
        for d in range(2):
            for cc in range(2):
                sg = gls[gi % 2]
                gi += 1
                for tb in range(5):
                    t0, tn = TB[tb]
                    bank = 2 + tb % 2
                    mm(pb[bank][:, 0:tn], wap[:, d, cc, :], aT[:, t0:t0 + tn], True, True)
                    A(lambda e, sg=sg, bank=bank, t0=t0, tn=tn, d=d, cc=cc: e.activation(
                        sg[:, t0:t0 + tn], pb[bank][:, 0:tn], AF.Exp, bias=nba[:, d, cc:cc + 1], scale=-1.0), [pb[bank], nba], [sg])
                A(lambda e, sg=sg: e.activation(sg[:, :], sg[:, :], AF.Ln, bias=one_col[:, :], scale=1.0), [sg, one_col], [sg])
                V(lambda e, sg=sg: e.tensor_scalar_mul(sg[:, :], sg[:, :], -1.0 / 16.0), [sg], [sg])
                P.dma("sync", S["gl_la"][d, cc], sg[:, :])
        P.barrier()
        P.release(m2)

        P.label = f"L{L}_P2e_tm"
        vst = [P.sb(f"vst{i}", [128, 4, 130], BF16) for i in range(2)]
        mvst = [P.sb(f"mvst{i}", [128, 4, 66], BF16) for i in range(2)]
        ost = [P.sb(f"ost{i}", [128, 256], F32) for i in range(2)]
        gvst = [P.sb(f"gvst{i}", [128, 256], BF16) for i in range(2)]
        rst = [P.sb(f"rst{i}", [128, 256], F32) for i in range(2)]
        for i in range(2):
            G(lambda e, i=i: e.memset(vst[i][:, :, :], 0.0), [], [vst[i]])
            G(lambda e, i=i: e.memset(vst[i][:, :, 128:129], 1.0), [], [vst[i]])
            G(lambda e, i=i: e.memset(mvst[i][:, :, :], 0.0), [], [mvst[i]])
            G(lambda e, i=i: e.memset(mvst[i][:, :, 64:65], 1.0), [], [mvst[i]])

        def tm_proj(bank, t, c0, ncol):
            for k in range(8):
                mm(pb[bank][:, 0:ncol], hT[:, k, t * 128:(t + 1) * 128], win[:, k, c0:c0 + ncol], k == 0, k == 7,
                   r=[win, ("hT", t)], w=[pb[bank]])

        for t in range(NT):
            b = t % 2
            ts_ = slice(t * 128, (t + 1) * 128)
            B0, B1, B2 = 3 * b, 3 * b + 1, 3 * b + 2
            tm_proj(B0, t, 1024, 512)
            V(lambda e, b=b, B0=B0: e.tensor_copy(vst[b][:, :, 0:128], pb[B0][:, :].rearrange("p (h d) -> p h d", h=4)), [pb[B0]], [vst[b]])
            P.dma("sync", S["da_v"][ts_, :], vst[b][:, :, :].rearrange("p h d -> p (h d)"))
            tm_proj(B1, t, 2048, 512)
            V(lambda e, b=b, B1=B1: e.tensor_copy(mvst[b][:, :, 0:64], pb[B1][:, 0:256].rearrange("p (h d) -> p h d", h=4)), [pb[B1]], [mvst[b]])
            A(lambda e, b=b, B1=B1: e.activation(ost[b][:, :], pb[B1][:, 256:512], AF.Sigmoid), [pb[B1]], [ost[b]])
            P.dma("sync", S["ml_v"][ts_, :], mvst[b][:, :, :].rearrange("p h d -> p (h d)"))
            P.dma("sync", S["ml_o"][ts_, :], ost[b][:, :])
            tm_proj(B2, t, 2832, 512)
            V(lambda e, b=b, B2=B2: e.tensor_copy(gvst[b][:, :], pb[B2][:, 0:256]), [pb[B2]], [gvst[b]])
            A(lambda e, b=b, B2=B2: e.activation(rst[b][:, :], pb[B2][:, 256:512], AF.Silu), [pb[B2]], [rst[b]])
            P.dma("sync", S["gl_v"][ts_, :], gvst[b][:, :])
            P.dma("sync", S["gl_r"][ts_, :], rst[b][:, :])
        if stop_after == "P2":
            return None

        P.label = f"L{L}_P3"
        P.barrier()
        P.release(persist_mark)
        qk = P.sb("qk", [128, 4, NTOK], BF16)
        kz = P.sb("kz", [128, 2, 4, NTOK], BF16)
        vv = P.sb("vv", [128, NT, 520], BF16)
        for m in range(2):
            G(lambda e, m=m: e.memset(kz[64 * (1 - m):64 * (1 - m) + 64, m, :, :], 0.0), [], [kz])
        for ch in range(4):
            P.dma("sync", qk[:, ch, :], S["da_qk"][ch])
            for m in range(2):
                P.dma("sync", kz[64 * m:64 * m + 64, m, ch, :], S["da_qk"][4 + ch, 64 * m:64 * m + 64, :])
        for t in range(NT):
            P.dma("sync", vv[:, t, :], S["da_v"][t * 128:(t + 1) * 128, :])
        for ex_ in (range(NE) if L == 0 else []):
            P.dma("sync", S["xslots"][ex_ * CAP:(ex_ + 1) * CAP, :].rearrange("(s p) d -> p s d", p=128), zt[:, :, :],
                  writes=[("xslots", t_, k__) for t_ in range(NT) for k__ in range(2)])
        gda = P.sb("gda", [128, 128], F32)
        P.dma("sync", gda[:, :], I["da_norm"][L:L + 1, :].broadcast_to([128, 128]))
        V(lambda e: e.tensor_scalar_mul(gda[:, :], gda[:, :], 1.0 - lam_init), [gda], [gda])
        Eb = [P.sb(f"Eb{i}", [128, 512], BF16) for i in range(3)]
        osb = [P.sb(f"osb{i}", [128, 128], F32) for i in range(2)]
        o2 = [P.sb(f"o2{i}", [128, 128], F32) for i in range(2)]
        sq = [P.sb(f"sq{i}", [128, 128], F32) for i in range(2)]
        rc = [P.sb(f"rc{i}", [128, 4], F32) for i in range(2)]
        oall = P.sb("oall", [128, NT, 4, 128], BF16)
        vvh = vv[:, :, :].rearrange("p t (h d) -> p t h d", h=4)
        qblocks = [(0, 256, [0, 1])] + [(256 + 512 * i, 512, list(range(NT))) for i in range(4)]
        nonlocal_ei = [0]
        oi = 0
        rnd = 0
        for h in range(4):
            for (q0, qn, kts) in qblocks:
                nqs = qn // 128
                ob = 2 + 3 * (rnd % 2)
                rnd += 1
                touched = set()
                seq = [(m, kt, qs) for m in range(2) for kt in kts for qs in range(nqs)]
                lastt = {}
                for (m, kt, qs) in seq:
                    lastt[(qs * 2 + m) // 3] = (m, kt, qs)
                steps = [(m, kt) for m in range(2) for kt in kts]

                def issue_scores(i):
                    m, kt = steps[i]
                    sbank = i % 2
                    mm(pb[sbank][:, 0:qn], kz[:, m, h, kt * 128:(kt + 1) * 128], qk[:, h, q0:q0 + qn], True, True,
                       r=[kz, qk], w=[pb[sbank]])
                    nonlocal_ei[0] += 1
                    E = Eb[nonlocal_ei[0] % 3]
                    A(lambda e, E=E, sbank=sbank, qn=qn: e.activation(E[:, 0:qn], pb[sbank][:, 0:qn], AF.Exp, scale=0.125),
                      [pb[sbank]], [E])
                    return E

                Es = {0: issue_scores(0)}
                for i, (m, kt) in enumerate(steps):
                    if i + 1 < len(steps):
                        Es[i + 1] = issue_scores(i + 1)
                    E = Es.pop(i)
                    for qs in range(nqs):
                        a = qs * 2 + m
                        bank = ob + a // 3
                        c0 = 130 * (a % 3)
                        st_ = bank not in touched
                        touched.add(bank)
                        sp_ = lastt[a // 3] == (m, kt, qs)
                        mm(pb[bank][:, c0:c0 + 129], E[:, qs * 128:(qs + 1) * 128], vvh[:, kt, h, 0:129], st_, sp_,
                           r=[E, vv], w=[pb[bank]])
                for qs in range(nqs):
                    j = oi % 2
                    oi += 1
                    a0, a1 = qs * 2, qs * 2 + 1
                    b0, c0 = ob + a0 // 3, 130 * (a0 % 3)
                    b1, c1 = ob + a1 // 3, 130 * (a1 % 3)
                    tq = (q0 + qs * 128) // 128
                    V(lambda e, j=j, b0=b0, c0=c0: e.reciprocal(rc[j][:, 0:1], pb[b0][:, c0 + 128:c0 + 129]), [pb[b0]], [rc[j]])
                    V(lambda e, j=j, b1=b1, c1=c1: e.reciprocal(rc[j][:, 1:2], pb[b1][:, c1 + 128:c1 + 129]), [pb[b1]], [rc[j]])
                    V(lambda e, j=j: e.tensor_tensor(rc[j][:, 1:2], rc[j][:, 1:2], nlam[:, :], ALU.mult), [rc[j], nlam], [rc[j]])
                    V(lambda e, j=j, b0=b0, c0=c0: e.tensor_scalar(osb[j][:, :], pb[b0][:, c0:c0 + 128], rc[j][:, 0:1], None, ALU.mult),
                      [pb[b0], rc[j]], [osb[j]])
                    V(lambda e, j=j, b1=b1, c1=c1: e.scalar_tensor_tensor(o2[j][:, :], pb[b1][:, c1:c1 + 128], rc[j][:, 1:2],
                                                                         osb[j][:, :], ALU.mult, ALU.add),
                      [pb[b1], rc[j], osb[j]], [o2[j]])
                    G(lambda e, j=j: e.tensor_tensor(sq[j][:, :], o2[j][:, :], o2[j][:, :], ALU.mult), [o2[j]], [sq[j]])
                    V(lambda e, j=j: e.tensor_reduce(rc[j][:, 2:3], sq[j][:, :], AX.X, ALU.add), [sq[j]], [rc[j]])
                    A(lambda e, j=j: e.activation(rc[j][:, 2:3], rc[j][:, 2:3], AF.Sqrt, bias=eps_col[:, :], scale=1.0 / 128.0),
                      [rc[j], eps_col], [rc[j]])
                    V(lambda e, j=j: e.reciprocal(rc[j][:, 3:4], rc[j][:, 2:3]), [rc[j]], [rc[j]])
                    V(lambda e, j=j, tq=tq, h=h: e.scalar_tensor_tensor(oall[:, tq, h, :], o2[j][:, :], rc[j][:, 3:4], gda[:, :], ALU.mult, ALU.mult),
                      [o2[j], rc[j], gda], [("oall", tq, h)])
        mixst = [P.sb(f"mixst{i}", [128, NTOK], BF16) for i in range(2)]
        ti = 0
        for h in range(4):
            mst = mixst[h % 2]
            for t0_ in range(0, NT, 4):
                nt_ = min(4, NT - t0_)
                bank = ti % 2
                ti += 1
                pT = pb[bank][:, :].bitcast(BF16)
                for k_ in range(nt_):
                    tr(pT[:, k_ * 128:(k_ + 1) * 128], oall[:, t0_ + k_, h, :], ident_bf[:, :], [("oall", t0_ + k_, h), ident_bf], [pb[bank]])
                if bank == 0:
                    A(lambda e, pT=pT, mst=mst, t0_=t0_, nt_=nt_: e.copy(mst[:, t0_ * 128:(t0_ + nt_) * 128], pT[:, 0:nt_ * 128]), [pb[bank]], [mst])
                else:
                    V(lambda e, pT=pT, mst=mst, t0_=t0_, nt_=nt_: e.tensor_copy(mst[:, t0_ * 128:(t0_ + nt_) * 128], pT[:, 0:nt_ * 128]), [pb[bank]], [mst])
            P.dma("sync", S["mixT"][h], mst[:, :])
        if stop_after == "P3":
            return None

        P.label = f"L{L}_P4/P5"
        def decay_attn(kind):
            P.barrier()
            P.release(persist_mark)
            ml = (kind == "ml")
            P.label = f"L{L}_P45_{kind}"
            ncc = 2
            Hc = 2
            dk = 64
            dva = 65 if ml else 64
            vstride = 66 if ml else 64
            qscale = (64 if ml else 32) ** -0.5
            qT = P.sb("qT", [128, ncc, NTOK], BF16)
            kT = P.sb("kT", [128, ncc, NTOK], BF16)
            for cc in range(ncc):
                P.dma("sync", qT[:, cc, :], S["ml_qk" if ml else "gl_qk"][cc])
                P.dma("sync", kT[:, cc, :], S["ml_qk" if ml else "gl_qk"][2 + cc])
            Vt = P.sb("Vt", [128, NT, 4 * vstride], BF16)
            for t in range(NT):
                P.dma("sync", Vt[:, t, :], S["ml_v" if ml else "gl_v"][t * 128:(t + 1) * 128, :])
            Vh = Vt[:, :, :].rearrange("p t (h d) -> p t h d", h=4)
            Hsum = P.sb("Hsum", [128, NT, 256], F32)
            Hs4 = Hsum[:, :, :].rearrange("p t (h d) -> p t h d", h=4)
            Hsum2 = P.sb("Hsum2", [128, NT, 256], F32)
            Hb4 = Hsum2[:, :, :].rearrange("p t (h d) -> p t h d", h=4)
            chains = [(d, cc) for d in range(2) for cc in range(ncc)]
            laC = {c: P.sb(f"la{c[0]}{c[1]}", [128, NTOK], F32) for c in chains}
            igC = {c: (P.sb(f"ig{c[0]}{c[1]}", [128, NTOK], F32) if ml else None) for c in chains}
            SstC = {c: P.sb(f"Sst{c[0]}{c[1]}", [128, dva], F32) for c in chains}
            SbfC = {c: P.sb(f"Sbf{c[0]}{c[1]}", [128, dva], BF16) for c in chains}
            NB = 4
            bT = [P.sb(f"bT{i}", [128, 128], F32) for i in range(NB)]
            pfx = [P.sb(f"pfx{i}", [128, 128], F32) for i in range(NB)]
            arg = [P.sb(f"arg{i}", [128, 128], F32) for i in range(NB)]
            eq = [P.sb(f"eq{i}", [128, 128], F32) for i in range(NB)]
            ek = [P.sb(f"ek{i}", [128, 128], F32) for i in range(NB)]
            ekh = [P.sb(f"ekh{i}", [128, 128], F32) for i in range(NB)]
            gam = [P.sb(f"gam{i}", [128, 1], F32) for i in range(NB)]
            qt_ = [P.sb(f"qt_{i}", [128, 128], BF16) for i in range(NB)]
            kt_ = [P.sb(f"kt_{i}", [128, 128], BF16) for i in range(NB)]
            kh_ = [P.sb(f"kh_{i}", [128, 128], BF16) for i in range(NB)]
            khT = [P.sb(f"khT{i}", [128, 128], BF16) for i in range(NB)]
            PT = [P.sb(f"PT{i}", [128, Hc, 128], BF16) for i in range(NB)]
            den = [P.sb(f"den{i}", [128, Hc, 1], F32) for i in range(NB)]
            for c in chains:
                P.dma("sync", laC[c][:, :], S["ml_la" if ml else "gl_la"][c[0], c[1]])
                if ml:
                    P.dma("sync", igC[c][:, :], S["ml_ig"][c[0], c[1]])
                V(lambda e, c=c: e.memset(SstC[c][:, :], 0.0), [], [SstC[c]])
                V(lambda e, c=c: e.memset(SbfC[c][:, :], 0.0), [], [SbfC[c]])
            orders = {0: list(range(NT)), 1: [1, 0] + list(range(NT - 1, 1, -1))}
            it = 0
            qz = [[P.sb(f"qz{i}_{hh}", [128, 128], BF16) for hh in range(Hc)] for i in range(NB)]
            for i in range(NB):
                for hh in range(Hc):
                    G(lambda e, i=i, hh=hh: e.memset(qz[i][hh][:, :], 0.0), [], [qz[i][hh]])
            for step in range(NT):
                ctxs = []
                for ci, (d, cc) in enumerate(chains):
                    la, ig = laC[(d, cc)], igC[(d, cc)]
                    t = orders[d][step]
                    j = ci
                    tsl = slice(t * 128, (t + 1) * 128)
                    if d == 0:
                        V(lambda e, j=j, tsl=tsl, la=la: e.tensor_tensor_scan(bT[j][:, :], ones_f[:, :], la[:, tsl], 0.0, ALU.mult, ALU.add),
                          [ones_f, la], [bT[j]])
                        tot = bT[j][:, 127:128]
                    else:
                        V(lambda e, j=j, tsl=tsl, la=la: e.tensor_tensor_scan(pfx[j][:, :], ones_f[:, :], la[:, tsl], 0.0, ALU.mult, ALU.add),
                          [ones_f, la], [pfx[j]])
                        V(lambda e, j=j, tsl=tsl, la=la: e.tensor_tensor(bT[j][:, :], la[:, tsl], pfx[j][:, :], ALU.subtract),
                          [la, pfx[j]], [bT[j]])
                        V(lambda e, j=j: e.tensor_scalar(bT[j][:, :], bT[j][:, :], pfx[j][:, 127:128], None, ALU.add),
                          [bT[j], pfx[j]], [bT[j]])
                        tot = bT[j][:, 0:1]
                    A(lambda e, j=j: e.activation(eq[j][:, :], bT[j][:, :], AF.Exp), [bT[j]], [eq[j]])
                    for hh in range(Hc):
                        V(lambda e, j=j, cc=cc, tsl=tsl, hh=hh: e.scalar_tensor_tensor(
                            qz[j][hh][64 * hh:64 * hh + 64, :], qT[64 * hh:64 * hh + 64, cc, tsl], qscale, eq[j][64 * hh:64 * hh + 64, :],
                            ALU.mult, ALU.mult), [qT, eq[j]], [qz[j][hh]])
                    if ml:
                        G(lambda e, j=j, tsl=tsl, ig=ig: e.tensor_tensor(arg[j][:, :], ig[:, tsl], bT[j][:, :], ALU.subtract), [ig, bT[j]], [arg[j]])
                        A(lambda e, j=j: e.activation(ek[j][:, :], arg[j][:, :], AF.Exp), [arg[j]], [ek[j]])
                        A(lambda e, j=j, tot=tot: e.activation(ekh[j][:, :], arg[j][:, :], AF.Exp, bias=tot, scale=1.0), [arg[j], bT[j]], [ekh[j]])
                    else:
                        A(lambda e, j=j: e.activation(ek[j][:, :], bT[j][:, :], AF.Exp, scale=-1.0), [bT[j]], [ek[j]])
                        A(lambda e, j=j, tot=tot: e.activation(ekh[j][:, :], bT[j][:, :], AF.Exp, bias=tot, scale=-1.0), [bT[j]], [ekh[j]])
                    A(lambda e, j=j, tot=tot: e.activation(gam[j][:, :], tot, AF.Exp), [bT[j]], [gam[j]])
                    G(lambda e, j=j, cc=cc, tsl=tsl: e.tensor_tensor(kt_[j][:, :], kT[:, cc, tsl], ek[j][:, :], ALU.mult), [kT, ek[j]], [kt_[j]])
                    G(lambda e, j=j, cc=cc, tsl=tsl: e.tensor_tensor(kh_[j][:, :], kT[:, cc, tsl], ekh[j][:, :], ALU.mult), [kT, ekh[j]], [kh_[j]])
                    pTk = pb[7][:, :].bitcast(BF16)
                    tr(pTk[:, 0:128], kh_[j][:, :], ident_bf[:, :], [kh_[j], ident_bf], [pb[7]])
                    A(lambda e, j=j, pTk=pTk: e.copy(khT[j][:, :], pTk[:, 0:128]), [pb[7]], [khT[j]])
                    for hh in range(Hc):
                        mm(pb[ci][:, hh * 128:(hh + 1) * 128], kt_[j][:, :], qz[j][hh][:, :], hh == 0, hh == Hc - 1,
                           r=[kt_[j], qz[j][hh]], w=[pb[ci]])
                    ctxs.append((d, cc, t, j, ci))
                for (d, cc, t, j, ci) in ctxs:
                    tri = tri_f if d == 0 else tri_b
                    Sbf = SbfC[(d, cc)]
                    Sst = SstC[(d, cc)]
                    hb = 4 + ci % 2
                    V(lambda e, j=j, tri=tri, ci=ci: e.tensor_tensor(
                        PT[j][:, :, :], pb[ci][:, 0:Hc * 128].rearrange("p (h l) -> p h l", h=Hc),
                        tri[:, :].unsqueeze(1).broadcast_to([128, Hc, 128]), ALU.mult), [pb[ci], tri], [PT[j]])
                    for hh in range(Hc):
                        head = Hc * cc + hh
                        hc0 = hh * 128
                        mm(pb[hb][:, hc0:hc0 + dva], PT[j][:, hh, :], Vh[:, t, head, 0:dva], hh == 0, False, r=[PT[j], Vt], w=[pb[hb]])
                        mm(pb[hb][:, hc0:hc0 + dva], qz[j][hh][:, :], Sbf[:, :], False, hh == Hc - 1, r=[qz[j][hh], Sbf], w=[pb[hb]])
                    Hx4 = Hs4 if d == 0 else Hb4
                    hkey = ("Hsum", t) if d == 0 else ("Hsum2", t)
                    if ml:
                        A(lambda e, j=j, hb=hb: e.activation(den[j][:, :, :], pb[hb][:, 0:Hc * 128].rearrange("p (h c) -> p h c", h=Hc)[:, :, 64:65],
                                                             AF.Abs), [pb[hb]], [den[j]])
                        V(lambda e, j=j: e.tensor_scalar_max(den[j][:, :, :], den[j][:, :, :], 1.0), [den[j]], [den[j]])
                        V(lambda e, j=j: e.reciprocal(den[j][:, :, :], den[j][:, :, :]), [den[j]], [den[j]])
                    for hh in range(Hc):
                        head = Hc * cc + hh
                        hc0 = hh * 128
                        if ml:
                            A(lambda e, j=j, hh=hh, hb=hb, hc0=hc0, head=head, t=t, Hx4=Hx4: e.activation(
                                Hx4[:, t, head, :], pb[hb][:, hc0:hc0 + 64], AF.Identity, scale=den[j][:, hh, :]), [pb[hb], den[j]], [hkey])
                        else:
                            A(lambda e, t=t, head=head, hb=hb, hc0=hc0, Hx4=Hx4: e.copy(Hx4[:, t, head, :], pb[hb][:, hc0:hc0 + 64]), [pb[hb]], [hkey])
                    for hh in range(Hc):
                        head = Hc * cc + hh
                        mm(pb[6][dk * hh:dk * (hh + 1), 0:dva], khT[j][:, dk * hh:dk * (hh + 1)], Vh[:, t, head, 0:dva], True, True,
                           r=[khT[j], Vt], w=[pb[6]])
                    V(lambda e, j=j, Sst=Sst: e.scalar_tensor_tensor(Sst[:, :], Sst[:, :], gam[j][:, :], pb[6][:, 0:dva], ALU.mult, ALU.add),
                      [Sst, gam[j], pb[6]], [Sst])
                    A(lambda e, Sst=Sst, Sbf=Sbf: e.copy(Sbf[:, :], Sst[:, :]), [Sst], [Sbf])
            P.label = f"L{L}_P45_{kind}_fin"
            gnm = P.sb("gnm", [128, 64], F32)
            P.dma("sync", gnm[:, :], I["ml_norm" if ml else "gl_norm"][L:L + 1, :].broadcast_to([128, 64]))
            gate = [P.sb(f"gate{i}", [128, 256], F32) for i in range(2)]
            sqh = [P.sb(f"sqh{i}", [128, 256], F32) for i in range(2)]
            ssq = [P.sb(f"ssq{i}", [128, 4], F32) for i in range(2)]
            obm = [P.sb(f"obm{i}", [128, 256], BF16) for i in range(2)]
            mst = [P.sb(f"mstm{i}", [128, NTOK], BF16) for i in range(2)]
            for t in range(NT):
                j = t % 2
                P.dma("sync", gate[j][:, :], S["ml_o" if ml else "gl_r"][t * 128:(t + 1) * 128, :])
                G(lambda e, j=j: e.tensor_tensor(gate[j][:, :].rearrange("p (h d) -> p h d", h=4),
                                                 gate[j][:, :].rearrange("p (h d) -> p h d", h=4),
                                                 gnm[:, :].unsqueeze(1).broadcast_to([128, 4, 64]), ALU.mult), [gate[j], gnm], [gate[j]])
                G(lambda e, t=t: e.tensor_tensor(Hsum[:, t, :], Hsum[:, t, :], Hsum2[:, t, :], ALU.add), [("Hsum", t), ("Hsum2", t)], [("Hsum", t)])
                V(lambda e, j=j, t=t: e.tensor_tensor(sqh[j][:, :], Hsum[:, t, :], Hsum[:, t, :], ALU.mult), [("Hsum", t)], [sqh[j]])
                V(lambda e, j=j: e.tensor_reduce(ssq[j][:, :], sqh[j][:, :].rearrange("p (h d) -> p h d", h=4), AX.X, ALU.add), [sqh[j]], [ssq[j]])
                A(lambda e, j=j: e.activation(ssq[j][:, :], ssq[j][:, :], AF.Sqrt, bias=eps_col[:, :], scale=1.0 / 64.0), [ssq[j], eps_col], [ssq[j]])
                V(lambda e, j=j: e.reciprocal(ssq[j][:, :], ssq[j][:, :]), [ssq[j]], [ssq[j]])
                for hh in range(4):
                    V(lambda e, j=j, t=t, hh=hh: e.scalar_tensor_tensor(obm[j][:, hh * 64:(hh + 1) * 64], Hs4[:, t, hh, :], ssq[j][:, hh:hh + 1],
                                                                        gate[j][:, hh * 64:(hh + 1) * 64], ALU.mult, ALU.mult),
                      [("Hsum", t), ssq[j], gate[j]], [obm[j]])
                pT = pb[5][:, :].bitcast(BF16)
                for c2_ in range(2):
                    tr(pT[:, c2_ * 128:(c2_ + 1) * 128], obm[j][:, c2_ * 128:(c2_ + 1) * 128], ident_bf[:, :], [obm[j], ident_bf], [pb[5]])
                    A(lambda e, c2_=c2_, t=t, pT=pT: e.copy(mst[c2_][:, t * 128:(t + 1) * 128], pT[:, c2_ * 128:(c2_ + 1) * 128]), [pb[5]], [mst[c2_]])
            base = 4 if ml else 6
            for c2_ in range(2):
                P.dma("sync", S["mixT"][base + c2_], mst[c2_][:, :])

        decay_attn("ml")
        if stop_after == "P4":
            return None
        decay_attn("gl")
        if stop_after == "P5":
            return None

        P.label = f"L{L}_P6"
        P.barrier()
        P.release(persist_mark)
        m6 = P.mark()
        mixT = P.sb("mixTs", [128, 8, NTOK], BF16)
        for ch in range(8):
            P.dma("sync", mixT[:, ch, :], S["mixT"][ch])
        wo = P.sb("wo", [128, 8, D], BF16)
        P.dma("gpsimd", wo[:, :, :], I["w_out"][L].rearrange("(k p) n -> p k n", p=128))
        g1bc = P.sb("g1bc", [128, 2, D], F32)
        for r in range(2):
            P.dma("sync", g1bc[:, r, :], S["modrow"][r:r + 1, 2 * D:3 * D].broadcast_to([128, D]))
        lng6 = P.sb("lng", [128, D], F32)
        lnb6 = P.sb("lnb", [128, D], F32)
        P.dma("sync", lng6[:, :], I["ln_mix_g"][L:L + 1, :].broadcast_to([128, D]))
        P.dma("sync", lnb6[:, :], I["ln_mix_b"][L:L + 1, :].broadcast_to([128, D]))
        s2bc = P.sb("s2bc", [128, 2, 2, D], F32)
        for r in range(2):
            P.dma("sync", s2bc[:, 0, r, :], S["modrow"][r:r + 1, 3 * D:4 * D].broadcast_to([128, D]))
            P.dma("sync", s2bc[:, 1, r, :], S["modrow"][r:r + 1, 4 * D:5 * D].broadcast_to([128, D]))
        V(lambda e: e.tensor_scalar_add(s2bc[:, 1, :, :], s2bc[:, 1, :, :], 1.0), [s2bc], [s2bc])
        V(lambda e: e.memset(cntE[:, :], 0.0), [], [cntE])
        fTM = [P.sb(f"fTM{i}", [128, D], BF16) for i in range(3)]
        ftmp = [P.sb(f"ftmp{i}", [128, D], F32) for i in range(3)]
        wr = P.sb("wr", [128, 8, 36], F32)
        P.dma("sync", wr[:, :, 0:4], I["moe_wg"][L].rearrange("(k p) n -> p k n", p=128))
        P.dma("sync", wr[:, :, 4:36], I["moe_we"][L].rearrange("(k p) n -> p k n", p=128))
        xt6v = [P.sb(f"xt6{i}", [128, D], F32) for i in range(3)]
        u6v = [P.sb(f"u6{i}", [128, D], F32) for i in range(3)]
        xn6v = [P.sb(f"xn6{i}", [128, D], F32) for i in range(3)]
        fTf = [P.sb(f"fTf{i}", [128, 8, 128], F32) for i in range(3)]
        stb6 = [(P.sb(f"st6{i}", [128, 2, 6], F32), P.sb(f"mv6{i}", [128, 2], F32), P.sb(f"rs6{i}", [128, 1], F32)) for i in range(3)]
        stc = [(P.sb(f"st7{i}", [128, 2, 6], F32), P.sb(f"mv7{i}", [128, 2], F32), P.sb(f"rs7{i}", [128, 1], F32)) for i in range(3)]
        rt2 = [dict(oh1=P.sb(f"oh1{i}", [128, 32], F32), oh2=P.sb(f"oh2{i}", [128, 32], F32), slot=P.sb(f"slot{i}", [128, 32], F32),
                    dst=P.sb(f"dst{i}", [128, 32], F32), ovm=P.sb(f"ovm{i}", [128, 32], F32), tm=P.sb(f"tm{i}", [128, 32], F32),
                    wvv=P.sb(f"wv{i}", [128, 32], F32), c4=P.sb(f"c4{i}", [128, 4], F32)) for i in range(3)]
        rt = [dict(lg=P.sb(f"lg{i}", [128, 36], F32), s1=P.sb(f"s1{i}", [128, 8], F32), oh=P.sb(f"oh{i}", [128, 4], F32),
                   ml_=P.sb(f"mlg{i}", [128, 32], F32), t32=P.sb(f"t32{i}", [128, 32], F32), ex=P.sb(f"ex{i}", [128, 32], F32),
                   e4=P.sb(f"e4{i}", [128, 4], F32)) for i in range(3)]
        def stA1(t):
            b = t % 3
            r = 1 if t < NCTX_T else 0
            tsl = slice(t * 128, (t + 1) * 128)
            P.dma("sync", xt6v[b][:, :], x_cur[tsl, :])
            for half in range(2):
                for k in range(8):
                    mm(pb[half][:, :], mixT[:, k, tsl], wo[:, k, half * 512:(half + 1) * 512], k == 0, k == 7, r=[mixT, wo], w=[pb[half]])
                V(lambda e, b=b, half=half, r=r: e.tensor_tensor(u6v[b][:, half * 512:(half + 1) * 512], pb[half][:, :],
                                                                  g1bc[:, r, half * 512:(half + 1) * 512], ALU.mult), [pb[half], g1bc], [u6v[b]])
                if debug:
                    pass
            V(lambda e, b=b: e.scalar_tensor_tensor(u6v[b][:, :], xt6v[b][:, :], DN_ALPHA, u6v[b][:, :], ALU.mult, ALU.add), [xt6v[b], u6v[b]], [u6v[b]])
            mean, rstd = ln_stats(u6v[b], stb6[b])
            V(lambda e, b=b, mean=mean, rstd=rstd: e.tensor_scalar(u6v[b][:, :], u6v[b][:, :], mean, rstd, ALU.subtract, ALU.mult),
              [u6v[b], stb6[b][1], stb6[b][2]], [u6v[b]])
            G(lambda e, b=b: e.tensor_tensor(u6v[b][:, :], u6v[b][:, :], lng6[:, :], ALU.mult), [u6v[b], lng6], [u6v[b]])
            G(lambda e, b=b: e.tensor_tensor(u6v[b][:, :], u6v[b][:, :], lnb6[:, :], ALU.add), [u6v[b], lnb6], [u6v[b]])
            P.dma("sync", S["x1"][tsl, :], u6v[b][:, :])

        def stA2(t):
            b = t % 3
            r = 1 if t < NCTX_T else 0
            tsl = slice(t * 128, (t + 1) * 128)
            mean2, rstd2 = ln_stats(u6v[b], stc[b])
            V(lambda e, b=b, mean2=mean2, rstd2=rstd2: e.tensor_scalar(xn6v[b][:, :], u6v[b][:, :], mean2, rstd2, ALU.subtract, ALU.mult),
              [u6v[b], stc[b][1], stc[b][2]], [xn6v[b]])
            for ch in range(8):
                bank = 2 + ch // 4
                tr(pb[bank][:, (ch % 4) * 128:(ch % 4 + 1) * 128], xn6v[b][:, ch * 128:(ch + 1) * 128], ident_f[:, :], [xn6v[b], ident_f], [pb[bank]])
            for ch in range(8):
                bank = 2 + ch // 4
                i_ = pb[bank][:, (ch % 4) * 128:(ch % 4 + 1) * 128]
                A(lambda e, b=b, ch=ch, i_=i_, r=r: e.activation(fTf[b][:, ch, :], i_, AF.Identity, bias=modcol[:, 24 + ch, r:r + 1],
                                                                 scale=modcol[:, 32 + ch, r:r + 1]), [pb[bank], modcol], [fTf[b]])
            G(lambda e, b=b, r=r: e.tensor_tensor(ftmp[b][:, :], xn6v[b][:, :], s2bc[:, 1, r, :], ALU.mult), [xn6v[b], s2bc], [ftmp[b]])
            G(lambda e, b=b, r=r: e.tensor_tensor(fTM[b][:, :], ftmp[b][:, :], s2bc[:, 0, r, :], ALU.add), [ftmp[b], s2bc], [fTM[b]])

        def stB(t):
            b = t % 3
            r = 1 if t < NCTX_T else 0
            tsl = slice(t * 128, (t + 1) * 128)
            for k in range(8):
                mm(pb[4][:, 0:36], fTf[b][:, k, :], wr[:, k, :], k == 0, k == 7, r=[fTf[b], wr], w=[pb[4]])
            R_ = rt[b]
            lg, s1, oh, mlg, t32, ex, e4 = R_["lg"], R_["s1"], R_["oh"], R_["ml_"], R_["t32"], R_["ex"], R_["e4"]
            V(lambda e, lg=lg: e.tensor_copy(lg[:, :], pb[4][:, 0:36]), [pb[4]], [lg])
            V(lambda e, lg=lg, s1=s1: e.tensor_reduce(s1[:, 0:1], lg[:, 0:4], AX.X, ALU.max), [lg], [s1])
            V(lambda e, s1=s1: e.tensor_scalar_mul(s1[:, 1:2], s1[:, 0:1], -1.0), [s1], [s1])
            A(lambda e, lg=lg, s1=s1, e4=e4: e.activation(e4[:, :], lg[:, 0:4], AF.Exp, bias=s1[:, 1:2], scale=1.0), [lg, s1], [e4])
            V(lambda e, s1=s1, e4=e4: e.tensor_reduce(s1[:, 2:3], e4[:, :], AX.X, ALU.add), [e4], [s1])
            V(lambda e, s1=s1: e.reciprocal(s1[:, 2:3], s1[:, 2:3]), [s1], [s1])
            V(lambda e, lg=lg, s1=s1, oh=oh: e.tensor_scalar(oh[:, :], lg[:, 0:4], s1[:, 0:1], None, ALU.is_ge), [lg, s1], [oh])
            V(lambda e, oh=oh: e.tensor_scalar(oh[:, :], oh[:, :], -1.0, 1e30, ALU.add, ALU.mult), [oh], [oh])
            for g in range(4):
                V(lambda e, g=g, lg=lg, oh=oh, mlg=mlg: e.tensor_scalar(mlg[:, g * 8:(g + 1) * 8], lg[:, 4 + g * 8:4 + (g + 1) * 8],
                                                                       oh[:, g:g + 1], None, ALU.add), [lg, oh], [mlg])
            V(lambda e, mlg=mlg, s1=s1: e.tensor_reduce(s1[:, 3:4], mlg[:, :], AX.X, ALU.max), [mlg], [s1])
            V(lambda e, mlg=mlg, s1=s1, t32=t32: e.tensor_scalar(t32[:, :], mlg[:, :], s1[:, 3:4], -1e30, ALU.is_ge, ALU.mult), [mlg, s1], [t32])
            V(lambda e, mlg=mlg, t32=t32: e.tensor_tensor(t32[:, :], t32[:, :], mlg[:, :], ALU.add), [mlg, t32], [t32])
            V(lambda e, t32=t32, s1=s1: e.tensor_reduce(s1[:, 4:5], t32[:, :], AX.X, ALU.max), [t32], [s1])
            V(lambda e, mlg=mlg, s1=s1, t32=t32: e.tensor_scalar(t32[:, :], mlg[:, :], s1[:, 4:5], None, ALU.is_ge), [mlg, s1], [t32])
            V(lambda e, s1=s1: e.tensor_scalar_mul(s1[:, 5:6], s1[:, 3:4], -1.0), [s1], [s1])
            A(lambda e, mlg=mlg, s1=s1, ex=ex: e.activation(ex[:, :], mlg[:, :], AF.Exp, bias=s1[:, 5:6], scale=1.0), [mlg, s1], [ex])
            V(lambda e, ex=ex, t32=t32: e.tensor_tensor(ex[:, :], ex[:, :], t32[:, :], ALU.mult), [ex, t32], [ex])
            V(lambda e, ex=ex, s1=s1: e.tensor_reduce(s1[:, 6:7], ex[:, :], AX.X, ALU.add), [ex], [s1])
            V(lambda e, s1=s1: e.reciprocal(s1[:, 6:7], s1[:, 6:7]), [s1], [s1])
            V(lambda e, s1=s1: e.tensor_tensor(s1[:, 6:7], s1[:, 6:7], s1[:, 2:3], ALU.mult), [s1], [s1])
            V(lambda e, ex=ex, s1=s1, t=t: e.tensor_scalar(Wt[:, t, :], ex[:, :], s1[:, 6:7], None, ALU.mult), [ex, s1], [Wt])
            Q_ = rt2[b]
            oh1, oh2, slot, dst, ovm, tm, wvv, c4 = Q_["oh1"], Q_["oh2"], Q_["slot"], Q_["dst"], Q_["ovm"], Q_["tm"], Q_["wvv"], Q_["c4"]
            V(lambda e, mlg=mlg, s1=s1, oh1=oh1: e.tensor_scalar(oh1[:, :], mlg[:, :], s1[:, 3:4], None, ALU.is_ge), [mlg, s1], [oh1])
            V(lambda e, t32=t32, oh1=oh1, oh2=oh2: e.tensor_tensor(oh2[:, :], t32[:, :], oh1[:, :], ALU.subtract), [t32, oh1], [oh2])
            mm(pb[5][:, 0:32], tri_f[:, :], t32[:, :], True, True, r=[tri_f, t32], w=[pb[5]])
            mm(pb[6][:, 0:32], ones_f[:, :], t32[:, :], True, True, r=[ones_f, t32], w=[pb[6]])
            V(lambda e, slot=slot, t32=t32: e.tensor_tensor(slot[:, :], pb[5][:, 0:32], t32[:, :], ALU.subtract), [pb[5], t32], [slot])
            V(lambda e, slot=slot: e.tensor_tensor(slot[:, :], slot[:, :], cntE[:, :], ALU.add), [slot, cntE], [slot])
            V(lambda e: e.tensor_tensor(cntE[:, :], cntE[:, :], pb[6][:, 0:32], ALU.add), [cntE, pb[6]], [cntE])
            V(lambda e, slot=slot, ovm=ovm: e.tensor_scalar(ovm[:, :], slot[:, :], float(CAP), None, ALU.is_ge), [slot], [ovm])
            V(lambda e, slot=slot, dst=dst: e.tensor_tensor(dst[:, :], slot[:, :], ecolC[:, :], ALU.add), [slot, ecolC], [dst])
            V(lambda e, dst=dst, ovm=ovm: e.scalar_tensor_tensor(dst[:, :], ovm[:, :], 1.0e6, dst[:, :], ALU.mult, ALU.add), [ovm, dst], [dst])
            V(lambda e, t=t, ovm=ovm, wvv=wvv: e.tensor_tensor(wvv[:, :], Wt[:, t, :], ovm[:, :], ALU.mult), [Wt, ovm], [wvv])
            V(lambda e, t=t, wvv=wvv: e.tensor_tensor(wvv[:, :], Wt[:, t, :], wvv[:, :], ALU.subtract), [Wt, wvv], [wvv])
            for k_, oh in ((0, oh1), (1, oh2)):
                V(lambda e, oh=oh, dst=dst, tm=tm: e.tensor_tensor(tm[:, :], oh[:, :], dst[:, :], ALU.mult), [oh, dst], [tm])
                V(lambda e, tm=tm, c4=c4, k_=k_: e.tensor_reduce(c4[:, k_:k_ + 1], tm[:, :], AX.X, ALU.add), [tm], [c4])
                V(lambda e, oh=oh, wvv=wvv, tm=tm: e.tensor_tensor(tm[:, :], oh[:, :], wvv[:, :], ALU.mult), [oh, wvv], [tm])
                V(lambda e, tm=tm, t=t, k_=k_: e.tensor_reduce(wsel[:, t, k_:k_ + 1], tm[:, :], AX.X, ALU.add), [tm], [wsel])
            V(lambda e, c4=c4, t=t: e.tensor_copy(idxs[:, t, :], c4[:, 0:2]), [c4], [idxs])
            for k_ in range(2):
                P.ops.append(dict(eng="gpsimd", dma=True, bar=None, r=P._keys([fTM[b], idxs]), w=[("xslots", t, k_)],
                                  fn=lambda e, b=b, t=t, k_=k_: e.indirect_dma_start(
                                      out=S["xslots"][:, :], out_offset=bass.IndirectOffsetOnAxis(idxs[:, t, k_:k_ + 1], 0),
                                      in_=fTM[b][:, :], in_offset=None, bounds_check=get_bc(e), oob_is_err=False)))
        for i_ in range(NT + 2):
            if i_ < NT:
                stA1(i_)
            if 0 <= i_ - 1 < NT:
                stA2(i_ - 1)
            if 0 <= i_ - 2 < NT:
                stB(i_ - 2)
        if debug:
            for t in range(NT):
                P.dma("sync", S["dbg_W"][t * 128:(t + 1) * 128, :], Wt[:, t, :])
        if stop_after == "P6":
            return None

        P.label = f"L{L}_P7"
        P.barrier()
        P.release(m6)
        yacc = P.sb("yacc", [128, NT, D], F32)
        after_yacc = P.mark()
        w1b = [P.sb(f"w1b{i}", [128, 8, 512], BF16) for i in range(2)]
        w3b = [P.sb(f"w3b{i}", [128, 8, 512], BF16) for i in range(2)]
        w2b = [P.sb(f"w2b{i}", [128, 4, D], BF16) for i in range(2)]
        xs = [P.sb(f"xs{i}", [128, NST, D], BF16) for i in range(2)]
        xT = [P.sb(f"xTs{i}", [128, 8, CAP], BF16) for i in range(2)]
        gTb = [P.sb(f"gTb{i}", [128, 4, CAP], BF16) for i in range(2)]
        s1b = [P.sb(f"s1b{i}", [128, 512], F32) for i in range(2)]
        ysb = [P.sb(f"ysb{i}", [128, D], BF16) for i in range(2)]
        SB = [(0, CAP // 2), (CAP // 2, CAP // 2)] if CAP > 512 else [(0, CAP)]
        yi = 0

        def moe_load(ex_):
            eb = ex_ % 2
            P.dma("gpsimd", w1b[eb][:, :, :], I["moe_w1"][L, ex_].rearrange("(k p) n -> p k n", p=128))
            P.dma("gpsimd", w3b[eb][:, :, :], I["moe_w3"][L, ex_].rearrange("(k p) n -> p k n", p=128))
            P.dma("gpsimd", w2b[eb][:, :, :], I["moe_w2"][L, ex_].rearrange("(k p) n -> p k n", p=128))
            P.dma("sync", xs[eb][:, :, :], S["xslots"][ex_ * CAP:(ex_ + 1) * CAP, :].rearrange("(s p) d -> p s d", p=128),
                  reads=[("xslots", t_, k__) for t_ in range(NT) for k__ in range(2)])

        def moe_transposes(ex_):
            eb = ex_ % 2
            for st in range(NST):
                bank = 6 + st % 2
                pT = pb[bank][:, :].bitcast(BF16)
                for k in range(8):
                    tr(pT[:, k * 128:(k + 1) * 128], xs[eb][:, st, k * 128:(k + 1) * 128], ident_bf[:, :], [xs[eb], ident_bf], [pb[bank]])
                if st % 2 == 0:
                    A(lambda e, eb=eb, st=st, pT=pT: e.copy(xT[eb][:, :, st * 128:(st + 1) * 128], pT[:, :].rearrange("p (k s) -> p k s", k=8)),
                      [pb[bank]], [xT[eb]])
                else:
                    V(lambda e, eb=eb, st=st, pT=pT: e.tensor_copy(xT[eb][:, :, st * 128:(st + 1) * 128], pT[:, :].rearrange("p (k s) -> p k s", k=8)),
                      [pb[bank]], [xT[eb]])

        moe_load(0)
        moe_transposes(0)
        for ex_ in range(NE):
            eb = ex_ % 2
            if ex_ + 1 < NE:
                moe_load(ex_ + 1)
            gT = gTb[eb]
            for (t0, tn) in SB:
                for hc in range(4):
                    b1, b3 = (hc % 2) * 2, (hc % 2) * 2 + 1
                    for k in range(8):
                        mm(pb[b1][:, 0:tn], w1b[eb][:, k, hc * 128:(hc + 1) * 128], xT[eb][:, k, t0:t0 + tn], k == 0, k == 7,
                           r=[w1b[eb], xT[eb]], w=[pb[b1]])
                    for k in range(8):
                        mm(pb[b3][:, 0:tn], w3b[eb][:, k, hc * 128:(hc + 1) * 128], xT[eb][:, k, t0:t0 + tn], k == 0, k == 7,
                           r=[w3b[eb], xT[eb]], w=[pb[b3]])
                    sb_ = s1b[hc % 2]
                    A(lambda e, sb_=sb_, b1=b1, tn=tn: e.activation(sb_[:, 0:tn], pb[b1][:, 0:tn], AF.Silu), [pb[b1]], [sb_])
                    V(lambda e, sb_=sb_, b3=b3, tn=tn, gT=gT, hc=hc, t0=t0: e.tensor_tensor(gT[:, hc, t0:t0 + tn], sb_[:, 0:tn], pb[b3][:, 0:tn], ALU.mult),
                      [sb_, pb[b3]], [gT])
            if ex_ + 1 < NE:
                moe_transposes(ex_ + 1)
            for st in range(NST):
                yb = ysb[yi % 2]
                yi += 1
                for half in range(2):
                    bank = 4 + half
                    for hc in range(4):
                        mm(pb[bank][:, :], gT[:, hc, st * 128:(st + 1) * 128], w2b[eb][:, hc, half * 512:(half + 1) * 512], hc == 0, hc == 3,
                           r=[gT, w2b[eb]], w=[pb[bank]])
                    if half == 0:
                        A(lambda e, yb=yb, bank=bank: e.copy(yb[:, 0:512], pb[bank][:, :]), [pb[bank]], [yb])
                    else:
                        V(lambda e, yb=yb, bank=bank: e.tensor_copy(yb[:, 512:1024], pb[bank][:, :]), [pb[bank]], [yb])
                r0 = ex_ * CAP + st * 128
                P.dma("sync", S["yslots"][r0:r0 + 128, :], yb[:, :], writes=[("yslots", ex_, st)])
        P.label = f"L{L}_P7_combine"
        yg = [P.sb(f"yg{i}", [128, D], BF16) for i in range(4)]
        for i in range(4):
            V(lambda e, i=i: e.memset(yg[i][:, :], 0.0), [], [yg[i]])
        for t in range(NT):
            g0, g1_ = yg[(2 * t) % 4], yg[(2 * t + 1) % 4]
            for k_, gt in ((0, g0), (1, g1_)):
                P.ops.append(dict(eng="gpsimd", dma=True, bar=None, r=[("yslots", e_, s_) for e_ in range(NE) for s_ in range(NST)] + P._keys([idxs]), w=P._keys([gt]),
                                  fn=lambda e, t=t, k_=k_, gt=gt: e.indirect_dma_start(
                                      out=gt[:, :], out_offset=None, in_=S["yslots"][:, :],
                                      in_offset=bass.IndirectOffsetOnAxis(idxs[:, t, k_:k_ + 1], 0),
                                      bounds_check=get_bc(e), oob_is_err=False)))
            V(lambda e, t=t, g0=g0: e.tensor_scalar(yacc[:, t, :], g0[:, :], wsel[:, t, 0:1], None, ALU.mult), [g0, wsel], [("yacc", t)])
            V(lambda e, t=t, g1_=g1_: e.scalar_tensor_tensor(yacc[:, t, :], g1_[:, :], wsel[:, t, 1:2], yacc[:, t, :], ALU.mult, ALU.add),
              [g1_, wsel, ("yacc", t)], [("yacc", t)])
        if debug:
            for t in range(NT):
                P.dma("sync", S["dbg_moe"][t * 128:(t + 1) * 128, :], yacc[:, t, :], reads=[("yacc", t)])
        if stop_after == "P7":
            return None

        P.label = f"L{L}_P8"
        P.barrier()
        P.release(after_yacc)
        g2bc = P.sb("g2bc", [128, 2, D], F32)
        for r in range(2):
            P.dma("sync", g2bc[:, r, :], S["modrow"][r:r + 1, 5 * D:6 * D].broadcast_to([128, D]))
        lng8v = P.sb("lng8", [128, D], F32)
        lnb8v = P.sb("lnb8", [128, D], F32)
        P.dma("sync", lng8v[:, :], I["ln_ffn_g"][L:L + 1, :].broadcast_to([128, D]))
        P.dma("sync", lnb8v[:, :], I["ln_ffn_b"][L:L + 1, :].broadcast_to([128, D]))
        xt8v = [P.sb(f"xt8{i}", [128, D], F32) for i in range(4)]
        u8v = [P.sb(f"u8{i}", [128, D], F32) for i in range(4)]
        stb8 = [(P.sb(f"st8{i}", [128, 2, 6], F32), P.sb(f"mv8{i}", [128, 2], F32), P.sb(f"rs8{i}", [128, 1], F32)) for i in range(4)]
        tiles8 = [t for t in range(NT) if not (last and t < NCTX_T)]

        def p8_s1(t):
            b = t % 4
            r = 1 if t < NCTX_T else 0
            st, mv, rstd = stb8[b]
            xb, ub = xt8v[b], u8v[b]
            P.dma("sync", xb[:, :], S["x1"][t * 128:(t + 1) * 128, :])
            V(lambda e: e.tensor_tensor(ub[:, :], yacc[:, t, :], g2bc[:, r, :], ALU.mult), [("yacc", t), g2bc], [ub])
            V(lambda e: e.scalar_tensor_tensor(ub[:, :], xb[:, :], DN_ALPHA, ub[:, :], ALU.mult, ALU.add), [xb, ub], [ub])
            V(lambda e: e.bn_stats(st[:, 0, :], ub[:, 0:512]), [ub], [st])
            V(lambda e: e.bn_stats(st[:, 1, :], ub[:, 512:1024]), [ub], [st])
            V(lambda e: e.bn_aggr(mv[:, :], st[:, :, :].rearrange("p a b -> p (a b)")), [st], [mv])

        def p8_s2(t):
            b = t % 4
            st, mv, rstd = stb8[b]
            ub = u8v[b]
            A(lambda e: e.activation(rstd[:, :], mv[:, 1:2], AF.Sqrt, bias=eps_col[:, :], scale=1.0), [mv, eps_col], [rstd])
            V(lambda e: e.reciprocal(rstd[:, :], rstd[:, :]), [rstd], [rstd])
            V(lambda e: e.tensor_scalar(ub[:, :], ub[:, :], mv[:, 0:1], rstd[:, 0:1], ALU.subtract, ALU.mult), [ub, mv, rstd], [ub])

        def p8_s3(t):
            b = t % 4
            ub = u8v[b]
            G(lambda e: e.tensor_tensor(ub[:, :], ub[:, :], lng8v[:, :], ALU.mult), [ub, lng8v], [ub])
            G(lambda e: e.tensor_tensor(ub[:, :], ub[:, :], lnb8v[:, :], ALU.add), [ub, lnb8v], [ub])
            if last:
                P.dma("sync", out_d[(t - NCTX_T) * 128:(t - NCTX_T + 1) * 128, :], ub[:, :])
            else:
                P.dma("sync", S["xnext"][t * 128:(t + 1) * 128, :], ub[:, :])

        n8 = len(tiles8)
        for i_ in range(n8 + 2):
            if i_ < n8:
                p8_s1(tiles8[i_])
            if 0 <= i_ - 1 < n8:
                p8_s2(tiles8[i_ - 1])
            if 0 <= i_ - 2 < n8:
                p8_s3(tiles8[i_ - 2])
        return S["xnext"]

    x_cur = I["xin"]
    for L_ in range(n_layers):
        x_cur = layer(L_, x_cur)
        if x_cur is None:
            break

    stats = P.emit()
    global _LAST_VLABELS
    _LAST_VLABELS = P.vlabels
    return nc, stats, consts, list(S.keys())


_CACHE = {}


def make_in_maps(inputs, consts):
    x = np.asarray(inputs["x"], np.float32)
    ctx = np.asarray(inputs["ctx"], np.float32)
    c = np.asarray(inputs["c"], np.float32)
    c_ctx = np.asarray(inputs["c_ctx"], np.float32)
    shared = {k: np.ascontiguousarray(np.asarray(inputs[k], np.float32)) for k in IN_SHAPES if k not in ("xin", "c2")}
    shared.update(consts)
    maps = []
    for b in range(x.shape[0]):
        m = dict(shared)
        m["xin"] = np.ascontiguousarray(np.concatenate([ctx[b], x[b]], axis=0))
        m["c2"] = np.ascontiguousarray(np.stack([c[b], c_ctx], axis=1))
        maps.append(m)
    return maps


def kernel(**inputs):
    if "nc" not in _CACHE:
        nc, stats, consts, _ = build(debug=False)
        _CACHE["nc"] = nc
        _CACHE["consts"] = consts
    nc = _CACHE["nc"]
    maps = make_in_maps(inputs, _CACHE["consts"])
    res = run_bass_kernel_spmd(nc, maps, core_ids=list(range(8)))
    out = np.stack([np.asarray(r["out"], np.float32) for r in res.results], axis=0)
    return out
```
